# Optimizing a Trainium2 kernel written in Bass

```python
import jax, jax.numpy as jnp
from jax import lax
import numpy as np

D_MODEL = 1024
BATCH = 4
SEQ = 4096
DEPTH = 1

MEM_LEN = 256
POOL_WINDOWS = (2, 4, 8, 16)
POOL_GROUPS = 4
POOL_GROUP_DIM = D_MODEL // 8
POOL_WIDTH = POOL_GROUPS * POOL_GROUP_DIM
RET_HEADS = 4
RET_QK_DIM = D_MODEL // 8
RET_V_DIM = D_MODEL // 4
RET_QK_WIDTH = RET_HEADS * RET_QK_DIM
RET_V_WIDTH = RET_HEADS * RET_V_DIM
RET_CHUNK = 128
ROPE_BASE = 10000.0
XA_HEADS = 4
XA_HEAD_DIM = D_MODEL // 8
XA_WIDTH = XA_HEADS * XA_HEAD_DIM
N_BRANCHES = 3
IN_WIDTHS = (POOL_WIDTH, RET_QK_WIDTH, RET_QK_WIDTH, RET_V_WIDTH, RET_V_WIDTH, XA_WIDTH, N_BRANCHES * D_MODEL)
N_GROUPS = 4
EXPERTS_PER_GROUP = 8
TOP_K = 2
D_EXPERT = D_MODEL // 4
LN_EPS = 1e-5
ALPHA = (2.0 * DEPTH) ** 0.25
BETA = (8.0 * DEPTH) ** -0.25

kernel_name = 'hybrid_pool_retention_memory_hmoe'


def layer_norm(x, w, b):
    xf = x.astype(jnp.float32)
    mu = jnp.mean(xf, axis=-1, keepdims=True)
    xc = xf - mu
    var = jnp.mean(xc * xc, axis=-1, keepdims=True)
    return (xc * lax.rsqrt(var + LN_EPS) * w + b).astype(x.dtype)


def head_group_norm(y, w):
    mu = jnp.mean(y, axis=-1, keepdims=True)
    yc = y - mu
    var = jnp.mean(yc * yc, axis=-1, keepdims=True)
    return yc * lax.rsqrt(var + LN_EPS) * w


def multiscale_pool(u, w_grp, scale):
    B, S, _ = u.shape
    uf = u.astype(jnp.float32)
    csum = jnp.pad(jnp.cumsum(uf, axis=1), ((0, 0), (1, 0), (0, 0)))
    hi = jnp.arange(1, S + 1)
    outs = []
    for g, w in enumerate(POOL_WINDOWS):
        sl = slice(g * POOL_GROUP_DIM, (g + 1) * POOL_GROUP_DIM)
        lo = jnp.maximum(hi - w, 0)
        cnt = (hi - lo).astype(jnp.float32)[None, :, None]
        mean = (csum[:, 1:, sl] - csum[:, lo, sl]) / cnt
        outs.append(mean - uf[:, :, sl])
    pooled = jnp.stack(outs, axis=2)
    mixed = jnp.einsum('bsgc,gcd->bsgd', pooled, w_grp.astype(jnp.float32))
    return (mixed.reshape(B, S, POOL_WIDTH) * scale).astype(u.dtype)


def rotary(x, positions):
    d = x.shape[-1]
    inv_freq = ROPE_BASE ** (-jnp.arange(d // 2, dtype=jnp.float32) / (d // 2))
    ang = positions.astype(jnp.float32)[:, :, None, None] * inv_freq
    cos, sin = jnp.cos(ang), jnp.sin(ang)
    xf = x.astype(jnp.float32)
    x1, x2 = xf[..., : d // 2], xf[..., d // 2:]
    return jnp.concatenate([x1 * cos - x2 * sin, x2 * cos + x1 * sin], axis=-1)


def retention(q, k, v):
    B, S, H, dk = q.shape
    dv = v.shape[-1]
    C = RET_CHUNK
    N = S // C
    log_gamma = jnp.log1p(-jnp.exp2(-5.0 - jnp.arange(H, dtype=jnp.float32)))
    qc = q.astype(jnp.float32).reshape(B, N, C, H, dk)
    kc = (k.astype(jnp.float32) * dk ** -0.5).reshape(B, N, C, H, dk)
    vc = v.astype(jnp.float32).reshape(B, N, C, H, dv)
    pos = jnp.arange(C, dtype=jnp.float32)
    diff = pos[:, None] - pos[None, :]
    intra_decay = jnp.where(diff >= 0, jnp.exp(log_gamma[:, None, None] * jnp.maximum(diff, 0.0)), 0.0)
    scores = jnp.einsum('bnchd,bnshd->bnhcs', qc, kc) * intra_decay
    intra = jnp.einsum('bnhcs,bnshe->bnche', scores, vc)
    k_decay = jnp.exp(log_gamma[:, None] * (C - 1.0 - pos)[None, :])
    kv = jnp.einsum('bnshd,hs,bnshe->nbhde', kc, k_decay, vc)
    chunk_decay = jnp.exp(log_gamma * C)[None, :, None, None]

    def step(state, kv_n):
        return chunk_decay * state + kv_n, state

    _, prev = lax.scan(step, jnp.zeros((B, H, dk, dv), jnp.float32), kv)
    q_decay = jnp.exp(log_gamma[:, None] * (pos + 1.0)[None, :])
    cross = jnp.einsum('bnchd,hc,nbhde->bnche', qc, q_decay, prev)
    return (intra + cross).reshape(B, S, H, dv)


def memory_cross_attention(q, mem, w_mem_kv):
    B, S, _ = q.shape
    kv = mem @ w_mem_kv
    k, v = jnp.split(kv, 2, axis=-1)
    qh = q.reshape(B, S, XA_HEADS, XA_HEAD_DIM)
    kh = k.reshape(B, -1, XA_HEADS, XA_HEAD_DIM)
    vh = v.reshape(B, -1, XA_HEADS, XA_HEAD_DIM)
    s = jnp.einsum('bshd,bmhd->bhsm', qh, kh).astype(jnp.float32) * XA_HEAD_DIM ** -0.5
    p = jax.nn.softmax(s, axis=-1).astype(vh.dtype)
    o = jnp.einsum('bhsm,bmhd->bshd', p, vh)
    return o.reshape(B, S, XA_WIDTH)


def hierarchical_moe(h, w_grp_router, b_grp_router, w_exp_router, b_exp_router, w_gate, w_up, w_down):
    B, S, D = h.shape
    t = h.reshape(B * S, D)
    grp_prob = jax.nn.softmax((t @ w_grp_router).astype(jnp.float32) + b_grp_router, axis=-1)
    p_grp, grp_sel = lax.top_k(grp_prob, 1)
    grp_onehot = jax.nn.one_hot(grp_sel[:, 0], N_GROUPS, dtype=jnp.float32)
    exp_logits = ((t @ w_exp_router).astype(jnp.float32) + b_exp_router).reshape(-1, N_GROUPS, EXPERTS_PER_GROUP)
    exp_logits = jnp.einsum('tge,tg->te', exp_logits, grp_onehot)
    top_v, top_i = lax.top_k(exp_logits, TOP_K)
    top_w = jax.nn.softmax(top_v, axis=-1) * p_grp
    w_in_grp = jnp.einsum('tk,tke->te', top_w, jax.nn.one_hot(top_i, EXPERTS_PER_GROUP, dtype=jnp.float32))
    gate = (grp_onehot[:, :, None] * w_in_grp[:, None, :]).astype(h.dtype)
    out = jnp.zeros_like(t)
    for g in range(N_GROUPS):
        a = jnp.einsum('td,edf->tef', t, w_gate[g])
        b = jnp.einsum('td,edf->tef', t, w_up[g])
        act = jax.nn.silu(a) * b * gate[:, g, :, None]
        out = out + jnp.einsum('tef,efd->td', act, w_down[g])
    return out.reshape(B, S, D)


def setup_inputs(seed: int = 0) -> dict:
    key = jax.random.key(seed)
    ks = jax.random.split(key, 24)
    f32 = jnp.float32

    def nrm(k, shape, fan_in, gain=1.0):
        return jax.random.normal(k, shape, f32) * (gain * fan_in ** -0.5)

    x = jax.random.normal(ks[0], (BATCH, SEQ, D_MODEL), f32)
    mem = jax.random.normal(ks[1], (BATCH, MEM_LEN, D_MODEL), f32)
    positions = jnp.arange(SEQ, dtype=jnp.int32)[None, :] + jax.random.randint(ks[2], (BATCH, 1), 0, 1024, dtype=jnp.int32)
    in_keys = jax.random.split(ks[3], len(IN_WIDTHS))
    in_gains = (1.0, 1.0, 1.0, BETA, 1.0, 1.0, 1.0)
    w_in = jnp.concatenate([nrm(k, (DEPTH, D_MODEL, w), D_MODEL, g) for k, w, g in zip(in_keys, IN_WIDTHS, in_gains)], axis=-1)
    w_pool_grp = nrm(ks[4], (DEPTH, POOL_GROUPS, POOL_GROUP_DIM, POOL_GROUP_DIM), POOL_GROUP_DIM)
    pool_scale = 0.5 + 0.05 * jax.random.normal(ks[5], (DEPTH, POOL_WIDTH), f32)
    ret_gn_w = 1.0 + 0.02 * jax.random.normal(ks[6], (DEPTH, RET_HEADS, RET_V_DIM), f32)
    w_mem_kv = jnp.concatenate([nrm(ks[7], (DEPTH, D_MODEL, XA_WIDTH), D_MODEL),
                                nrm(ks[8], (DEPTH, D_MODEL, XA_WIDTH), D_MODEL, BETA)], axis=-1)
    w_br_pool = nrm(ks[9], (DEPTH, POOL_WIDTH, D_MODEL), POOL_WIDTH)
    w_br_ret = nrm(ks[10], (DEPTH, RET_V_WIDTH, D_MODEL), RET_V_WIDTH)
    w_br_xa = nrm(ks[11], (DEPTH, XA_WIDTH, D_MODEL), XA_WIDTH)
    w_out = nrm(ks[12], (DEPTH, D_MODEL, D_MODEL), D_MODEL, BETA)
    ln1_w = 1.0 + 0.02 * jax.random.normal(ks[13], (DEPTH, D_MODEL), f32)
    ln1_b = 0.02 * jax.random.normal(ks[14], (DEPTH, D_MODEL), f32)
    w_grp_router = nrm(ks[15], (DEPTH, D_MODEL, N_GROUPS), D_MODEL)
    b_grp_router = 0.01 * jax.random.normal(ks[16], (DEPTH, N_GROUPS), f32)
    w_exp_router = nrm(ks[17], (DEPTH, D_MODEL, N_GROUPS * EXPERTS_PER_GROUP), D_MODEL)
    b_exp_router = 0.01 * jax.random.normal(ks[18], (DEPTH, N_GROUPS * EXPERTS_PER_GROUP), f32)
    w_exp_gate = nrm(ks[19], (DEPTH, N_GROUPS, EXPERTS_PER_GROUP, D_MODEL, D_EXPERT), D_MODEL)
    w_exp_up = nrm(ks[20], (DEPTH, N_GROUPS, EXPERTS_PER_GROUP, D_MODEL, D_EXPERT), D_MODEL)
    w_exp_down = nrm(ks[21], (DEPTH, N_GROUPS, EXPERTS_PER_GROUP, D_EXPERT, D_MODEL), D_EXPERT, BETA)
    ln2_w = 1.0 + 0.02 * jax.random.normal(ks[22], (DEPTH, D_MODEL), f32)
    ln2_b = 0.02 * jax.random.normal(ks[23], (DEPTH, D_MODEL), f32)
    return {'x': x, 'mem': mem, 'positions': positions, 'w_in': w_in, 'w_pool_grp': w_pool_grp,
            'pool_scale': pool_scale, 'ret_gn_w': ret_gn_w, 'w_mem_kv': w_mem_kv, 'w_br_pool': w_br_pool,
            'w_br_ret': w_br_ret, 'w_br_xa': w_br_xa, 'w_out': w_out, 'ln1_w': ln1_w, 'ln1_b': ln1_b,
            'w_grp_router': w_grp_router, 'b_grp_router': b_grp_router, 'w_exp_router': w_exp_router,
            'b_exp_router': b_exp_router, 'w_exp_gate': w_exp_gate, 'w_exp_up': w_exp_up,
            'w_exp_down': w_exp_down, 'ln2_w': ln2_w, 'ln2_b': ln2_b}


def reference(x, mem, positions, w_in, w_pool_grp, pool_scale, ret_gn_w, w_mem_kv, w_br_pool, w_br_ret,
              w_br_xa, w_out, ln1_w, ln1_b, w_grp_router, b_grp_router, w_exp_router, b_exp_router,
              w_exp_gate, w_exp_up, w_exp_down, ln2_w, ln2_b):
    B, S, _ = x.shape
    split_points = [int(p) for p in np.cumsum(IN_WIDTHS)[:-1]]
    for l in range(DEPTH):
        proj = x @ w_in[l]
        u_pool, r_q, r_k, r_v, r_g, xa_q, gate_logits = jnp.split(proj, split_points, axis=-1)
        y_pool = multiscale_pool(u_pool, w_pool_grp[l], pool_scale[l])
        q = rotary(r_q.reshape(B, S, RET_HEADS, RET_QK_DIM), positions)
        k = rotary(r_k.reshape(B, S, RET_HEADS, RET_QK_DIM), positions)
        y_ret = retention(q, k, r_v.reshape(B, S, RET_HEADS, RET_V_DIM))
        y_ret = head_group_norm(y_ret, ret_gn_w[l]).reshape(B, S, RET_V_WIDTH)
        y_ret = (jax.nn.silu(r_g.astype(jnp.float32)) * y_ret).astype(x.dtype)
        y_xa = memory_cross_attention(xa_q, mem, w_mem_kv[l])
        g = jax.nn.sigmoid(gate_logits.astype(jnp.float32)).astype(x.dtype).reshape(B, S, N_BRANCHES, D_MODEL)
        merged = (g[:, :, 0] * (y_pool @ w_br_pool[l])
                  + g[:, :, 1] * (y_ret @ w_br_ret[l])
                  + g[:, :, 2] * (y_xa @ w_br_xa[l]))
        x = layer_norm(ALPHA * x + merged @ w_out[l], ln1_w[l], ln1_b[l])
        moe = hierarchical_moe(x, w_grp_router[l], b_grp_router[l], w_exp_router[l], b_exp_router[l],
                               w_exp_gate[l], w_exp_up[l], w_exp_down[l])
        x = layer_norm(ALPHA * x + moe, ln2_w[l], ln2_b[l])
    return x
```

```python
from contextlib import ExitStack
import numpy as np
import concourse.bass as bass
import concourse.mybir as mybir
from concourse.bass_utils import run_bass_kernel_spmd

F32 = mybir.dt.float32
BF16 = mybir.dt.bfloat16
I32 = mybir.dt.int32
ALU = mybir.AluOpType
AF = mybir.ActivationFunctionType
AX = mybir.AxisListType

D = 1024
SEQ = 4096
T = 2048
NT = 16
NG = 4
C = 128
H = 4
DV = 256
NEXP = 32
LN_EPS = 1e-5
ALPHA = 2.0 ** 0.25
NSLOT = 6
ENGS = ("pe", "act", "dve", "pool", "sp")


class Prog:
    def __init__(self, nc, es):
        self.nc = nc
        self.es = es
        self.streams = {e: [] for e in ENGS}
        self.sems = {}
        self.cnt = {}
        self.waited = {e: {} for e in ENGS}
        self.res = {}
        self.store_events = []
        for e in ("pe", "act", "dve", "pool"):
            self._sem(e)

    def _sem(self, name):
        if name not in self.sems:
            self.sems[name] = self.es.enter_context(self.nc.semaphore("s_" + str(name)))
            self.cnt[name] = 0
        return self.sems[name]

    def _deps(self, eng, reads, writes):
        deps = {}

        def add(ev):
            if ev is None:
                return
            s, v = ev
            if deps.get(s, 0) < v:
                deps[s] = v

        for r in reads:
            st = self.res.get(r)
            if st:
                add(st["w"])
        for w in writes:
            st = self.res.get(w)
            if st:
                add(st["w"])
                for ev in st["r"]:
                    add(ev)
        for s, v in deps.items():
            if eng == "pe" and s == "pe":
                continue
            if self.waited[eng].get(s, 0) < v:
                self.waited[eng][s] = v
                sem = self.sems[s]
                self.streams[eng].append(lambda e, sem=sem, v=v: e.wait_ge(sem, v))

    def _mark(self, ev, reads, writes):
        for r in reads:
            st = self.res.setdefault(r, {"w": None, "r": []})
            st["r"].append(ev)
            if len(st["r"]) > 64:
                best = {}
                for s, v in st["r"]:
                    best[s] = max(best.get(s, 0), v)
                st["r"] = list(best.items())
        for w in writes:
            self.res[w] = {"w": ev, "r": []}

    def op(self, eng, fns, reads=(), writes=()):
        if callable(fns):
            fns = [fns]
        self._deps(eng, reads, writes)
        self.cnt[eng] += 1
        ev = (eng, self.cnt[eng])
        sem = self.sems[eng]
        st = self.streams[eng]
        for f in fns[:-1]:
            st.append(f)
        last = fns[-1]
        st.append(lambda e, last=last, sem=sem: last(e).then_inc(sem, 1))
        self._mark(ev, reads, writes)
        return ev

    def dma(self, queue, fn, semkey, reads=(), writes=(), is_store=False):
        self._deps(queue, reads, writes)
        sem = self._sem(semkey)
        self.cnt[semkey] += 16
        ev = (semkey, self.cnt[semkey])
        self.streams[queue].append(lambda e, fn=fn, sem=sem: fn(e).then_inc(sem, 16))
        self._mark(ev, reads, writes)
        if is_store:
            self.store_events.append(ev)
        return ev

    def barrier(self, engs=("pe", "act", "dve", "sp")):
        for eng in engs:
            for sname, v in self.cnt.items():
                if v == 0 or str(sname).startswith("w"):
                    continue
                if eng == "pe" and sname == "pe":
                    continue
                if self.waited[eng].get(sname, 0) < v:
                    self.waited[eng][sname] = v
                    sem = self.sems[sname]
                    self.streams[eng].append(lambda e, sem=sem, v=v: e.wait_ge(sem, v))

    def finish(self):
        for s, v in self.cnt.items():
            if s in ("pe", "act", "dve", "pool") or v == 0:
                continue
            if self.waited["sp"].get(s, 0) < v:
                self.waited["sp"][s] = v
                sem = self.sems[s]
                self.streams["sp"].append(lambda e, sem=sem, v=v: e.wait_ge(sem, v))

    def emit(self):
        nc = self.nc
        with nc.Block() as block:
            @block.tensor
            def _(e):
                for f in self.streams["pe"]:
                    f(e)

            @block.scalar
            def _(e):
                for f in self.streams["act"]:
                    f(e)

            @block.vector
            def _(e):
                for f in self.streams["dve"]:
                    f(e)

            @block.gpsimd
            def _(e):
                for f in self.streams["pool"]:
                    f(e)

            @block.sync
            def _(e):
                for f in self.streams["sp"]:
                    f(e)


class WStream:
    def __init__(self, P, slots, items, n_init=None):
        self.P = P
        self.slots = slots
        self.items = items
        self.idx = {it[0]: i for i, it in enumerate(items)}
        self.emitted = 0
        self.rel = set()
        self.wm = 0
        for _ in range(min(n_init if n_init is not None else len(slots), len(items))):
            self._emit_next()

    def kick(self, after_keys=()):
        self.P._deps("pool", tuple(after_keys), ())
        while self.emitted < self.wm + len(self.slots) and self.emitted < len(self.items):
            self._emit_next()

    def _emit_next(self):
        i = self.emitted
        if i >= len(self.items):
            return
        P = self.P
        s = i % len(self.slots)
        slot = self.slots[s]
        allkeys = [f"ring{s}", f"ring{s}a", f"ring{s}b"]
        P._deps("pool", (), allkeys)
        for k in allkeys:
            P.res[k] = {"w": None, "r": []}
        for sub, fn in self.items[i][1]:
            semkey = f"w{s}{sub}"
            sem = P._sem(semkey)
            P.cnt[semkey] += 16
            ev = (semkey, P.cnt[semkey])
            P.streams["pool"].append(lambda e, fn=fn, slot=slot, sem=sem: fn(e, slot).then_inc(sem, 16))
            P.res[f"ring{s}{sub}"] = {"w": ev, "r": []}
        self.emitted += 1

    def get(self, name):
        i = self.idx[name]
        assert i < self.emitted, f"weight item {name} not yet emitted"
        s = i % len(self.slots)
        keys = [f"ring{s}{sub}" for sub, _ in self.items[i][1]]
        return self.slots[s], keys

    def release(self, name):
        i = self.idx[name]
        self.rel.add(i)
        while self.wm in self.rel:
            self.wm += 1
        while self.emitted < self.wm + len(self.slots) and self.emitted < len(self.items):
            self._emit_next()


def build_program(debug=False):
    nc = bass.Bass("TRN2", target_bir_lowering=False)

    def din(name, shape, dt=F32):
        return nc.dram_tensor(name, list(shape), dt, kind="ExternalInput").ap()

    x_own = din("x_own", [T, D])
    x_prev = din("x_prev", [T, D])
    pos_own = din("pos_own", [128, NT], I32)
    pos_prev = din("pos_prev", [128, NT], I32)
    mem = din("mem", [256, D])
    w_in = din("w_in", [D, 7168])
    w_pool_grp = din("w_pool_grp", [4, 128, 128])
    pool_scale = din("pool_scale", [128, 4])
    ret_gn_w = din("ret_gn_w", [1024])
    w_mem_kv = din("w_mem_kv", [D, 1024])
    w_br_pool = din("w_br_pool", [512, D])
    w_br_ret = din("w_br_ret", [1024, D])
    w_br_xa = din("w_br_xa", [512, D])
    w_out = din("w_out", [D, D])
    ln1_w = din("ln1_w", [D])
    ln1_b = din("ln1_b", [D])
    ln2_w = din("ln2_w", [D])
    ln2_b = din("ln2_b", [D])
    w_router = din("w_router", [D, 36])
    b_router = din("b_router", [1, 36])
    w_exp_gate = din("w_exp_gate", [NEXP, D, 256])
    w_exp_up = din("w_exp_up", [NEXP, D, 256])
    w_exp_down = din("w_exp_down", [NEXP, 256, D])
    c_ident = din("c_ident", [128, 128])
    c_invf = din("c_invf", [128, 64])
    c_maskT = din("c_maskT", [128, 4, 128])
    c_qdec = din("c_qdec", [128, 4])
    c_kdec = din("c_kdec", [128, 4])
    c_invcnt = din("c_invcnt", [128, 4, 16])
    y_out = nc.dram_tensor("y_out", [T, D], F32, kind="ExternalOutput").ap()
    dbg = {}
    if debug:
        dbg["mT"] = nc.dram_tensor("dbg_mT", [128, 8, T], BF16, kind="ExternalOutput").ap()
        dbg["acc"] = nc.dram_tensor("dbg_acc", [128, NT, D], F32, kind="ExternalOutput").ap()
        dbg["gates"] = nc.dram_tensor("dbg_gates", [128, NT, 32], F32, kind="ExternalOutput").ap()
        dbg["z"] = nc.dram_tensor("dbg_z", [128, 8, T], BF16, kind="ExternalOutput").ap()

    with ExitStack() as es:
        P = Prog(nc, es)

        def sb(name, shape, dt):
            return es.enter_context(nc.sbuf_tensor(name, list(shape), dt))

        A = sb("A", [128, 8, T], BF16)
        M = sb("M", [128, 8, T], BF16)
        ACC = sb("ACC", [128, NT, D], F32)
        ring = [sb(f"ring{i}", [128, 4096], BF16) for i in range(NSLOT)]
        UN = sb("UN", [128, 3072], F32)
        lnw = sb("lnw", [128, D], F32)
        lnb = sb("lnb", [128, D], F32)
        identf = sb("identf", [128, 128], F32)
        identb = sb("identb", [128, 128], BF16)
        invf = sb("invf", [128, 64], F32)
        kdec = sb("kdec", [128, 4], F32)
        invcnt = sb("invcnt", [128, 4, 16], F32)
        pscale = sb("pscale", [128, 4], F32)
        wr32 = sb("wr32", [128, 8, 36], F32)
        br32 = sb("br32", [128, 36], F32)
        wr_hi = sb("wr_hi", [128, 8, 36], BF16)
        wr_lo = sb("wr_lo", [128, 8, 36], BF16)
        x1Tlo = sb("x1Tlo", [128, 8, 128], BF16)
        onesb = sb("onesb", [128, 128], BF16)
        wgrp_b = sb("wgrp_b", [128, 4, 128], BF16)
        gates = sb("gates", [128, NT, 32], F32)
        epsT = sb("epsT", [128, 1], F32)
        small = sb("small", [128, 256], F32)
        xTh = sb("xTh", [128, 8, 16], BF16)
        posi = sb("posi", [128, 2, NT], I32)
        posf = sb("posf", [128, 2, NT], F32)

        S = UN[:, 0:1024].rearrange("p (h c) -> p h c", h=4)
        S_bf = UN[:, 1024:1536].bitcast(BF16).rearrange("p (h c) -> p h c", h=4)
        maskT = UN[:, 1536:2048].rearrange("p (h c) -> p h c", h=4)
        qdec = UN[:, 2048:2052]
        qrd = UN[:, 2056:2312].bitcast(BF16).rearrange("p (h c) -> p h c", h=4)
        xo = [UN[:, 0:1024], UN[:, 1024:2048]]
        hib = UN[:, 2048:2560].bitcast(BF16)
        lob = UN[:, 2560:3072].bitcast(BF16)

        accb = ACC[:].rearrange("p t d -> p (t d)")
        accbf = accb.bitcast(BF16)

        def acc_f32(off_bytes, n):
            assert off_bytes % 4 == 0 and off_bytes + 4 * n <= 65536
            return accb[:, off_bytes // 4: off_bytes // 4 + n]

        def acc_bf16(off_bytes, n):
            assert off_bytes % 2 == 0 and off_bytes + 2 * n <= 65536
            return accbf[:, off_bytes // 2: off_bytes // 2 + n]

        Z = acc_bf16(0, 8 * T).rearrange("p (k t) -> p k t", k=8)
        SC0 = 32768
        Mb = M[:].rearrange("p k t -> p (k t)")
        Mf = Mb.bitcast(F32)

        def m_f32(off_bytes, n):
            assert off_bytes % 4 == 0 and off_bytes + 4 * n <= 32768
            return Mf[:, off_bytes // 4: off_bytes // 4 + n]

        def m_bf16(off_bytes, n):
            assert off_bytes % 2 == 0 and off_bytes + 2 * n <= 32768
            return Mb[:, off_bytes // 2: off_bytes // 2 + n]

        PB = [es.enter_context(nc.psum_tensor(f"pb{i}", [128, 1024], F32)) for i in range(4)]

        def bank(i):
            return PB[i // 2][:, (i % 2) * 512:(i % 2) * 512 + 512]

        def bank_bf(i):
            return bank(i).bitcast(BF16)

        def col_chunk(w, col0):
            def f(e, slot):
                return e.dma_start(out=slot[:].rearrange("p (k n) -> p k n", k=8),
                                   in_=w[:, col0:col0 + 512].rearrange("(k p) n -> p k n", p=128))
            return f

        def mat_rows4(w):
            def f(e, slot):
                return e.dma_start(out=slot[:].rearrange("p (k n) -> p k n", k=4),
                                   in_=w.rearrange("(k p) n -> p k n", p=128))
            return f

        def exp_gu(w, ei, c0):
            def f(e, slot):
                return e.dma_start(out=slot[:].rearrange("p (k n) -> p k n", k=8)[:, :, c0:c0 + 256],
                                   in_=w[ei].rearrange("(k p) n -> p k n", p=128))
            return f

        def exp_d(ei):
            def f(e, slot):
                return e.dma_start(out=slot[:, 0:2048].rearrange("p (k n) -> p k n", k=2),
                                   in_=w_exp_down[ei].rearrange("(k p) n -> p k n", p=128))
            return f

        COL_POOL, COL_Q, COL_K, COL_V, COL_G, COL_XQ, COL_GATE = 0, 512, 1024, 1536, 2560, 3584, 4096
        items = [
            ("wk", [("", col_chunk(w_in, COL_K))]),
            ("wv0", [("", col_chunk(w_in, COL_V))]),
            ("wv1", [("", col_chunk(w_in, COL_V + 512))]),
            ("wq", [("", col_chunk(w_in, COL_Q))]),
            ("wg0", [("", col_chunk(w_in, COL_G))]),
            ("wg1", [("", col_chunk(w_in, COL_G + 512))]),
            ("gt1a", [("", col_chunk(w_in, COL_GATE + 1024))]),
            ("gt1b", [("", col_chunk(w_in, COL_GATE + 1536))]),
            ("wbr0", [("", col_chunk(w_br_ret, 0))]),
            ("wbr1", [("", col_chunk(w_br_ret, 512))]),
            ("wpool", [("", col_chunk(w_in, COL_POOL))]),
            ("gt0a", [("", col_chunk(w_in, COL_GATE))]),
            ("gt0b", [("", col_chunk(w_in, COL_GATE + 512))]),
            ("wbp", [("", mat_rows4(w_br_pool))]),
            ("wmk", [("", col_chunk(w_mem_kv, 0))]),
            ("wmv", [("", col_chunk(w_mem_kv, 512))]),
            ("wxq", [("", col_chunk(w_in, COL_XQ))]),
            ("gt2a", [("", col_chunk(w_in, COL_GATE + 2048))]),
            ("gt2b", [("", col_chunk(w_in, COL_GATE + 2560))]),
            ("wbx", [("", mat_rows4(w_br_xa))]),
            ("wo0", [("", col_chunk(w_out, 0))]),
            ("wo1", [("", col_chunk(w_out, 512))]),
        ]
        for ei in range(NEXP):
            items.append((f"egu{ei}", [("a", exp_gu(w_exp_gate, ei, 0)), ("b", exp_gu(w_exp_up, ei, 256))]))
            items.append((f"ed{ei}", [("", exp_d(ei))]))

        def ld(dst, src, key):
            P.dma("sp", lambda e: e.dma_start(out=dst, in_=src), "ld_" + key, writes=[key])

        wgrp_f = acc_f32(53248, 512).rearrange("p (g d) -> p g d", g=4)
        ld(identf[:], c_ident, "identf")
        pre_thunks = [
            lambda: ld(posi[:, 0, :], pos_own, "posi0"),
            lambda: ld(posi[:, 1, :], pos_prev, "posi1"),
            lambda: ld(invf[:], c_invf, "invf"),
            lambda: ld(kdec[:], c_kdec, "kdec"),
            lambda: ld(maskT, c_maskT, "maskT"),
            lambda: ld(qdec, c_qdec, "qdec"),
            lambda: ld(invcnt[:], c_invcnt, "invcnt"),
            lambda: ld(pscale[:], pool_scale, "pscale"),
            lambda: ld(wr32[:], w_router.rearrange("(k p) n -> p k n", p=128), "wr32"),
            lambda: ld(br32[:], b_router[0].partition_broadcast(128), "br32"),
            lambda: ld(wgrp_f, w_pool_grp.rearrange("g c d -> c g d"), "wgrp_f"),
        ]

        W = WStream(P, ring, items, n_init=3)
        try:

            P.op("dve", lambda e: e.tensor_copy(out=identb[:], in_=identf[:]), reads=["identf"], writes=["identb"])
            wr_hif = acc_f32(55296, 288).rearrange("p (k n) -> p k n", k=8)
            post_thunks = [
                lambda: P.op("dve", lambda e: e.memset(S, 0.0), writes=["S"]),
                lambda: P.op("dve", lambda e: e.memset(S_bf, 0.0), writes=["S_bf"]),
                lambda: P.op("dve", lambda e: e.memset(onesb[:], 1.0), writes=["onesb"]),
                lambda: P.op("dve", lambda e: e.memset(epsT[:], LN_EPS), writes=["epsT"]),
                lambda: P.op("dve", lambda e: e.tensor_copy(out=wr_hi[:], in_=wr32[:]), reads=["wr32"], writes=["wr_hi"]),
                lambda: P.op("dve", lambda e: e.tensor_copy(out=wr_hif, in_=wr_hi[:]), reads=["wr_hi"], writes=["wr_hif"]),
                lambda: P.op("dve", lambda e: e.tensor_tensor(out=wr_lo[:], in0=wr32[:], in1=wr_hif, op=ALU.subtract), reads=["wr32", "wr_hif"], writes=["wr_lo"]),
                lambda: P.op("dve", lambda e: e.tensor_copy(out=wgrp_b[:], in_=wgrp_f), reads=["wgrp_f"], writes=["wgrp_b"]),
            ]

            posf_thunk = lambda: P.op("dve", lambda e: e.tensor_copy(out=posf[:], in_=posi[:]), reads=["posi0", "posi1"], writes=["posf"])
            tab_own = m_f32(0, NT * 192).rearrange("p (t c) -> p t c", t=NT)
            tab_prev = acc_f32(SC0, NT * 192).rearrange("p (t c) -> p t c", t=NT)
            tmpA = acc_f32(SC0 + 12288, NT * 128).rearrange("p (t c) -> p t c", t=NT)
            tmpI = m_f32(12288, NT * 128).bitcast(I32).rearrange("p (t c) -> p t c", t=NT)
            tab_thunks = []
            for which, tab, key in ((1, tab_prev, "tab_prev"), (0, tab_own, "tab_own")):
                pz = posf[:, which, :]
                tab_thunks.append(lambda pz=pz: P.op("dve", lambda e: e.tensor_tensor(
                    out=tmpA[:, :, 0:64], in0=pz.unsqueeze(2).broadcast_to([128, NT, 64]),
                    in1=invf[:].unsqueeze(1).broadcast_to([128, NT, 64]), op=ALU.mult),
                    reads=["posf", "invf"], writes=["tmpA"]))
                tab_thunks.append(lambda: P.op("dve", lambda e: e.tensor_scalar(out=tmpA[:, :, 64:128], in0=tmpA[:, :, 0:64], scalar1=0.25, scalar2=None, op0=ALU.add),
                                               reads=["tmpA"], writes=["tmpA"]))
                tab_thunks.append(lambda: P.op("dve", lambda e: e.tensor_copy(out=tmpI, in_=tmpA), reads=["tmpA"], writes=["tmpI"]))
                tab_thunks.append(lambda: P.op("dve", lambda e: e.tensor_tensor(out=tmpA, in0=tmpA, in1=tmpI, op=ALU.subtract), reads=["tmpA", "tmpI"], writes=["tmpA"]))
                tab_thunks.append(lambda: P.op("dve", lambda e: e.scalar_tensor_tensor(out=tmpA, in0=tmpA, scalar=0.5, in1=tmpA, op0=ALU.is_gt, op1=ALU.subtract),
                                               reads=["tmpA"], writes=["tmpA"]))
                tab_thunks.append(lambda tab=tab, key=key: P.op("act", lambda e: e.activation(out=tab[:, :, 0:128], in_=tmpA, func=AF.Sin, scale=float(-2.0 * np.pi)),
                                                               reads=["tmpA"], writes=[key]))
                tab_thunks.append(lambda tab=tab, key=key: P.op("act", lambda e: e.copy(out=tab[:, :, 128:192], in_=tab[:, :, 0:64]), reads=[key], writes=[key]))

            tab_thunks[:] = pre_thunks[0:3] + [posf_thunk] + tab_thunks + pre_thunks[3:] + post_thunks

            def drip():
                if tab_thunks:
                    tab_thunks.pop(0)()

            def load_transposed(src, dstT, ntiles, tag, xs_l, xb_l, halo=False, drip_on=False):
                nb = len(xs_l)
                for t in range(ntiles):
                    b = t % nb
                    P.dma("sp", lambda e, t=t, b=b: e.dma_start(out=xs_l[b], in_=src[t * 128:(t + 1) * 128, :]), f"ld_x{b}", writes=[f"xs{b}"])
                    P.op("act", lambda e, b=b: e.copy(out=xb_l[b], in_=xs_l[b]), reads=[f"xs{b}"], writes=[f"xb{b}"])
                    pb_ = 6 + (t % 2)
                    pt = bank_bf(pb_).rearrange("p (k t) -> p k t", k=8)
                    P.op("pe", [lambda e, k=k, b=b, pt=pt: e.transpose(out=pt[:, k, :], in_=xb_l[b][:, k * 128:(k + 1) * 128], identity=identb[:])
                                for k in range(8)], reads=[f"xb{b}", "identb"], writes=[f"B{pb_}"])
                    P.op("dve", lambda e, t=t, pt=pt: e.tensor_copy(out=dstT[:, :, t * 128:(t + 1) * 128], in_=pt),
                         reads=[f"B{pb_}"], writes=[f"{tag}{t}"])
                    if halo and t == ntiles - 1:
                        P.op("dve", lambda e, pt=pt: e.tensor_copy(out=xTh[:], in_=pt[:, :, 112:128]), reads=[f"B{pb_}"], writes=["xTh"])
                    if drip_on:
                        drip()

            xT = A[:]
            xTp = Z
            xsA = [acc_f32(57344, 1024), acc_f32(61440, 1024)]
            xbA = [m_bf16(24576, 1024), m_bf16(26624, 1024)]
            load_transposed(x_prev, xTp, NT, "xTp", xsA, xbA, halo=True, drip_on=True)
            load_transposed(x_own, xT, NT, "xT", xsA, xbA, drip_on=True)
            W.kick(after_keys=["xs0", "xs1"])
            while tab_thunks:
                drip()
            XT_ALL = [f"xT{t}" for t in range(NT)]
            P.barrier()
            _stop(1)

            GAM = [1.0 - 2.0 ** (-5.0 - h) for h in range(H)]
            GAMC = [float(g ** C) for g in GAM]
            h4 = lambda ap: ap.rearrange("p (h c) -> p h c", h=4)
            t12 = h4(m_f32(12288, 512))
            t34 = h4(m_f32(14336, 512))
            q_rot = h4(m_bf16(16384, 512))
            k_rot = h4(m_bf16(17408, 512))
            vd = h4(m_bf16(18432, 1024))
            qT = h4(m_bf16(20480, 512))
            qTd = h4(m_bf16(21504, 512))
            kT = h4(m_bf16(22528, 512))
            smk = h4(m_bf16(23552, 512))
            sg = h4(m_f32(24576, 1024))
            gnw = h4(acc_f32(SC0 + 12288, 1024))
            yn = h4(acc_f32(SC0 + 16384, 1024))
            yret = acc_bf16(SC0 + 20480, 1024)
            stt = small[:, 0:24].rearrange("p (h s) -> p h s", h=4)
            mv = small[:, 24:32].rearrange("p (h s) -> p h s", h=4)
            rstd = small[:, 32:36]
            sq = small[:, 36:40]

            P.dma("sp", lambda e: e.dma_start(out=gnw.rearrange("p h c -> p (h c)"), in_=ret_gn_w.partition_broadcast(128)), "ld_gnw", writes=["gnw"])

            def rot(ps_bank, tab, t, dst, dst_key, tab_key):
                src = h4(bank(ps_bank))
                cs1 = tab[:, t, 64:192].unsqueeze(1).broadcast_to([128, 4, 128])
                cs2 = tab[:, t, 0:128].unsqueeze(1).broadcast_to([128, 4, 128])
                P.op("dve", lambda e: e.tensor_tensor(out=t12, in0=src, in1=cs1, op=ALU.mult), reads=[f"B{ps_bank}", tab_key], writes=["t12"])
                P.op("dve", lambda e: e.tensor_tensor(out=t34, in0=src, in1=cs2, op=ALU.mult), reads=[f"B{ps_bank}", tab_key], writes=["t34"])
                P.op("dve", lambda e: e.tensor_tensor(out=dst[:, :, 0:64], in0=t12[:, :, 0:64], in1=t12[:, :, 64:128], op=ALU.subtract),
                     reads=["t12"], writes=[dst_key + "a"])
                P.op("dve", lambda e: e.tensor_tensor(out=dst[:, :, 64:128], in0=t34[:, :, 0:64], in1=t34[:, :, 64:128], op=ALU.add),
                     reads=["t34"], writes=[dst_key + "b"])

            def proj_tok(ps_bank, srcT, t, wname, src_keys):
                slot, keys = W.get(wname)
                wv = slot[:].rearrange("p (k n) -> p k n", k=8)
                P.op("pe", [lambda e, k=k: e.matmul(bank(ps_bank), lhsT=srcT[:, k, t * 128:(t + 1) * 128], rhs=wv[:, k, :],
                                                    start=(k == 0), stop=(k == 7)) for k in range(8)],
                     reads=keys + src_keys, writes=[f"B{ps_bank}"])

            def ret_chunk(srcT, src_tag, t, tab, tab_key, full):
                skeys = [f"{src_tag}{t}"]
                proj_tok(1, srcT, t, "wk", skeys)
                proj_tok(2, srcT, t, "wv0", skeys)
                proj_tok(3, srcT, t, "wv1", skeys)
                if full:
                    proj_tok(0, srcT, t, "wq", skeys)
                    proj_tok(4, srcT, t, "wg0", skeys)
                    proj_tok(5, srcT, t, "wg1", skeys)
                rot(1, tab, t, k_rot, "k_rot", tab_key)
                vps = h4(PB[1][:])
                P.op("act", [lambda e, h=h: e.activation(out=vd[:, h, :], in_=vps[:, h, :], func=AF.Identity, scale=kdec[:, h:h + 1]) for h in range(4)],
                     reads=["B2", "B3", "kdec"], writes=["vd"])
                if full:
                    rot(0, tab, t, q_rot, "q_rot", tab_key)
                    P.op("act", lambda e: e.activation(out=sg.rearrange("p h c -> p (h c)"), in_=PB[2][:], func=AF.Silu),
                         reads=["B4", "B5"], writes=["sg"])
                    if t == 0: _stop(13)
                    tp = bank_bf(6).rearrange("p (k c) -> p k c", k=8)
                    P.op("pe", [lambda e, h=h: e.transpose(out=tp[:, h, :], in_=q_rot[:, h, :], identity=identb[:]) for h in range(4)] +
                         [lambda e, h=h: e.transpose(out=tp[:, 4 + h, :], in_=k_rot[:, h, :], identity=identb[:]) for h in range(4)],
                         reads=["q_rota", "q_rotb", "k_rota", "k_rotb", "identb"], writes=["B6"])
                    if t == 0: _stop(141)
                    P.op("act", lambda e: e.copy(out=qT, in_=tp[:, 0:4, :]), reads=["B6"], writes=["qT"])
                    if t == 0: _stop(142)
                    P.op("act", [lambda e, h=h: e.activation(out=qrd[:, h, :], in_=q_rot[:, h, :], func=AF.Identity, scale=qdec[:, h:h + 1]) for h in range(4)],
                         reads=["q_rota", "q_rotb", "qdec"], writes=["qrd"])
                    tpd = bank_bf(7)[:, 0:512].rearrange("p (k c) -> p k c", k=4)
                    P.op("pe", [lambda e, h=h: e.transpose(out=tpd[:, h, :], in_=qrd[:, h, :], identity=identb[:]) for h in range(4)],
                         reads=["qrd", "identb"], writes=["B7"])
                    P.op("act", lambda e: e.copy(out=qTd, in_=tpd), reads=["B7"], writes=["qTd"])
                    if t == 0: _stop(143)
                    P.op("act", lambda e: e.copy(out=kT, in_=tp[:, 4:8, :]), reads=["B6"], writes=["kT"])
                    if t == 0: _stop(14)
                    sc = h4(bank(7))
                    P.op("pe", [lambda e, h=h: e.matmul(sc[:, h, :], lhsT=kT[:, h, :], rhs=qT[:, h, :], start=True, stop=True) for h in range(4)],
                         reads=["kT", "qT"], writes=["B7"])
                    P.op("dve", lambda e: e.tensor_tensor(out=smk, in0=sc, in1=maskT, op=ALU.mult), reads=["B7", "maskT"], writes=["smk"])
                    yps = h4(PB[0][:])
                    fl = []
                    for h in range(4):
                        fl.append(lambda e, h=h: e.matmul(yps[:, h, :], lhsT=smk[:, h, :], rhs=vd[:, h, :], start=True, stop=False))
                        fl.append(lambda e, h=h: e.matmul(yps[:, h, :], lhsT=qTd[:, h, :], rhs=S_bf[:, h, :], start=False, stop=True))
                    P.op("pe", fl, reads=["smk", "vd", "qTd", "S_bf"], writes=["B0", "B1"])
                    if t == 0: _stop(15)
                kvps = h4(PB[1][:])
                P.op("pe", [lambda e, h=h: e.matmul(kvps[:, h, :], lhsT=k_rot[:, h, :], rhs=vd[:, h, :], start=True, stop=True) for h in range(4)],
                     reads=["k_rota", "k_rotb", "vd"], writes=["B2", "B3"])
                P.op("dve", [lambda e, h=h: e.scalar_tensor_tensor(out=S[:, h, :], in0=S[:, h, :], scalar=GAMC[h], in1=kvps[:, h, :],
                                                                   op0=ALU.mult, op1=ALU.add) for h in range(4)],
                     reads=["B2", "B3", "S"], writes=["S"])
                P.op("act", lambda e: e.copy(out=S_bf, in_=S), reads=["S"], writes=["S_bf"])
                if full and t == 0: _stop(16)
                if full:
                    yps = h4(PB[0][:])
                    P.op("dve", [lambda e, h=h: e.bn_stats(out=stt[:, h, :], in_=yps[:, h, :]) for h in range(4)], reads=["B0", "B1"], writes=["stt"])
                    P.op("dve", [lambda e, h=h: e.bn_aggr(out=mv[:, h, :], in_=stt[:, h, :]) for h in range(4)], reads=["stt"], writes=["mv"])
                    P.op("act", lambda e: e.activation(out=sq, in_=mv[:, :, 1], func=AF.Sqrt, bias=epsT[:, 0:1]), reads=["mv", "epsT"], writes=["sq"])
                    P.op("dve", lambda e: e.reciprocal(out=rstd, in_=sq), reads=["sq"], writes=["rstd"])
                    P.op("dve", [lambda e, h=h: e.scalar_tensor_tensor(out=yn[:, h, :], in0=yps[:, h, :], scalar=mv[:, h, 0:1], in1=gnw[:, h, :],
                                                                       op0=ALU.subtract, op1=ALU.mult) for h in range(4)],
                         reads=["B0", "B1", "mv", "gnw"], writes=["yn"])
                    yr = h4(yret)
                    P.op("dve", [lambda e, h=h: e.scalar_tensor_tensor(out=yr[:, h, :], in0=yn[:, h, :], scalar=rstd[:, h:h + 1], in1=sg[:, h, :],
                                                                       op0=ALU.mult, op1=ALU.mult) for h in range(4)],
                         reads=["yn", "rstd", "sg"], writes=["yret"])
                    if t == 0: _stop(17)
                    tp = bank_bf(6).rearrange("p (k c) -> p k c", k=8)
                    P.op("pe", [lambda e, k=k: e.transpose(out=tp[:, k, :], in_=yret[:, k * 128:(k + 1) * 128], identity=identb[:]) for k in range(8)],
                         reads=["yret", "identb"], writes=["B6"])
                    P.op("act", lambda e: e.copy(out=Z[:, :, t * 128:(t + 1) * 128], in_=tp), reads=["B6"], writes=[f"yT{t}", f"xTp{t}"])
                    if t == 0: _stop(18)

            _stop(10)
            for t in range(NT):
                ret_chunk(xTp, "xTp", t, tab_prev, "tab_prev", False)
                if t == 0:
                    _stop(11)
            _stop(12)
            def o_e1(t):
                sk = [f"xT{t}"]
                proj_tok(1, xT, t, "wk", sk)
                proj_tok(2, xT, t, "wv0", sk)
                proj_tok(3, xT, t, "wv1", sk)
                proj_tok(0, xT, t, "wq", sk)

            def o_e2(t):
                rot(1, tab_own, t, k_rot, "k_rot", "tab_own")
                vps = h4(PB[1][:])
                P.op("act", [lambda e, h=h: e.activation(out=vd[:, h, :], in_=vps[:, h, :], func=AF.Identity, scale=kdec[:, h:h + 1]) for h in range(4)],
                     reads=["B2", "B3", "kdec"], writes=["vd"])
                rot(0, tab_own, t, q_rot, "q_rot", "tab_own")
                P.op("act", [lambda e, h=h: e.activation(out=qrd[:, h, :], in_=q_rot[:, h, :], func=AF.Identity, scale=qdec[:, h:h + 1]) for h in range(4)],
                     reads=["q_rota", "q_rotb", "qdec"], writes=["qrd"])
                tp = bank_bf(6).rearrange("p (k c) -> p k c", k=8)
                tpd = bank_bf(7)[:, 0:512].rearrange("p (k c) -> p k c", k=4)
                P.op("pe", [lambda e, h=h: e.transpose(out=tp[:, h, :], in_=q_rot[:, h, :], identity=identb[:]) for h in range(4)] +
                     [lambda e, h=h: e.transpose(out=tp[:, 4 + h, :], in_=k_rot[:, h, :], identity=identb[:]) for h in range(4)],
                     reads=["q_rota", "q_rotb", "k_rota", "k_rotb", "identb"], writes=["B6"])
                P.op("pe", [lambda e, h=h: e.transpose(out=tpd[:, h, :], in_=qrd[:, h, :], identity=identb[:]) for h in range(4)],
                     reads=["qrd", "identb"], writes=["B7"])
                P.op("act", lambda e: e.copy(out=qT, in_=tp[:, 0:4, :]), reads=["B6"], writes=["qT"])
                P.op("dve", lambda e: e.tensor_copy(out=kT, in_=tp[:, 4:8, :]), reads=["B6", "qT"], writes=["kT"])
                P.op("act", lambda e: e.copy(out=qTd, in_=tpd), reads=["B7"], writes=["qTd"])
                sc = h4(bank(7))
                P.op("pe", [lambda e, h=h: e.matmul(sc[:, h, :], lhsT=kT[:, h, :], rhs=qT[:, h, :], start=True, stop=True) for h in range(4)],
                     reads=["kT", "qT"], writes=["B7"])
                P.op("dve", lambda e: e.tensor_tensor(out=smk, in0=sc, in1=maskT, op=ALU.mult), reads=["B7", "maskT"], writes=["smk"])

            def o_g(t):
                sk = [f"xT{t}"]
                proj_tok(4, xT, t, "wg0", sk)
                proj_tok(5, xT, t, "wg1", sk)

            def o_e3(t):
                P.op("act", lambda e: e.activation(out=sg.rearrange("p h c -> p (h c)"), in_=PB[2][:], func=AF.Silu),
                     reads=["B4", "B5"], writes=["sg"])

            def o_e4(t):
                yps = h4(PB[2][:])
                fl = []
                for h in range(4):
                    fl.append(lambda e, h=h: e.matmul(yps[:, h, :], lhsT=smk[:, h, :], rhs=vd[:, h, :], start=True, stop=False))
                    fl.append(lambda e, h=h: e.matmul(yps[:, h, :], lhsT=qTd[:, h, :], rhs=S_bf[:, h, :], start=False, stop=True))
                P.op("pe", fl, reads=["smk", "vd", "qTd", "S_bf"], writes=["B4", "B5"])

            def o_e5(t):
                kvps = h4(PB[1][:])
                P.op("pe", [lambda e, h=h: e.matmul(kvps[:, h, :], lhsT=k_rot[:, h, :], rhs=vd[:, h, :], start=True, stop=True) for h in range(4)],
                     reads=["k_rota", "k_rotb", "vd"], writes=["B2", "B3"])
                P.op("dve", [lambda e, h=h: e.scalar_tensor_tensor(out=S[:, h, :], in0=S[:, h, :], scalar=GAMC[h], in1=kvps[:, h, :],
                                                                   op0=ALU.mult, op1=ALU.add) for h in range(4)],
                     reads=["B2", "B3", "S"], writes=["S"])
                P.op("act", lambda e: e.copy(out=S_bf, in_=S), reads=["S"], writes=["S_bf"])

            def o_e6a(t):
                yps = h4(PB[2][:])
                P.op("dve", [lambda e, h=h: e.bn_stats(out=stt[:, h, :], in_=yps[:, h, :]) for h in range(4)], reads=["B4", "B5"], writes=["stt"])
                P.op("dve", [lambda e, h=h: e.bn_aggr(out=mv[:, h, :], in_=stt[:, h, :]) for h in range(4)], reads=["stt"], writes=["mv"])
                P.op("act", lambda e: e.activation(out=sq, in_=mv[:, :, 1], func=AF.Sqrt, bias=epsT[:, 0:1]), reads=["mv", "epsT"], writes=["sq"])
                P.op("dve", lambda e: e.reciprocal(out=rstd, in_=sq), reads=["sq"], writes=["rstd"])
                P.op("dve", [lambda e, h=h: e.scalar_tensor_tensor(out=yn[:, h, :], in0=yps[:, h, :], scalar=mv[:, h, 0:1], in1=gnw[:, h, :],
                                                                   op0=ALU.subtract, op1=ALU.mult) for h in range(4)],
                     reads=["B4", "B5", "mv", "gnw"], writes=["yn"])
                yr = h4(yret)
                P.op("dve", [lambda e, h=h: e.scalar_tensor_tensor(out=yr[:, h, :], in0=yn[:, h, :], scalar=rstd[:, h:h + 1], in1=sg[:, h, :],
                                                                   op0=ALU.mult, op1=ALU.mult) for h in range(4)],
                     reads=["yn", "rstd", "sg"], writes=["yret"])

            def o_e6b(t):
                tp = bank_bf(6).rearrange("p (k c) -> p k c", k=8)
                P.op("pe", [lambda e, k=k: e.transpose(out=tp[:, k, :], in_=yret[:, k * 128:(k + 1) * 128], identity=identb[:]) for k in range(8)],
                     reads=["yret", "identb"], writes=["B6"])
                P.op("act", lambda e, t=t: e.copy(out=Z[:, :, t * 128:(t + 1) * 128], in_=tp), reads=["B6"], writes=[f"yT{t}", f"xTp{t}"])

            o_e1(0)
            for t in range(NT):
                o_g(t)
                o_e2(t)
                o_e3(t)
                if t == NT - 1:
                    W.release("wg0")
                    W.release("wg1")
                o_e4(t)
                o_e5(t)
                o_e6a(t)
                if t + 1 < NT:
                    o_e1(t + 1)
                    if t + 1 == NT - 1:
                        for nm in ("wk", "wv0", "wv1", "wq"):
                            W.release(nm)
                o_e6b(t)
            P.barrier()
            _stop(2)
            if debug:
                P.dma("sp", lambda e: e.dma_start(out=dbg["z"], in_=Z), "st_dbg", reads=[f"yT{t}" for t in range(NT)], is_store=True)

            gsb = [acc_f32(SC0 + i * 2048, 512) for i in range(2)]
            tmpm = [acc_bf16(SC0 + 4096 + i * 2048, 512) for i in range(2)]
            MERGE_KEYS = [f"M{j}_{g}" for j in range(8) for g in range(NG)]

            def merge_branch(yT, ykeys, nkc, gta, gtb, wlist, first, hook=None):
                ga_s, ga_k = W.get(gta)
                gb_s, gb_k = W.get(gtb)
                it = 0
                for j in range(8):
                    if j == 4:
                        W.release(gta)
                        if nkc == 8:
                            W.release(wlist[0])
                    gs_, gk_ = (ga_s, ga_k) if j < 4 else (gb_s, gb_k)
                    gv = gs_[:].rearrange("p (k n) -> p k n", k=8)
                    if nkc == 8:
                        ws_, wk_ = W.get(wlist[0] if j < 4 else wlist[1])
                        wv = ws_[:].rearrange("p (k n) -> p k n", k=8)
                        lhs = [wv[:, k, (j % 4) * 128:(j % 4) * 128 + 128] for k in range(8)]
                    else:
                        ws_, wk_ = W.get(wlist[0])
                        wv = ws_[:].rearrange("p (k n) -> p k n", k=4)
                        lhs = [wv[:, k, j * 128:(j + 1) * 128] for k in range(4)]
                    for g in range(NG):
                        b = it % 2
                        it += 1
                        cols = slice(g * 512, (g + 1) * 512)
                        P.op("pe", [lambda e, k=k, gv=gv, j=j, cols=cols, b=b: e.matmul(bank(b), lhsT=gv[:, k, (j % 4) * 128:(j % 4) * 128 + 128],
                                                                                      rhs=xT[:, k, cols], start=(k == 0), stop=(k == 7)) for k in range(8)],
                             reads=gk_ + XT_ALL[g * 4:(g + 1) * 4], writes=[f"B{b}"])
                        P.op("pe", [lambda e, k=k, lhs=lhs, cols=cols, b=b: e.matmul(bank(2 + b), lhsT=lhs[k], rhs=yT[:, k, cols],
                                                                                   start=(k == 0), stop=(k == nkc - 1)) for k in range(nkc)],
                             reads=wk_ + ykeys, writes=[f"B{2 + b}"])
                        P.op("act", lambda e, b=b: e.activation(out=gsb[b], in_=bank(b), func=AF.Sigmoid), reads=[f"B{b}"], writes=[f"gsb{b}"])
                        mk = f"M{j}_{g}"
                        if first:
                            P.op("dve", lambda e, b=b, j=j, cols=cols: e.tensor_tensor(out=M[:, j, cols], in0=bank(2 + b), in1=gsb[b], op=ALU.mult),
                                 reads=[f"B{2 + b}", f"gsb{b}"], writes=[mk])
                        else:
                            P.op("dve", lambda e, b=b: e.tensor_tensor(out=tmpm[b], in0=bank(2 + b), in1=gsb[b], op=ALU.mult),
                                 reads=[f"B{2 + b}", f"gsb{b}"], writes=[f"tmpm{b}"])
                            P.op("dve", lambda e, b=b, j=j, cols=cols: e.tensor_tensor(out=M[:, j, cols], in0=M[:, j, cols], in1=tmpm[b], op=ALU.add),
                                 reads=[f"tmpm{b}", mk], writes=[mk])
                        if hook is not None:
                            hook(it - 1)

            UW = 16 + 512
            ub = [acc_f32(40960 + i * 2176, UW) for i in range(3)]
            pooled = acc_bf16(47488, 512)
            YP = acc_bf16(48512, 4 * T).rearrange("p (k t) -> p k t", k=4)
            wp_s, wp_k = W.get("wpool")
            wpv = wp_s[:].rearrange("p (k n) -> p k n", k=8)
            WINS = (2, 4, 8, 16)

            def pool_iter(i):
                gi, g = i // NG, i % NG
                b = i % 2
                P.op("pe", [lambda e, k=k: e.matmul(bank(4 + b), lhsT=wpv[:, k, gi * 128:(gi + 1) * 128],
                                                    rhs=xT[:, k, g * 512:(g + 1) * 512], start=(k == 0), stop=(k == 7)) for k in range(8)],
                     reads=wp_k + XT_ALL[g * 4:(g + 1) * 4], writes=[f"B{4 + b}"])
                if g == 0:
                    hb = 7 - b
                    P.op("pe", [lambda e, k=k: e.matmul(bank(hb)[:, 0:16], lhsT=wpv[:, k, gi * 128:(gi + 1) * 128], rhs=xTh[:, k, :],
                                                        start=(k == 0), stop=(k == 7)) for k in range(8)],
                         reads=wp_k + ["xTh"], writes=[f"B{hb}"])
                    P.op("act", lambda e: e.copy(out=ub[0][:, 0:16], in_=bank(hb)[:, 0:16]), reads=[f"B{hb}"], writes=["ub0h"])
                P.op("act", lambda e: e.copy(out=ub[0][:, 16:UW], in_=bank(4 + b)), reads=[f"B{4 + b}"], writes=["ub0m"])
                cur = 0
                for s_ in range(gi + 1):
                    sh = 1 << s_
                    nxt = 1 if cur != 1 else 2
                    P.op("dve", lambda e, cur=cur, nxt=nxt, sh=sh: e.tensor_tensor(out=ub[nxt][:, sh:UW], in0=ub[cur][:, sh:UW],
                                                                                  in1=ub[cur][:, 0:UW - sh], op=ALU.add),
                         reads=["ub0m", "ub0h", f"ubs{cur}"], writes=[f"ubs{nxt}"])
                    cur = nxt
                w = WINS[gi]
                P.op("dve", lambda e, cur=cur: e.scalar_tensor_tensor(out=pooled, in0=ub[cur][:, 16:UW], scalar=float(1.0 / w),
                                                                      in1=ub[0][:, 16:UW], op0=ALU.mult, op1=ALU.subtract),
                     reads=[f"ubs{cur}", "ub0m"], writes=["pooled"])
                if g == 0:
                    P.op("dve", lambda e, cur=cur: e.tensor_tensor(out=ub[cur][:, 16:32], in0=ub[cur][:, 16:32], in1=invcnt[:, gi, :], op=ALU.mult),
                         reads=[f"ubs{cur}", "invcnt", "pooled"], writes=[f"ubs{cur}"])
                    P.op("dve", lambda e, cur=cur: e.tensor_tensor(out=pooled[:, 0:16], in0=ub[cur][:, 16:32], in1=ub[0][:, 16:32], op=ALU.subtract),
                         reads=[f"ubs{cur}", "ub0m"], writes=["pooled"])
                if g < NG - 1:
                    P.op("act", lambda e: e.copy(out=ub[0][:, 0:16], in_=ub[0][:, UW - 16:UW]), reads=["ub0m"], writes=["ub0h"])

            def pool_iter_b(i):
                gi, g = i // NG, i % NG
                b = i % 2
                P.op("pe", lambda e: e.matmul(bank(6 + b), lhsT=wgrp_b[:, gi, :], rhs=pooled, start=True, stop=True),
                     reads=["wgrp_b", "pooled"], writes=[f"B{6 + b}"])
                P.op("act", lambda e: e.activation(out=YP[:, gi, g * 512:(g + 1) * 512], in_=bank(6 + b), func=AF.Identity,
                                                   scale=pscale[:, gi:gi + 1]),
                     reads=[f"B{6 + b}", "pscale"], writes=[f"yp{gi}_{g}"])

            def pool_hook(it):
                if it % 2 == 1:
                    pool_iter(it // 2)
                elif it >= 2:
                    pool_iter_b(it // 2 - 1)

            YT_KEYS = [f"yT{t}" for t in range(NT)]
            merge_branch(Z, YT_KEYS, 8, "gt1a", "gt1b", ["wbr0", "wbr1"], True, hook=pool_hook)
            pool_iter_b(15)
            for nm in ("gt1a", "gt1b", "wbr0", "wbr1"):
                W.release(nm)
            P.barrier()
            _stop(3)

            W.release("wpool")
            YP_KEYS = [f"yp{gi}_{g}" for gi in range(4) for g in range(NG)]
            merge_branch(YP, YP_KEYS, 4, "gt0a", "gt0b", ["wbp"], False)
            for nm in ("gt0a", "gt0b", "wbp"):
                W.release(nm)
            P.barrier()
            _stop(4)

            memT = acc_bf16(40960, 8 * 256).rearrange("p (k m) -> p k m", k=8)
            KmT = acc_bf16(45056, 4 * 256).rearrange("p (h m) -> p h m", h=4)
            Vm = acc_bf16(47104, 2 * 512).rearrange("p (c n) -> p c n", c=2)
            xqT = acc_bf16(49152, 512)
            expT = acc_bf16(50176, 1024).rearrange("p (c n) -> p c n", c=2)
            lnS = acc_f32(52224, 512)
            rS = acc_f32(54272, 512)
            xsX = [acc_f32(57344, 1024)]
            xbX = [acc_bf16(61440, 1024)]
            load_transposed(mem, memT, 2, "memT", xsX, xbX)
            wmk_s, wmk_k = W.get("wmk")
            wmv_s, wmv_k = W.get("wmv")
            wmkv = wmk_s[:].rearrange("p (k n) -> p k n", k=8)
            wmvv = wmv_s[:].rearrange("p (k n) -> p k n", k=8)
            for h in range(4):
                b = h % 2
                P.op("pe", [lambda e, k=k, h=h, b=b: e.matmul(bank(4 + b)[:, 0:256], lhsT=wmkv[:, k, h * 128:(h + 1) * 128], rhs=memT[:, k, :],
                                                            start=(k == 0), stop=(k == 7)) for k in range(8)],
                     reads=wmk_k + ["memT0", "memT1"], writes=[f"B{4 + b}"])
                P.op("act", lambda e, h=h, b=b: e.copy(out=KmT[:, h, :], in_=bank(4 + b)[:, 0:256]), reads=[f"B{4 + b}"], writes=[f"KmT{h}"])
            for c in range(2):
                b = c % 2
                P.op("pe", [lambda e, k=k, c=c, b=b: e.matmul(bank(4 + b), lhsT=memT[:, k, c * 128:(c + 1) * 128], rhs=wmvv[:, k, :],
                                                            start=(k == 0), stop=(k == 7)) for k in range(8)],
                     reads=wmv_k + ["memT0", "memT1"], writes=[f"B{4 + b}"])
                P.op("act", lambda e, c=c, b=b: e.copy(out=Vm[:, c, :], in_=bank(4 + b)), reads=[f"B{4 + b}"], writes=[f"Vm{c}"])
            W.release("wmk")
            W.release("wmv")
            wxq_s, wxq_k = W.get("wxq")
            wxqv = wxq_s[:].rearrange("p (k n) -> p k n", k=8)
            YX = Z
            XSC = float(128.0 ** -0.5)
            expT2 = [expT, acc_bf16(63488, 1024).rearrange("p (c n) -> p c n", c=2)]

            def xa_a(i):
                h, g = i // NG, i % NG
                p = i % 2
                cols = slice(g * 512, (g + 1) * 512)
                P.op("pe", [lambda e, k=k: e.matmul(bank(4), lhsT=wxqv[:, k, h * 128:(h + 1) * 128], rhs=xT[:, k, cols],
                                                    start=(k == 0), stop=(k == 7)) for k in range(8)],
                     reads=wxq_k + XT_ALL[g * 4:(g + 1) * 4], writes=["B4"])
                P.op("act", lambda e: e.activation(out=xqT, in_=bank(4), func=AF.Copy, scale=XSC), reads=["B4"], writes=["xqT"])
                for c in range(2):
                    P.op("pe", lambda e, c=c: e.matmul(bank(5 + c), lhsT=KmT[:, h, c * 128:(c + 1) * 128], rhs=xqT, start=True, stop=True),
                         reads=[f"KmT{h}", "xqT"], writes=[f"B{5 + c}"])
                    P.op("act", lambda e, c=c: e.activation(out=expT2[p][:, c, :], in_=bank(5 + c), func=AF.Exp), reads=[f"B{5 + c}"],
                         writes=[f"expT{p}{c}"])

            def xa_b(i):
                h, g = i // NG, i % NG
                p = i % 2
                cols = slice(g * 512, (g + 1) * 512)
                ek = [f"expT{p}0", f"expT{p}1"]
                P.op("pe", [lambda e, c=c: e.matmul(bank(7), lhsT=Vm[:, c, h * 128:(h + 1) * 128], rhs=expT2[p][:, c, :], start=(c == 0), stop=(c == 1))
                            for c in range(2)], reads=["Vm0", "Vm1"] + ek, writes=["B7"])
                P.op("pe", [lambda e, c=c: e.matmul(bank(0), lhsT=onesb[:], rhs=expT2[p][:, c, :], start=(c == 0), stop=(c == 1)) for c in range(2)],
                     reads=["onesb"] + ek, writes=["B0"])
                P.op("act", lambda e: e.activation(out=lnS, in_=bank(0), func=AF.Ln), reads=["B0"], writes=["lnS"])
                P.op("act", lambda e: e.activation(out=rS, in_=lnS, func=AF.Exp, scale=-1.0), reads=["lnS"], writes=["rS"])
                P.op("dve", lambda e: e.tensor_tensor(out=YX[:, h, cols], in0=bank(7), in1=rS, op=ALU.mult),
                     reads=["B7", "rS"], writes=[f"yx{h}_{g}"])

            xa_a(0)
            for i in range(16):
                if i + 1 < 16:
                    xa_a(i + 1)
                xa_b(i)
            W.release("wxq")
            YX_KEYS = [f"yx{h}_{g}" for h in range(4) for g in range(NG)]
            merge_branch(YX, YX_KEYS, 4, "gt2a", "gt2b", ["wbx"], False)
            for nm in ("gt2a", "gt2b", "wbx"):
                W.release(nm)
            P.barrier()
            _stop(5)
            if debug:
                P.dma("sp", lambda e: e.dma_start(out=dbg["mT"], in_=M[:]), "st_dbg", reads=MERGE_KEYS, is_store=True)

            ld(lnw[:], ln1_w.partition_broadcast(128), "lnw")
            ld(lnb[:], ln1_b.partition_broadcast(128), "lnb")
            wo0_s, wo0_k = W.get("wo0")
            wo1_s, wo1_k = W.get("wo1")
            wov = [wo0_s[:].rearrange("p (k n) -> p k n", k=8), wo1_s[:].rearrange("p (k n) -> p k n", k=8)]
            x1T = A[:]
            st6 = small[:, 40:52].rearrange("p (c s) -> p c s", c=2)
            mv1 = small[:, 52:54]
            r1 = small[:, 54:55]
            nmr = small[:, 55:56]
            s1 = small[:, 56:57]
            L = small[:, 64:100]
            gneg = small[:, 100:101]
            gsum = small[:, 101:102]
            pgrp = small[:, 102:103]
            oh = small[:, 104:108]
            ge = small[:, 108:112]
            sel = small[:, 112:120]
            sel2 = small[:, 120:128]
            m1 = small[:, 128:129]
            m2 = small[:, 129:130]
            dd = small[:, 130:131]
            e2 = small[:, 131:132]
            w1p = small[:, 132:133]
            w2p = small[:, 133:134]
            mk1 = small[:, 136:144]
            mk2 = small[:, 144:152]
            wg8 = small[:, 152:160]

            def layer_norm(src, dst, tagr, tagw):
                P.op("dve", [lambda e, c=c: e.bn_stats(out=st6[:, c, :], in_=src[:, c * 512:(c + 1) * 512]) for c in range(2)], reads=tagr, writes=["st6"])
                P.op("dve", lambda e: e.bn_aggr(out=mv1, in_=st6.rearrange("p c s -> p (c s)")), reads=["st6"], writes=["mv1"])
                P.op("act", lambda e: e.activation(out=s1, in_=mv1[:, 1:2], func=AF.Sqrt, bias=epsT[:, 0:1]), reads=["mv1", "epsT"], writes=["s1"])
                P.op("dve", lambda e: e.reciprocal(out=r1, in_=s1), reads=["s1"], writes=["r1"])
                P.op("dve", lambda e: e.scalar_tensor_tensor(out=nmr, in0=mv1[:, 0:1], scalar=-1.0, in1=r1, op0=ALU.mult, op1=ALU.mult),
                     reads=["mv1", "r1"], writes=["nmr"])
                P.op("act", lambda e: e.activation(out=dst, in_=src, func=AF.Identity, scale=r1, bias=nmr), reads=tagr + ["r1", "nmr"], writes=tagw)
                P.op("dve", lambda e: e.tensor_tensor(out=dst, in0=dst, in1=lnw[:], op=ALU.mult), reads=tagw + ["lnw"], writes=tagw)
                P.op("dve", lambda e: e.tensor_tensor(out=dst, in0=dst, in1=lnb[:], op=ALU.add), reads=tagw + ["lnb"], writes=tagw)

            Lg = small[:, 160:224].rearrange("p (t g) -> p t g", t=NT)

            def attn_mm(t):
                bi = t % 2
                fl = []
                for hf in range(2):
                    for k in range(8):
                        fl.append(lambda e, hf=hf, k=k: e.matmul(PB[bi][:, hf * 512:(hf + 1) * 512], lhsT=M[:, k, t * 128:(t + 1) * 128],
                                                                rhs=wov[hf][:, k, :], start=(k == 0), stop=(k == 7)))
                P.op("pe", fl, reads=wo0_k + wo1_k + [f"M{j}_{t // 4}" for j in range(8)], writes=[f"B{2 * bi}", f"B{2 * bi + 1}"])

            def front_ops(t):
                bi = t % 2
                buf = xo[bi]
                bk = f"xo{bi}"
                o = 40 + bi * 20
                st6p = small[:, o:o + 12].rearrange("p (c s) -> p c s", c=2)
                mv1p = small[:, o + 12:o + 14]
                r1p = small[:, o + 14:o + 15]
                nmrp = small[:, o + 15:o + 16]
                s1p = small[:, o + 16:o + 17]
                k = f"ln{bi}"
                return [
                    lambda: P.dma("sp", lambda e: e.dma_start(out=buf, in_=x_own[t * 128:(t + 1) * 128, :]), f"ld_xo{bi}", writes=[bk]),
                    lambda: P.op("dve", lambda e: e.scalar_tensor_tensor(out=buf, in0=buf, scalar=ALPHA, in1=PB[bi][:], op0=ALU.mult, op1=ALU.add),
                                 reads=[bk, f"B{2 * bi}", f"B{2 * bi + 1}"], writes=[bk]),
                    lambda: P.op("dve", [lambda e, c=c: e.bn_stats(out=st6p[:, c, :], in_=buf[:, c * 512:(c + 1) * 512]) for c in range(2)],
                                 reads=[bk], writes=[k + "st"]),
                    lambda: P.op("dve", lambda e: e.bn_aggr(out=mv1p, in_=st6p.rearrange("p c s -> p (c s)")), reads=[k + "st"], writes=[k + "mv"]),
                    lambda: P.op("act", lambda e: e.activation(out=s1p, in_=mv1p[:, 1:2], func=AF.Sqrt, bias=epsT[:, 0:1]), reads=[k + "mv", "epsT"], writes=[k + "s1"]),
                    lambda: P.op("dve", lambda e: e.reciprocal(out=r1p, in_=s1p), reads=[k + "s1"], writes=[k + "r1"]),
                    lambda: P.op("dve", lambda e: e.scalar_tensor_tensor(out=nmrp, in0=mv1p[:, 0:1], scalar=-1.0, in1=r1p, op0=ALU.mult, op1=ALU.mult),
                                 reads=[k + "mv", k + "r1"], writes=[k + "nmr"]),
                    lambda: P.op("act", lambda e: e.activation(out=buf, in_=buf, func=AF.Identity, scale=r1p, bias=nmrp), reads=[bk, k + "r1", k + "nmr"], writes=[bk]),
                    lambda: P.op("dve", lambda e: e.tensor_tensor(out=buf, in0=buf, in1=lnw[:], op=ALU.mult), reads=[bk, "lnw"], writes=[bk]),
                    lambda: P.op("dve", lambda e: e.tensor_tensor(out=buf, in0=buf, in1=lnb[:], op=ALU.add), reads=[bk, "lnb"], writes=[bk]),
                ]

            def back_ops(t):
                bi = t % 2
                buf = xo[bi]
                bk = f"xo{bi}"
                bh, bl = 4 + 2 * bi, 5 + 2 * bi
                tph = bank_bf(bh).rearrange("p (k c) -> p k c", k=8)
                tpl = bank_bf(bl).rearrange("p (k c) -> p k c", k=8)
                lg = bank(bh)[:, 0:36]

                def router():
                    fl = []
                    for k in range(8):
                        fl.append(lambda e, k=k: e.matmul(lg, lhsT=x1T[:, k, t * 128:(t + 1) * 128], rhs=wr_hi[:, k, :], start=(k == 0), stop=False))
                        fl.append(lambda e, k=k: e.matmul(lg, lhsT=x1Tlo[:, k, :], rhs=wr_hi[:, k, :], start=False, stop=False))
                        fl.append(lambda e, k=k: e.matmul(lg, lhsT=x1T[:, k, t * 128:(t + 1) * 128], rhs=wr_lo[:, k, :], start=False, stop=(k == 7)))
                    P.op("pe", fl, reads=[f"x1T{t}", "x1Tlo", "wr_hi", "wr_lo"], writes=[f"B{bh}"])
                return [
                    lambda: P.op("act", lambda e: e.copy(out=hib, in_=buf), reads=[bk], writes=["hib"]),
                    lambda: P.op("act", lambda e: e.copy(out=ACC[:, t, :], in_=hib), reads=["hib"], writes=[f"acc{t}"]),
                    lambda: P.op("dve", lambda e: e.tensor_tensor(out=lob, in0=buf, in1=ACC[:, t, :], op=ALU.subtract), reads=[bk, f"acc{t}"], writes=["lob"]),
                    lambda: P.op("act", lambda e: e.activation(out=ACC[:, t, :], in_=buf, func=AF.Copy, scale=ALPHA), reads=[bk], writes=[f"acc{t}"]),
                    lambda: P.op("pe", [lambda e, k=k: e.transpose(out=tph[:, k, :], in_=hib[:, k * 128:(k + 1) * 128], identity=identb[:]) for k in range(8)],
                                 reads=["hib", "identb"], writes=[f"B{bh}"]),
                    lambda: P.op("pe", [lambda e, k=k: e.transpose(out=tpl[:, k, :], in_=lob[:, k * 128:(k + 1) * 128], identity=identb[:]) for k in range(8)],
                                 reads=["lob", "identb"], writes=[f"B{bl}"]),
                    lambda: P.op("act", lambda e: e.copy(out=x1T[:, :, t * 128:(t + 1) * 128], in_=tph), reads=[f"B{bh}"], writes=[f"x1T{t}"]),
                    lambda: P.op("dve", lambda e: e.tensor_copy(out=x1Tlo[:], in_=tpl), reads=[f"B{bl}"], writes=["x1Tlo"]),
                    router,
                    lambda: P.op("dve", lambda e: e.tensor_tensor(out=Lg[:, t, :], in0=lg[:, 0:4], in1=br32[:, 0:4], op=ALU.add), reads=[f"B{bh}", "br32"], writes=[f"Lg{t}"]),
                    lambda: P.op("dve", lambda e: e.tensor_tensor(out=gates[:, t, :], in0=lg[:, 4:36], in1=br32[:, 4:36], op=ALU.add), reads=[f"B{bh}", "br32"], writes=[f"gate{t}"]),
                ]

            attn_mm(0)
            attn_mm(1)
            for f in front_ops(0):
                f()
            FO = [0, 1, 2, 3, 4, 5, 6, 7, 8, 9]
            ORDER = [("f", 0), ("f", 1), ("b", 0), ("f", 2), ("b", 1), ("f", 3), ("f", 4), ("b", 2), ("f", 5), ("f", 6), ("b", 3),
                     ("f", 7), ("b", 4), ("b", 5), ("b", 6), ("f", 8), ("b", 7), ("b", 8), ("f", 9), ("b", 9), ("b", 10)]
            for t in range(NT):
                if t + 2 < NT:
                    attn_mm(t + 2)
                fl_ = front_ops(t + 1) if t + 1 < NT else None
                bl_ = back_ops(t)
                for kind, idx in ORDER:
                    if kind == "f":
                        if fl_ is not None:
                            fl_[idx]()
                    else:
                        bl_[idx]()
            P.barrier()
            GK = [f"gate{t}" for t in range(NT)]
            LK = [f"Lg{t}" for t in range(NT)]
            un = lambda o, n: UN[:, o:o + n]
            gmax = un(0, 16)
            gsum = un(16, 16)
            pgrp = un(32, 16)
            m1 = un(48, 16)
            m2 = un(64, 16)
            e2 = un(80, 16)
            w1 = un(96, 16)
            w2 = un(112, 16)
            Lgs = un(128, 64).rearrange("p (t g) -> p t g", t=NT)
            oh = un(192, 64).rearrange("p (t g) -> p t g", t=NT)
            ge = un(256, 64).rearrange("p (t g) -> p t g", t=NT)
            sel = un(320, 128).rearrange("p (t e) -> p t e", t=NT)
            sel2 = un(448, 128).rearrange("p (t e) -> p t e", t=NT)
            mk1 = un(576, 128).rearrange("p (t e) -> p t e", t=NT)
            mk2 = un(704, 128).rearrange("p (t e) -> p t e", t=NT)
            wg8 = un(832, 128).rearrange("p (t e) -> p t e", t=NT)
            E4 = gates[:].rearrange("p t (g e) -> p t g e", g=4)
            E4T = gates[:].rearrange("p t (g e) -> p t e g", g=4)
            bc3 = lambda a, n: a.unsqueeze(2).broadcast_to([128, NT, n])
            P.op("dve", lambda e: e.tensor_reduce(out=gmax, in_=Lg, axis=AX.X, op=ALU.max), reads=LK, writes=["gmax"])
            P.op("dve", lambda e: e.tensor_tensor(out=Lgs, in0=Lg, in1=bc3(gmax, 4), op=ALU.subtract), reads=LK + ["gmax"], writes=["Lgs"])
            P.op("dve", lambda e: e.tensor_scalar(out=oh, in0=Lgs, scalar1=0.0, scalar2=None, op0=ALU.is_equal), reads=["Lgs"], writes=["oh"])
            P.op("act", lambda e: e.activation(out=ge, in_=Lgs, func=AF.Exp), reads=["Lgs"], writes=["ge"])
            P.op("dve", lambda e: e.tensor_reduce(out=gsum, in_=ge, axis=AX.X, op=ALU.add), reads=["ge"], writes=["gsum"])
            P.op("dve", lambda e: e.reciprocal(out=pgrp, in_=gsum), reads=["gsum"], writes=["pgrp"])
            P.op("dve", lambda e: e.tensor_tensor(out=E4, in0=E4, in1=oh.unsqueeze(3).broadcast_to([128, NT, 4, 8]), op=ALU.mult), reads=GK + ["oh"], writes=GK)
            P.op("dve", lambda e: e.tensor_reduce(out=sel, in_=E4T, axis=AX.X, op=ALU.add), reads=GK, writes=["sel"])
            P.op("dve", lambda e: e.tensor_reduce(out=m1, in_=sel, axis=AX.X, op=ALU.max), reads=["sel"], writes=["m1"])
            P.op("dve", lambda e: e.tensor_tensor(out=mk1, in0=sel, in1=bc3(m1, 8), op=ALU.is_equal), reads=["sel", "m1"], writes=["mk1"])
            P.op("dve", lambda e: e.scalar_tensor_tensor(out=sel2, in0=mk1, scalar=-1.0e30, in1=sel, op0=ALU.mult, op1=ALU.add), reads=["mk1", "sel"], writes=["sel2"])
            P.op("dve", lambda e: e.tensor_reduce(out=m2, in_=sel2, axis=AX.X, op=ALU.max), reads=["sel2"], writes=["m2"])
            P.op("dve", lambda e: e.tensor_tensor(out=mk2, in0=sel2, in1=bc3(m2, 8), op=ALU.is_equal), reads=["sel2", "m2"], writes=["mk2"])
            P.op("dve", lambda e: e.tensor_tensor(out=e2, in0=m2, in1=m1, op=ALU.subtract), reads=["m1", "m2"], writes=["e2"])
            P.op("act", lambda e: e.activation(out=e2, in_=e2, func=AF.Exp), reads=["e2"], writes=["e2"])
            P.op("dve", lambda e: e.tensor_scalar(out=w2, in0=e2, scalar1=1.0, scalar2=None, op0=ALU.add), reads=["e2"], writes=["w2"])
            P.op("dve", lambda e: e.reciprocal(out=w1, in_=w2), reads=["w2"], writes=["w1"])
            P.op("dve", lambda e: e.tensor_tensor(out=w1, in0=w1, in1=pgrp, op=ALU.mult), reads=["w1", "pgrp"], writes=["w1"])
            P.op("dve", lambda e: e.tensor_tensor(out=w2, in0=e2, in1=w1, op=ALU.mult), reads=["e2", "w1", "w2"], writes=["w2"])
            P.op("dve", lambda e: e.tensor_tensor(out=mk1, in0=mk1, in1=bc3(w1, 8), op=ALU.mult), reads=["mk1", "w1"], writes=["mk1"])
            P.op("dve", lambda e: e.tensor_tensor(out=mk2, in0=mk2, in1=bc3(w2, 8), op=ALU.mult), reads=["mk2", "w2"], writes=["mk2"])
            P.op("dve", lambda e: e.tensor_tensor(out=wg8, in0=mk1, in1=mk2, op=ALU.add), reads=["mk1", "mk2"], writes=["wg8"])
            P.op("dve", lambda e: e.tensor_tensor(out=E4, in0=wg8.unsqueeze(2).broadcast_to([128, NT, 4, 8]),
                                                  in1=oh.unsqueeze(3).broadcast_to([128, NT, 4, 8]), op=ALU.mult), reads=["wg8", "oh"], writes=GK)
            W.release("wo0")
            W.release("wo1")
            P.barrier()
            _stop(6)
            if debug:
                P.dma("sp", lambda e: e.dma_start(out=dbg["acc"], in_=ACC[:]), "st_dbg", reads=[f"acc{t}" for t in range(NT)], is_store=True)
                P.dma("sp", lambda e: e.dma_start(out=dbg["gates"], in_=gates[:]), "st_dbg", reads=[f"gate{t}" for t in range(NT)], is_store=True)
                P.barrier()

            sa = [m_f32(i * 4096, 1024).rearrange("p (c n) -> p c n", c=2) for i in range(2)]
            hT = [m_bf16(16384 + i * 2048, 1024).rearrange("p (c n) -> p c n", c=2) for i in range(2)]
            X1T_ALL = [f"x1T{t}" for t in range(NT)]
            ld(lnw[:], ln2_w.partition_broadcast(128), "lnw")
            ld(lnb[:], ln2_b.partition_broadcast(128), "lnb")
            n_exp = NEXP if not MOE_LIMIT else MOE_LIMIT
            slots_of = {}

            def moe_ug(ei, g, sb_):
                gu_s, gu_k = W.get(f"egu{ei}")
                guv = gu_s[:].rearrange("p (k n) -> p k n", k=8)
                cols = slice(g * 512, (g + 1) * 512)
                for fc in range(2):
                    P.op("pe", [lambda e, k=k, fc=fc, cols=cols, guv=guv: e.matmul(bank(fc), lhsT=guv[:, k, fc * 128:(fc + 1) * 128], rhs=x1T[:, k, cols],
                                                                                   start=(k == 0), stop=(k == 7)) for k in range(8)],
                         reads=gu_k + X1T_ALL[g * 4:(g + 1) * 4], writes=[f"B{fc}"])
                    P.op("pe", [lambda e, k=k, fc=fc, cols=cols, guv=guv: e.matmul(bank(2 + fc), lhsT=guv[:, k, 256 + fc * 128:256 + (fc + 1) * 128], rhs=x1T[:, k, cols],
                                                                                   start=(k == 0), stop=(k == 7)) for k in range(8)],
                         reads=gu_k + X1T_ALL[g * 4:(g + 1) * 4], writes=[f"B{2 + fc}"])
                    P.op("act", lambda e, fc=fc, sb_=sb_: e.activation(out=sa[sb_][:, fc, :], in_=bank(fc), func=AF.Silu), reads=[f"B{fc}"], writes=[f"sa{sb_}{fc}"])
                    P.op("dve", lambda e, fc=fc, sb_=sb_: e.tensor_tensor(out=hT[sb_][:, fc, :], in0=bank(2 + fc), in1=sa[sb_][:, fc, :], op=ALU.mult),
                         reads=[f"B{2 + fc}", f"sa{sb_}{fc}"], writes=[f"hT{sb_}{fc}"])
                if g == NG - 1:
                    W.release(f"egu{ei}")

            def moe_d(ei, g, sb_):
                d_s, d_k = W.get(f"ed{ei}")
                dv = d_s[:, 0:2048].rearrange("p (k n) -> p k n", k=2)
                for tt in range(4):
                    t = g * 4 + tt
                    pb = 2 + (tt % 2)
                    fl = []
                    for hf in range(2):
                        for fc in range(2):
                            fl.append(lambda e, hf=hf, fc=fc, tt=tt, pb=pb, sb_=sb_, dv=dv: e.matmul(PB[pb][:, hf * 512:(hf + 1) * 512], lhsT=hT[sb_][:, fc, tt * 128:(tt + 1) * 128],
                                                                                                    rhs=dv[:, fc, hf * 512:(hf + 1) * 512], start=(fc == 0), stop=(fc == 1)))
                    P.op("pe", fl, reads=d_k + [f"hT{sb_}0", f"hT{sb_}1"], writes=[f"B{2 * pb}", f"B{2 * pb + 1}"])
                    P.op("dve", lambda e, t=t, pb=pb, ei=ei: e.scalar_tensor_tensor(out=ACC[:, t, :], in0=PB[pb][:], scalar=gates[:, t, ei:ei + 1], in1=ACC[:, t, :],
                                                                                   op0=ALU.mult, op1=ALU.add),
                         reads=[f"B{2 * pb}", f"B{2 * pb + 1}", f"gate{t}", f"acc{t}"], writes=[f"acc{t}"])
                    if ei == n_exp - 1:
                        bi = t % 2
                        o = xo[bi]
                        layer_norm(ACC[:, t, :], o, [f"acc{t}"], [f"xo{bi}"])
                        P.dma("sp", lambda e, t=t, o=o: e.dma_start(out=y_out[t * 128:(t + 1) * 128, :], in_=o), f"st_y{bi}", reads=[f"xo{bi}"], is_store=True)
                if g == NG - 1:
                    W.release(f"ed{ei}")

            def ug_pieces(ei, g, sb_):
                gu_s, gu_k = W.get(f"egu{ei}")
                guv = gu_s[:].rearrange("p (k n) -> p k n", k=8)
                cols = slice(g * 512, (g + 1) * 512)
                rk = gu_k + X1T_ALL[g * 4:(g + 1) * 4]
                pieces = []
                for fc in range(2):
                    def pa(fc=fc):
                        P.op("pe", [lambda e, k=k, fc=fc: e.matmul(bank(fc), lhsT=guv[:, k, fc * 128:(fc + 1) * 128], rhs=x1T[:, k, cols],
                                                                     start=(k == 0), stop=(k == 7)) for k in range(8)], reads=rk, writes=[f"B{fc}"])
                        P.op("act", lambda e, fc=fc: e.activation(out=sa[sb_][:, fc, :], in_=bank(fc), func=AF.Silu), reads=[f"B{fc}"], writes=[f"sa{sb_}{fc}"])

                    def pb_(fc=fc):
                        P.op("pe", [lambda e, k=k, fc=fc: e.matmul(bank(2 + fc), lhsT=guv[:, k, 256 + fc * 128:256 + (fc + 1) * 128], rhs=x1T[:, k, cols],
                                                                     start=(k == 0), stop=(k == 7)) for k in range(8)], reads=rk, writes=[f"B{2 + fc}"])
                        P.op("dve", lambda e, fc=fc: e.tensor_tensor(out=hT[sb_][:, fc, :], in0=bank(2 + fc), in1=sa[sb_][:, fc, :], op=ALU.mult),
                             reads=[f"B{2 + fc}", f"sa{sb_}{fc}"], writes=[f"hT{sb_}{fc}"])
                    pieces += [pa, pb_]
                return pieces

            def d_tile(ei, g, sb_, tt):
                d_s, d_k = W.get(f"ed{ei}")
                dv = d_s[:, 0:2048].rearrange("p (k n) -> p k n", k=2)
                t = g * 4 + tt
                pb = 2 + (tt % 2)
                fl = []
                for hf in range(2):
                    for fc in range(2):
                        fl.append(lambda e, hf=hf, fc=fc: e.matmul(PB[pb][:, hf * 512:(hf + 1) * 512], lhsT=hT[sb_][:, fc, tt * 128:(tt + 1) * 128],
                                                                  rhs=dv[:, fc, hf * 512:(hf + 1) * 512], start=(fc == 0), stop=(fc == 1)))
                P.op("pe", fl, reads=d_k + [f"hT{sb_}0", f"hT{sb_}1"], writes=[f"B{2 * pb}", f"B{2 * pb + 1}"])
                P.op("dve", lambda e: e.scalar_tensor_tensor(out=ACC[:, t, :], in0=PB[pb][:], scalar=gates[:, t, ei:ei + 1], in1=ACC[:, t, :],
                                                             op0=ALU.mult, op1=ALU.add),
                     reads=[f"B{2 * pb}", f"B{2 * pb + 1}", f"gate{t}", f"acc{t}"], writes=[f"acc{t}"])
                if ei == n_exp - 1:
                    bi = t % 2
                    o = xo[bi]
                    layer_norm(ACC[:, t, :], o, [f"acc{t}"], [f"xo{bi}"])
                    P.dma("sp", lambda e: e.dma_start(out=y_out[t * 128:(t + 1) * 128, :], in_=o), f"st_y{bi}", reads=[f"xo{bi}"], is_store=True)

            steps = [(ei, g) for ei in range(n_exp) for g in range(NG)]
            for i in range(len(steps) + 1):
                pieces = ug_pieces(steps[i][0], steps[i][1], i % 2) if i < len(steps) else [None] * 4
                for q in range(4):
                    if pieces[q] is not None:
                        pieces[q]()
                    if i > 0:
                        pe_, pg_ = steps[i - 1]
                        d_tile(pe_, pg_, (i - 1) % 2, q)
                if i < len(steps) and steps[i][1] == NG - 1:
                    W.release(f"egu{steps[i][0]}")
                if i > 0 and steps[i - 1][1] == NG - 1:
                    W.release(f"ed{steps[i - 1][0]}")
        except _StopBuild:
            P.dma("sp", lambda e: e.dma_start(out=y_out[0:128, 0:128], in_=identf[:]), "st_y0", reads=["identf"], is_store=True)
        P.finish()
        P.emit()
    return nc


MOE_LIMIT = 0
STOP = 0


class _StopBuild(Exception):
    pass


def _stop(n):
    if STOP == n:
        raise _StopBuild()

_NC_CACHE = {}


def _consts(hf):
    c = {}
    c["c_ident"] = np.eye(128, dtype=np.float32)
    inv_freq = (10000.0 ** (-np.arange(64, dtype=np.float32) / np.float32(64))).astype(np.float32)
    c["c_invf"] = np.tile((inv_freq.astype(np.float64) / (2 * np.pi)).astype(np.float32)[None, :], (128, 1))
    hh = np.arange(4, dtype=np.float64)
    lg = np.log1p(-np.exp2(-5.0 - hh))
    s = np.arange(128)[:, None]
    cc = np.arange(128)[None, :]
    maskT = np.zeros((128, 4, 128), np.float64)
    for h in range(4):
        maskT[:, h, :] = np.where(s <= cc, np.exp(lg[h] * (cc - 127.0)), 0.0) * np.ones((128, 1))
    c["c_maskT"] = maskT.astype(np.float32)
    qd = np.exp(lg[None, :] * (np.arange(128)[:, None] + 1.0))
    c["c_qdec"] = qd.astype(np.float32)
    kd = np.exp(lg[None, :] * (127.0 - np.arange(128)[:, None])) * (128.0 ** -0.5)
    c["c_kdec"] = kd.astype(np.float32)
    ic = np.zeros((4, 16), np.float64)
    for gi, w in enumerate((2, 4, 8, 16)):
        for t in range(16):
            ic[gi, t] = 1.0 / (min(t + 1, w) if hf == 0 else w)
    c["c_invcnt"] = np.tile(ic[None, :, :], (128, 1, 1)).astype(np.float32)
    return c


def kernel(x, mem, positions, w_in, w_pool_grp, pool_scale, ret_gn_w, w_mem_kv, w_br_pool, w_br_ret,
           w_br_xa, w_out, ln1_w, ln1_b, w_grp_router, b_grp_router, w_exp_router, b_exp_router,
           w_exp_gate, w_exp_up, w_exp_down, ln2_w, ln2_b, _debug=False):
    f = lambda a: np.ascontiguousarray(np.asarray(a, dtype=np.float32))
    x = f(x)
    mem = f(mem)
    positions = np.asarray(positions, dtype=np.int32)
    shared = {
        "w_in": f(w_in)[0], "w_pool_grp": f(w_pool_grp)[0],
        "pool_scale": np.ascontiguousarray(f(pool_scale)[0].reshape(4, 128).T),
        "ret_gn_w": f(ret_gn_w)[0].reshape(1024), "w_mem_kv": f(w_mem_kv)[0], "w_br_pool": f(w_br_pool)[0],
        "w_br_ret": f(w_br_ret)[0], "w_br_xa": f(w_br_xa)[0], "w_out": f(w_out)[0],
        "ln1_w": f(ln1_w)[0], "ln1_b": f(ln1_b)[0], "ln2_w": f(ln2_w)[0], "ln2_b": f(ln2_b)[0],
        "w_router": np.ascontiguousarray(np.concatenate([f(w_grp_router)[0], f(w_exp_router)[0]], axis=1)),
        "b_router": np.ascontiguousarray(np.concatenate([f(b_grp_router)[0], f(b_exp_router)[0]], axis=0)[None, :]),
        "w_exp_gate": f(w_exp_gate)[0].reshape(32, 1024, 256), "w_exp_up": f(w_exp_up)[0].reshape(32, 1024, 256),
        "w_exp_down": f(w_exp_down)[0].reshape(32, 256, 1024),
    }
    key = bool(_debug)
    if key not in _NC_CACHE:
        _NC_CACHE[key] = build_program(debug=_debug)
    nc = _NC_CACHE[key]
    in_maps = []
    for c in range(8):
        b, hf = c // 2, c % 2
        m = dict(shared)
        m.update(_consts(hf))
        m["x_own"] = np.ascontiguousarray(x[b, hf * T:(hf + 1) * T])
        m["x_prev"] = np.ascontiguousarray(x[b, 0:T]) if hf == 1 else np.zeros((T, D), np.float32)
        po = positions[b, hf * T:(hf + 1) * T].reshape(NT, 128).T
        pp = positions[b, 0:T].reshape(NT, 128).T if hf == 1 else np.zeros((128, NT), np.int32)
        m["pos_own"] = np.ascontiguousarray(po)
        m["pos_prev"] = np.ascontiguousarray(pp)
        m["mem"] = np.ascontiguousarray(mem[b])
        in_maps.append(m)
    res = run_bass_kernel_spmd(nc, in_maps, core_ids=list(range(8)))
    out = np.zeros((4, SEQ, D), np.float32)
    for c in range(8):
        b, hf = c // 2, c % 2
        out[b, hf * T:(hf + 1) * T] = res.results[c]["y_out"]
    if _debug:
        return out, res
    return out
```

```python
from contextlib import ExitStack
import numpy as np
import concourse.bass as bass
import concourse.mybir as mybir
from concourse.bass_utils import run_bass_kernel_spmd

F32 = mybir.dt.float32
BF16 = mybir.dt.bfloat16
I32 = mybir.dt.int32
ALU = mybir.AluOpType
AF = mybir.ActivationFunctionType
AX = mybir.AxisListType

D = 1024
SEQ = 4096
T = 2048
NT = 16
NG = 4
C = 128
H = 4
DV = 256
NEXP = 32
LN_EPS = 1e-5
ALPHA = 2.0 ** 0.25
NSLOT = 6
ENGS = ("pe", "act", "dve", "pool", "sp")


class Prog:
    def __init__(self, nc, es):
        self.nc = nc
        self.es = es
        self.streams = {e: [] for e in ENGS}
        self.sems = {}
        self.cnt = {}
        self.waited = {e: {} for e in ENGS}
        self.res = {}
        self.store_events = []
        for e in ("pe", "act", "dve", "pool"):
            self._sem(e)

    def _sem(self, name):
        if name not in self.sems:
            self.sems[name] = self.es.enter_context(self.nc.semaphore("s_" + str(name)))
            self.cnt[name] = 0
        return self.sems[name]

    def _deps(self, eng, reads, writes):
        deps = {}

        def add(ev):
            if ev is None:
                return
            s, v = ev
            if deps.get(s, 0) < v:
                deps[s] = v

        for r in reads:
            st = self.res.get(r)
            if st:
                add(st["w"])
        for w in writes:
            st = self.res.get(w)
            if st:
                add(st["w"])
                for ev in st["r"]:
                    add(ev)
        for s, v in deps.items():
            if eng == "pe" and s == "pe":
                continue
            if self.waited[eng].get(s, 0) < v:
                self.waited[eng][s] = v
                sem = self.sems[s]
                self.streams[eng].append(lambda e, sem=sem, v=v: e.wait_ge(sem, v))

    def _mark(self, ev, reads, writes):
        for r in reads:
            st = self.res.setdefault(r, {"w": None, "r": []})
            st["r"].append(ev)
            if len(st["r"]) > 64:
                best = {}
                for s, v in st["r"]:
                    best[s] = max(best.get(s, 0), v)
                st["r"] = list(best.items())
        for w in writes:
            self.res[w] = {"w": ev, "r": []}

    def op(self, eng, fns, reads=(), writes=()):
        if callable(fns):
            fns = [fns]
        self._deps(eng, reads, writes)
        self.cnt[eng] += 1
        ev = (eng, self.cnt[eng])
        sem = self.sems[eng]
        st = self.streams[eng]
        for f in fns[:-1]:
            st.append(f)
        last = fns[-1]
        st.append(lambda e, last=last, sem=sem: last(e).then_inc(sem, 1))
        self._mark(ev, reads, writes)
        return ev

    def dma(self, queue, fn, semkey, reads=(), writes=(), is_store=False):
        self._deps(queue, reads, writes)
        sem = self._sem(semkey)
        self.cnt[semkey] += 16
        ev = (semkey, self.cnt[semkey])
        self.streams[queue].append(lambda e, fn=fn, sem=sem: fn(e).then_inc(sem, 16))
        self._mark(ev, reads, writes)
        if is_store:
            self.store_events.append(ev)
        return ev

    def barrier(self, engs=("pe", "act", "dve", "sp")):
        for eng in engs:
            for sname, v in self.cnt.items():
                if v == 0 or str(sname).startswith("w"):
                    continue
                if eng == "pe" and sname == "pe":
                    continue
                if self.waited[eng].get(sname, 0) < v:
                    self.waited[eng][sname] = v
                    sem = self.sems[sname]
                    self.streams[eng].append(lambda e, sem=sem, v=v: e.wait_ge(sem, v))

    def finish(self):
        for s, v in self.cnt.items():
            if s in ("pe", "act", "dve", "pool") or v == 0:
                continue
            if self.waited["sp"].get(s, 0) < v:
                self.waited["sp"][s] = v
                sem = self.sems[s]
                self.streams["sp"].append(lambda e, sem=sem, v=v: e.wait_ge(sem, v))

    def emit(self):
        nc = self.nc
        with nc.Block() as block:
            @block.tensor
            def _(e):
                for f in self.streams["pe"]:
                    f(e)

            @block.scalar
            def _(e):
                for f in self.streams["act"]:
                    f(e)

            @block.vector
            def _(e):
                for f in self.streams["dve"]:
                    f(e)

            @block.gpsimd
            def _(e):
                for f in self.streams["pool"]:
                    f(e)

            @block.sync
            def _(e):
                for f in self.streams["sp"]:
                    f(e)


class WStream:
    def __init__(self, P, slots, items, n_init=None):
        self.P = P
        self.slots = slots
        self.items = items
        self.idx = {it[0]: i for i, it in enumerate(items)}
        self.emitted = 0
        self.rel = set()
        self.wm = 0
        for _ in range(min(n_init if n_init is not None else len(slots), len(items))):
            self._emit_next()

    def kick(self, after_keys=()):
        self.P._deps("pool", tuple(after_keys), ())
        while self.emitted < self.wm + len(self.slots) and self.emitted < len(self.items):
            self._emit_next()

    def _emit_next(self):
        i = self.emitted
        if i >= len(self.items):
            return
        P = self.P
        s = i % len(self.slots)
        slot = self.slots[s]
        allkeys = [f"ring{s}", f"ring{s}a", f"ring{s}b"]
        P._deps("pool", (), allkeys)
        for k in allkeys:
            P.res[k] = {"w": None, "r": []}
        for sub, fn in self.items[i][1]:
            semkey = f"w{s}{sub}"
            sem = P._sem(semkey)
            P.cnt[semkey] += 16
            ev = (semkey, P.cnt[semkey])
            P.streams["pool"].append(lambda e, fn=fn, slot=slot, sem=sem: fn(e, slot).then_inc(sem, 16))
            P.res[f"ring{s}{sub}"] = {"w": ev, "r": []}
        self.emitted += 1

    def get(self, name):
        i = self.idx[name]
        assert i < self.emitted, f"weight item {name} not yet emitted"
        s = i % len(self.slots)
        keys = [f"ring{s}{sub}" for sub, _ in self.items[i][1]]
        return self.slots[s], keys

    def release(self, name):
        i = self.idx[name]
        self.rel.add(i)
        while self.wm in self.rel:
            self.wm += 1
        while self.emitted < self.wm + len(self.slots) and self.emitted < len(self.items):
            self._emit_next()


def build_program(debug=False):
    nc = bass.Bass("TRN2", target_bir_lowering=False)

    def din(name, shape, dt=F32):
        return nc.dram_tensor(name, list(shape), dt, kind="ExternalInput").ap()

    x_own = din("x_own", [T, D])
    x_prev = din("x_prev", [T, D])
    pos_own = din("pos_own", [128, NT], I32)
    pos_prev = din("pos_prev", [128, NT], I32)
    mem = din("mem", [256, D])
    w_in = din("w_in", [D, 7168])
    w_pool_grp = din("w_pool_grp", [4, 128, 128])
    pool_scale = din("pool_scale", [128, 4])
    ret_gn_w = din("ret_gn_w", [1024])
    w_mem_kv = din("w_mem_kv", [D, 1024])
    w_br_pool = din("w_br_pool", [512, D])
    w_br_ret = din("w_br_ret", [1024, D])
    w_br_xa = din("w_br_xa", [512, D])
    w_out = din("w_out", [D, D])
    ln1_w = din("ln1_w", [D])
    ln1_b = din("ln1_b", [D])
    ln2_w = din("ln2_w", [D])
    ln2_b = din("ln2_b", [D])
    w_router = din("w_router", [D, 36])
    b_router = din("b_router", [1, 36])
    w_exp_gate = din("w_exp_gate", [NEXP, D, 256])
    w_exp_up = din("w_exp_up", [NEXP, D, 256])
    w_exp_down = din("w_exp_down", [NEXP, 256, D])
    c_ident = din("c_ident", [128, 128])
    c_invf = din("c_invf", [128, 64])
    c_maskT = din("c_maskT", [128, 4, 128])
    c_qdec = din("c_qdec", [128, 4])
    c_kdec = din("c_kdec", [128, 4])
    c_invcnt = din("c_invcnt", [128, 4, 16])
    y_out = nc.dram_tensor("y_out", [T, D], F32, kind="ExternalOutput").ap()
    dbg = {}
    if debug:
        dbg["mT"] = nc.dram_tensor("dbg_mT", [128, 8, T], BF16, kind="ExternalOutput").ap()
        dbg["acc"] = nc.dram_tensor("dbg_acc", [128, NT, D], F32, kind="ExternalOutput").ap()
        dbg["gates"] = nc.dram_tensor("dbg_gates", [128, NT, 32], F32, kind="ExternalOutput").ap()
        dbg["z"] = nc.dram_tensor("dbg_z", [128, 8, T], BF16, kind="ExternalOutput").ap()

    with ExitStack() as es:
        P = Prog(nc, es)

        def sb(name, shape, dt):
            return es.enter_context(nc.sbuf_tensor(name, list(shape), dt))

        A = sb("A", [128, 8, T], BF16)
        M = sb("M", [128, 8, T], BF16)
        ACC = sb("ACC", [128, NT, D], F32)
        ring = [sb(f"ring{i}", [128, 4096], BF16) for i in range(NSLOT)]
        UN = sb("UN", [128, 3072], F32)
        lnw = sb("lnw", [128, D], F32)
        lnb = sb("lnb", [128, D], F32)
        identf = sb("identf", [128, 128], F32)
        identb = sb("identb", [128, 128], BF16)
        invf = sb("invf", [128, 64], F32)
        kdec = sb("kdec", [128, 4], F32)
        invcnt = sb("invcnt", [128, 4, 16], F32)
        pscale = sb("pscale", [128, 4], F32)
        wr32 = sb("wr32", [128, 8, 36], F32)
        br32 = sb("br32", [128, 36], F32)
        wr_hi = sb("wr_hi", [128, 8, 36], BF16)
        wr_lo = sb("wr_lo", [128, 8, 36], BF16)
        x1Tlo = sb("x1Tlo", [128, 8, 128], BF16)
        onesb = sb("onesb", [128, 128], BF16)
        wgrp_b = sb("wgrp_b", [128, 4, 128], BF16)
        gates = sb("gates", [128, NT, 32], F32)
        epsT = sb("epsT", [128, 1], F32)
        small = sb("small", [128, 256], F32)
        xTh = sb("xTh", [128, 8, 16], BF16)
        posi = sb("posi", [128, 2, NT], I32)
        posf = sb("posf", [128, 2, NT], F32)

        S = UN[:, 0:1024].rearrange("p (h c) -> p h c", h=4)
        S_bf = UN[:, 1024:1536].bitcast(BF16).rearrange("p (h c) -> p h c", h=4)
        maskT = UN[:, 1536:2048].rearrange("p (h c) -> p h c", h=4)
        qdec = UN[:, 2048:2052]
        qrd = UN[:, 2056:2312].bitcast(BF16).rearrange("p (h c) -> p h c", h=4)
        xo = [UN[:, 0:1024], UN[:, 1024:2048]]
        hib = UN[:, 2048:2560].bitcast(BF16)
        lob = UN[:, 2560:3072].bitcast(BF16)

        accb = ACC[:].rearrange("p t d -> p (t d)")
        accbf = accb.bitcast(BF16)

        def acc_f32(off_bytes, n):
            assert off_bytes % 4 == 0 and off_bytes + 4 * n <= 65536
            return accb[:, off_bytes // 4: off_bytes // 4 + n]

        def acc_bf16(off_bytes, n):
            assert off_bytes % 2 == 0 and off_bytes + 2 * n <= 65536
            return accbf[:, off_bytes // 2: off_bytes // 2 + n]

        Z = acc_bf16(0, 8 * T).rearrange("p (k t) -> p k t", k=8)
        SC0 = 32768
        Mb = M[:].rearrange("p k t -> p (k t)")
        Mf = Mb.bitcast(F32)

        def m_f32(off_bytes, n):
            assert off_bytes % 4 == 0 and off_bytes + 4 * n <= 32768
            return Mf[:, off_bytes // 4: off_bytes // 4 + n]

        def m_bf16(off_bytes, n):
            assert off_bytes % 2 == 0 and off_bytes + 2 * n <= 32768
            return Mb[:, off_bytes // 2: off_bytes // 2 + n]

        PB = [es.enter_context(nc.psum_tensor(f"pb{i}", [128, 1024], F32)) for i in range(4)]

        def bank(i):
            return PB[i // 2][:, (i % 2) * 512:(i % 2) * 512 + 512]

        def bank_bf(i):
            return bank(i).bitcast(BF16)

        def col_chunk(w, col0):
            def f(e, slot):
                return e.dma_start(out=slot[:].rearrange("p (k n) -> p k n", k=8),
                                   in_=w[:, col0:col0 + 512].rearrange("(k p) n -> p k n", p=128))
            return f

        def mat_rows4(w):
            def f(e, slot):
                return e.dma_start(out=slot[:].rearrange("p (k n) -> p k n", k=4),
                                   in_=w.rearrange("(k p) n -> p k n", p=128))
            return f

        def exp_gu(w, ei, c0):
            def f(e, slot):
                return e.dma_start(out=slot[:].rearrange("p (k n) -> p k n", k=8)[:, :, c0:c0 + 256],
                                   in_=w[ei].rearrange("(k p) n -> p k n", p=128))
            return f

        def exp_d(ei):
            def f(e, slot):
                return e.dma_start(out=slot[:, 0:2048].rearrange("p (k n) -> p k n", k=2),
                                   in_=w_exp_down[ei].rearrange("(k p) n -> p k n", p=128))
            return f

        COL_POOL, COL_Q, COL_K, COL_V, COL_G, COL_XQ, COL_GATE = 0, 512, 1024, 1536, 2560, 3584, 4096
        items = [
            ("wk", [("", col_chunk(w_in, COL_K))]),
            ("wv0", [("", col_chunk(w_in, COL_V))]),
            ("wv1", [("", col_chunk(w_in, COL_V + 512))]),
            ("wq", [("", col_chunk(w_in, COL_Q))]),
            ("wg0", [("", col_chunk(w_in, COL_G))]),
            ("wg1", [("", col_chunk(w_in, COL_G + 512))]),
            ("gt1a", [("", col_chunk(w_in, COL_GATE + 1024))]),
            ("gt1b", [("", col_chunk(w_in, COL_GATE + 1536))]),
            ("wbr0", [("", col_chunk(w_br_ret, 0))]),
            ("wbr1", [("", col_chunk(w_br_ret, 512))]),
            ("wpool", [("", col_chunk(w_in, COL_POOL))]),
            ("gt0a", [("", col_chunk(w_in, COL_GATE))]),
            ("gt0b", [("", col_chunk(w_in, COL_GATE + 512))]),
            ("wbp", [("", mat_rows4(w_br_pool))]),
            ("wmk", [("", col_chunk(w_mem_kv, 0))]),
            ("wmv", [("", col_chunk(w_mem_kv, 512))]),
            ("wxq", [("", col_chunk(w_in, COL_XQ))]),
            ("gt2a", [("", col_chunk(w_in, COL_GATE + 2048))]),
            ("gt2b", [("", col_chunk(w_in, COL_GATE + 2560))]),
            ("wbx", [("", mat_rows4(w_br_xa))]),
            ("wo0", [("", col_chunk(w_out, 0))]),
            ("wo1", [("", col_chunk(w_out, 512))]),
        ]
        for ei in range(NEXP):
            items.append((f"egu{ei}", [("a", exp_gu(w_exp_gate, ei, 0)), ("b", exp_gu(w_exp_up, ei, 256))]))
            items.append((f"ed{ei}", [("", exp_d(ei))]))

        def ld(dst, src, key):
            P.dma("sp", lambda e: e.dma_start(out=dst, in_=src), "ld_" + key, writes=[key])

        wgrp_f = acc_f32(53248, 512).rearrange("p (g d) -> p g d", g=4)
        ld(identf[:], c_ident, "identf")
        pre_thunks = [
            lambda: ld(posi[:, 0, :], pos_own, "posi0"),
            lambda: ld(posi[:, 1, :], pos_prev, "posi1"),
            lambda: ld(invf[:], c_invf, "invf"),
            lambda: ld(kdec[:], c_kdec, "kdec"),
            lambda: ld(maskT, c_maskT, "maskT"),
            lambda: ld(qdec, c_qdec, "qdec"),
            lambda: ld(invcnt[:], c_invcnt, "invcnt"),
            lambda: ld(pscale[:], pool_scale, "pscale"),
            lambda: ld(wr32[:], w_router.rearrange("(k p) n -> p k n", p=128), "wr32"),
            lambda: ld(br32[:], b_router[0].partition_broadcast(128), "br32"),
            lambda: ld(wgrp_f, w_pool_grp.rearrange("g c d -> c g d"), "wgrp_f"),
        ]

        W = WStream(P, ring, items, n_init=3)
        try:

            P.op("dve", lambda e: e.tensor_copy(out=identb[:], in_=identf[:]), reads=["identf"], writes=["identb"])
            wr_hif = acc_f32(55296, 288).rearrange("p (k n) -> p k n", k=8)
            post_thunks = [
                lambda: P.op("dve", lambda e: e.memset(S, 0.0), writes=["S"]),
                lambda: P.op("dve", lambda e: e.memset(S_bf, 0.0), writes=["S_bf"]),
                lambda: P.op("dve", lambda e: e.memset(onesb[:], 1.0), writes=["onesb"]),
                lambda: P.op("dve", lambda e: e.memset(epsT[:], LN_EPS), writes=["epsT"]),
                lambda: P.op("dve", lambda e: e.tensor_copy(out=wr_hi[:], in_=wr32[:]), reads=["wr32"], writes=["wr_hi"]),
                lambda: P.op("dve", lambda e: e.tensor_copy(out=wr_hif, in_=wr_hi[:]), reads=["wr_hi"], writes=["wr_hif"]),
                lambda: P.op("dve", lambda e: e.tensor_tensor(out=wr_lo[:], in0=wr32[:], in1=wr_hif, op=ALU.subtract), reads=["wr32", "wr_hif"], writes=["wr_lo"]),
                lambda: P.op("dve", lambda e: e.tensor_copy(out=wgrp_b[:], in_=wgrp_f), reads=["wgrp_f"], writes=["wgrp_b"]),
            ]

            posf_thunk = lambda: P.op("dve", lambda e: e.tensor_copy(out=posf[:], in_=posi[:]), reads=["posi0", "posi1"], writes=["posf"])
            tab_own = m_f32(0, NT * 192).rearrange("p (t c) -> p t c", t=NT)
            tab_prev = acc_f32(SC0, NT * 192).rearrange("p (t c) -> p t c", t=NT)
            tmpA = acc_f32(SC0 + 12288, NT * 128).rearrange("p (t c) -> p t c", t=NT)
            tmpI = m_f32(12288, NT * 128).bitcast(I32).rearrange("p (t c) -> p t c", t=NT)
            tab_thunks = []
            for which, tab, key in ((1, tab_prev, "tab_prev"), (0, tab_own, "tab_own")):
                pz = posf[:, which, :]
                tab_thunks.append(lambda pz=pz: P.op("dve", lambda e: e.tensor_tensor(
                    out=tmpA[:, :, 0:64], in0=pz.unsqueeze(2).broadcast_to([128, NT, 64]),
                    in1=invf[:].unsqueeze(1).broadcast_to([128, NT, 64]), op=ALU.mult),
                    reads=["posf", "invf"], writes=["tmpA"]))
                tab_thunks.append(lambda: P.op("dve", lambda e: e.tensor_scalar(out=tmpA[:, :, 64:128], in0=tmpA[:, :, 0:64], scalar1=0.25, scalar2=None, op0=ALU.add),
                                               reads=["tmpA"], writes=["tmpA"]))
                tab_thunks.append(lambda: P.op("dve", lambda e: e.tensor_copy(out=tmpI, in_=tmpA), reads=["tmpA"], writes=["tmpI"]))
                tab_thunks.append(lambda: P.op("dve", lambda e: e.tensor_tensor(out=tmpA, in0=tmpA, in1=tmpI, op=ALU.subtract), reads=["tmpA", "tmpI"], writes=["tmpA"]))
                tab_thunks.append(lambda: P.op("dve", lambda e: e.scalar_tensor_tensor(out=tmpA, in0=tmpA, scalar=0.5, in1=tmpA, op0=ALU.is_gt, op1=ALU.subtract),
                                               reads=["tmpA"], writes=["tmpA"]))
                tab_thunks.append(lambda tab=tab, key=key: P.op("act", lambda e: e.activation(out=tab[:, :, 0:128], in_=tmpA, func=AF.Sin, scale=float(-2.0 * np.pi)),
                                                               reads=["tmpA"], writes=[key]))
                tab_thunks.append(lambda tab=tab, key=key: P.op("act", lambda e: e.copy(out=tab[:, :, 128:192], in_=tab[:, :, 0:64]), reads=[key], writes=[key]))

            tab_thunks[:] = pre_thunks[0:3] + [posf_thunk] + tab_thunks + pre_thunks[3:] + post_thunks

            def drip():
                if tab_thunks:
                    tab_thunks.pop(0)()

            def load_transposed(src, dstT, ntiles, tag, xs_l, xb_l, halo=False, drip_on=False):
                for t in range(ntiles):
                    bs = t % len(xs_l)
                    b = t % len(xb_l)
                    P.dma("sp", lambda e, t=t, bs=bs: e.dma_start(out=xs_l[bs], in_=src[t * 128:(t + 1) * 128, :]), f"ld_x{bs}", writes=[f"xs{bs}"])
                    P.op("act", lambda e, b=b, bs=bs: e.copy(out=xb_l[b], in_=xs_l[bs]), reads=[f"xs{bs}"], writes=[f"xb{b}"])
                    pb_ = 6 + (t % 2)
                    pt = bank_bf(pb_).rearrange("p (k t) -> p k t", k=8)
                    P.op("pe", [lambda e, k=k, b=b, pt=pt: e.transpose(out=pt[:, k, :], in_=xb_l[b][:, k * 128:(k + 1) * 128], identity=identb[:])
                                for k in range(8)], reads=[f"xb{b}", "identb"], writes=[f"B{pb_}"])
                    P.op("dve", lambda e, t=t, pt=pt: e.tensor_copy(out=dstT[:, :, t * 128:(t + 1) * 128], in_=pt),
                         reads=[f"B{pb_}"], writes=[f"{tag}{t}"])
                    if halo and t == ntiles - 1:
                        P.op("dve", lambda e, pt=pt: e.tensor_copy(out=xTh[:], in_=pt[:, :, 112:128]), reads=[f"B{pb_}"], writes=["xTh"])
                    if drip_on:
                        drip()

            xT = A[:]
            xTp = Z
            xsA = [acc_f32(57344, 1024), acc_f32(61440, 1024), m_f32(20480, 1024), m_f32(28672, 1024)]
            xbA = [m_bf16(24576, 1024), m_bf16(26624, 1024)]
            load_transposed(x_prev, xTp, NT, "xTp", xsA, xbA, halo=True, drip_on=True)
            load_transposed(x_own, xT, NT, "xT", xsA, xbA, drip_on=True)
            W.kick(after_keys=["xs0", "xs1", "xs2", "xs3"])
            while tab_thunks:
                drip()
            XT_ALL = [f"xT{t}" for t in range(NT)]
            P.barrier()
            _stop(1)

            GAM = [1.0 - 2.0 ** (-5.0 - h) for h in range(H)]
            GAMC = [float(g ** C) for g in GAM]
            h4 = lambda ap: ap.rearrange("p (h c) -> p h c", h=4)
            t12 = h4(m_f32(12288, 512))
            t34 = h4(m_f32(14336, 512))
            q_rot = h4(m_bf16(16384, 512))
            k_rot = h4(m_bf16(17408, 512))
            vd = h4(m_bf16(18432, 1024))
            qT = h4(m_bf16(20480, 512))
            qTd = h4(m_bf16(21504, 512))
            kT = h4(m_bf16(22528, 512))
            smk = h4(m_bf16(23552, 512))
            sg = h4(m_f32(24576, 1024))
            gnw = h4(acc_f32(SC0 + 12288, 1024))
            yn = h4(acc_f32(SC0 + 16384, 1024))
            yret = acc_bf16(SC0 + 20480, 1024)
            stt = small[:, 0:24].rearrange("p (h s) -> p h s", h=4)
            mv = small[:, 24:32].rearrange("p (h s) -> p h s", h=4)
            rstd = small[:, 32:36]
            sq = small[:, 36:40]

            P.dma("sp", lambda e: e.dma_start(out=gnw.rearrange("p h c -> p (h c)"), in_=ret_gn_w.partition_broadcast(128)), "ld_gnw", writes=["gnw"])

            def rot(ps_bank, tab, t, dst, dst_key, tab_key):
                src = h4(bank(ps_bank))
                cs1 = tab[:, t, 64:192].unsqueeze(1).broadcast_to([128, 4, 128])
                cs2 = tab[:, t, 0:128].unsqueeze(1).broadcast_to([128, 4, 128])
                P.op("dve", lambda e: e.tensor_tensor(out=t12, in0=src, in1=cs1, op=ALU.mult), reads=[f"B{ps_bank}", tab_key], writes=["t12"])
                P.op("dve", lambda e: e.tensor_tensor(out=t34, in0=src, in1=cs2, op=ALU.mult), reads=[f"B{ps_bank}", tab_key], writes=["t34"])
                P.op("dve", lambda e: e.tensor_tensor(out=dst[:, :, 0:64], in0=t12[:, :, 0:64], in1=t12[:, :, 64:128], op=ALU.subtract),
                     reads=["t12"], writes=[dst_key + "a"])
                P.op("dve", lambda e: e.tensor_tensor(out=dst[:, :, 64:128], in0=t34[:, :, 0:64], in1=t34[:, :, 64:128], op=ALU.add),
                     reads=["t34"], writes=[dst_key + "b"])

            def proj_tok(ps_bank, srcT, t, wname, src_keys):
                slot, keys = W.get(wname)
                wv = slot[:].rearrange("p (k n) -> p k n", k=8)
                P.op("pe", [lambda e, k=k: e.matmul(bank(ps_bank), lhsT=srcT[:, k, t * 128:(t + 1) * 128], rhs=wv[:, k, :],
                                                    start=(k == 0), stop=(k == 7)) for k in range(8)],
                     reads=keys + src_keys, writes=[f"B{ps_bank}"])

            def ret_chunk(srcT, src_tag, t, tab, tab_key, full):
                skeys = [f"{src_tag}{t}"]
                proj_tok(1, srcT, t, "wk", skeys)
                proj_tok(2, srcT, t, "wv0", skeys)
                proj_tok(3, srcT, t, "wv1", skeys)
                if full:
                    proj_tok(0, srcT, t, "wq", skeys)
                    proj_tok(4, srcT, t, "wg0", skeys)
                    proj_tok(5, srcT, t, "wg1", skeys)
                rot(1, tab, t, k_rot, "k_rot", tab_key)
                vps = h4(PB[1][:])
                P.op("act", [lambda e, h=h: e.activation(out=vd[:, h, :], in_=vps[:, h, :], func=AF.Identity, scale=kdec[:, h:h + 1]) for h in range(4)],
                     reads=["B2", "B3", "kdec"], writes=["vd"])
                if full:
                    rot(0, tab, t, q_rot, "q_rot", tab_key)
                    P.op("act", lambda e: e.activation(out=sg.rearrange("p h c -> p (h c)"), in_=PB[2][:], func=AF.Silu),
                         reads=["B4", "B5"], writes=["sg"])
                    if t == 0: _stop(13)
                    tp = bank_bf(6).rearrange("p (k c) -> p k c", k=8)
                    P.op("pe", [lambda e, h=h: e.transpose(out=tp[:, h, :], in_=q_rot[:, h, :], identity=identb[:]) for h in range(4)] +
                         [lambda e, h=h: e.transpose(out=tp[:, 4 + h, :], in_=k_rot[:, h, :], identity=identb[:]) for h in range(4)],
                         reads=["q_rota", "q_rotb", "k_rota", "k_rotb", "identb"], writes=["B6"])
                    if t == 0: _stop(141)
                    P.op("act", lambda e: e.copy(out=qT, in_=tp[:, 0:4, :]), reads=["B6"], writes=["qT"])
                    if t == 0: _stop(142)
                    P.op("act", [lambda e, h=h: e.activation(out=qrd[:, h, :], in_=q_rot[:, h, :], func=AF.Identity, scale=qdec[:, h:h + 1]) for h in range(4)],
                         reads=["q_rota", "q_rotb", "qdec"], writes=["qrd"])
                    tpd = bank_bf(7)[:, 0:512].rearrange("p (k c) -> p k c", k=4)
                    P.op("pe", [lambda e, h=h: e.transpose(out=tpd[:, h, :], in_=qrd[:, h, :], identity=identb[:]) for h in range(4)],
                         reads=["qrd", "identb"], writes=["B7"])
                    P.op("act", lambda e: e.copy(out=qTd, in_=tpd), reads=["B7"], writes=["qTd"])
                    if t == 0: _stop(143)
                    P.op("act", lambda e: e.copy(out=kT, in_=tp[:, 4:8, :]), reads=["B6"], writes=["kT"])
                    if t == 0: _stop(14)
                    sc = h4(bank(7))
                    P.op("pe", [lambda e, h=h: e.matmul(sc[:, h, :], lhsT=kT[:, h, :], rhs=qT[:, h, :], start=True, stop=True) for h in range(4)],
                         reads=["kT", "qT"], writes=["B7"])
                    P.op("dve", lambda e: e.tensor_tensor(out=smk, in0=sc, in1=maskT, op=ALU.mult), reads=["B7", "maskT"], writes=["smk"])
                    yps = h4(PB[0][:])
                    fl = []
                    for h in range(4):
                        fl.append(lambda e, h=h: e.matmul(yps[:, h, :], lhsT=smk[:, h, :], rhs=vd[:, h, :], start=True, stop=False))
                        fl.append(lambda e, h=h: e.matmul(yps[:, h, :], lhsT=qTd[:, h, :], rhs=S_bf[:, h, :], start=False, stop=True))
                    P.op("pe", fl, reads=["smk", "vd", "qTd", "S_bf"], writes=["B0", "B1"])
                    if t == 0: _stop(15)
                kvps = h4(PB[1][:])
                P.op("pe", [lambda e, h=h: e.matmul(kvps[:, h, :], lhsT=k_rot[:, h, :], rhs=vd[:, h, :], start=True, stop=True) for h in range(4)],
                     reads=["k_rota", "k_rotb", "vd"], writes=["B2", "B3"])
                P.op("dve", [lambda e, h=h: e.scalar_tensor_tensor(out=S[:, h, :], in0=S[:, h, :], scalar=GAMC[h], in1=kvps[:, h, :],
                                                                   op0=ALU.mult, op1=ALU.add) for h in range(4)],
                     reads=["B2", "B3", "S"], writes=["S"])
                P.op("act", lambda e: e.copy(out=S_bf, in_=S), reads=["S"], writes=["S_bf"])
                if full and t == 0: _stop(16)
                if full:
                    yps = h4(PB[0][:])
                    P.op("dve", [lambda e, h=h: e.bn_stats(out=stt[:, h, :], in_=yps[:, h, :]) for h in range(4)], reads=["B0", "B1"], writes=["stt"])
                    P.op("dve", [lambda e, h=h: e.bn_aggr(out=mv[:, h, :], in_=stt[:, h, :]) for h in range(4)], reads=["stt"], writes=["mv"])
                    P.op("act", lambda e: e.activation(out=sq, in_=mv[:, :, 1], func=AF.Sqrt, bias=epsT[:, 0:1]), reads=["mv", "epsT"], writes=["sq"])
                    P.op("dve", lambda e: e.reciprocal(out=rstd, in_=sq), reads=["sq"], writes=["rstd"])
                    P.op("dve", [lambda e, h=h: e.scalar_tensor_tensor(out=yn[:, h, :], in0=yps[:, h, :], scalar=mv[:, h, 0:1], in1=gnw[:, h, :],
                                                                       op0=ALU.subtract, op1=ALU.mult) for h in range(4)],
                         reads=["B0", "B1", "mv", "gnw"], writes=["yn"])
                    yr = h4(yret)
                    P.op("dve", [lambda e, h=h: e.scalar_tensor_tensor(out=yr[:, h, :], in0=yn[:, h, :], scalar=rstd[:, h:h + 1], in1=sg[:, h, :],
                                                                       op0=ALU.mult, op1=ALU.mult) for h in range(4)],
                         reads=["yn", "rstd", "sg"], writes=["yret"])
                    if t == 0: _stop(17)
                    tp = bank_bf(6).rearrange("p (k c) -> p k c", k=8)
                    P.op("pe", [lambda e, k=k: e.transpose(out=tp[:, k, :], in_=yret[:, k * 128:(k + 1) * 128], identity=identb[:]) for k in range(8)],
                         reads=["yret", "identb"], writes=["B6"])
                    P.op("act", lambda e: e.copy(out=Z[:, :, t * 128:(t + 1) * 128], in_=tp), reads=["B6"], writes=[f"yT{t}", f"xTp{t}"])
                    if t == 0: _stop(18)

            _stop(10)
            for t in range(NT):
                ret_chunk(xTp, "xTp", t, tab_prev, "tab_prev", False)
                if t == 0:
                    _stop(11)
            _stop(12)
            def o_e1(t):
                sk = [f"xT{t}"]
                proj_tok(1, xT, t, "wk", sk)
                proj_tok(2, xT, t, "wv0", sk)
                proj_tok(3, xT, t, "wv1", sk)
                proj_tok(0, xT, t, "wq", sk)

            def o_e2(t):
                rot(1, tab_own, t, k_rot, "k_rot", "tab_own")
                vps = h4(PB[1][:])
                P.op("act", [lambda e, h=h: e.activation(out=vd[:, h, :], in_=vps[:, h, :], func=AF.Identity, scale=kdec[:, h:h + 1]) for h in range(4)],
                     reads=["B2", "B3", "kdec"], writes=["vd"])
                rot(0, tab_own, t, q_rot, "q_rot", "tab_own")
                P.op("act", [lambda e, h=h: e.activation(out=qrd[:, h, :], in_=q_rot[:, h, :], func=AF.Identity, scale=qdec[:, h:h + 1]) for h in range(4)],
                     reads=["q_rota", "q_rotb", "qdec"], writes=["qrd"])
                tp = bank_bf(6).rearrange("p (k c) -> p k c", k=8)
                tpd = bank_bf(7)[:, 0:512].rearrange("p (k c) -> p k c", k=4)
                P.op("pe", [lambda e, h=h: e.transpose(out=tp[:, h, :], in_=q_rot[:, h, :], identity=identb[:]) for h in range(4)] +
                     [lambda e, h=h: e.transpose(out=tp[:, 4 + h, :], in_=k_rot[:, h, :], identity=identb[:]) for h in range(4)],
                     reads=["q_rota", "q_rotb", "k_rota", "k_rotb", "identb"], writes=["B6"])
                P.op("pe", [lambda e, h=h: e.transpose(out=tpd[:, h, :], in_=qrd[:, h, :], identity=identb[:]) for h in range(4)],
                     reads=["qrd", "identb"], writes=["B7"])
                P.op("act", lambda e: e.copy(out=qT, in_=tp[:, 0:4, :]), reads=["B6"], writes=["qT"])
                P.op("dve", lambda e: e.tensor_copy(out=kT, in_=tp[:, 4:8, :]), reads=["B6", "qT"], writes=["kT"])
                P.op("act", lambda e: e.copy(out=qTd, in_=tpd), reads=["B7"], writes=["qTd"])
                sc = h4(bank(7))
                P.op("pe", [lambda e, h=h: e.matmul(sc[:, h, :], lhsT=kT[:, h, :], rhs=qT[:, h, :], start=True, stop=True) for h in range(4)],
                     reads=["kT", "qT"], writes=["B7"])
                P.op("dve", lambda e: e.tensor_tensor(out=smk, in0=sc, in1=maskT, op=ALU.mult), reads=["B7", "maskT"], writes=["smk"])

            def o_g(t):
                sk = [f"xT{t}"]
                proj_tok(4, xT, t, "wg0", sk)
                proj_tok(5, xT, t, "wg1", sk)

            def o_e3(t):
                P.op("act", lambda e: e.activation(out=sg.rearrange("p h c -> p (h c)"), in_=PB[2][:], func=AF.Silu),
                     reads=["B4", "B5"], writes=["sg"])

            def o_e4(t):
                yps = h4(PB[2][:])
                fl = []
                for h in range(4):
                    fl.append(lambda e, h=h: e.matmul(yps[:, h, :], lhsT=smk[:, h, :], rhs=vd[:, h, :], start=True, stop=False))
                    fl.append(lambda e, h=h: e.matmul(yps[:, h, :], lhsT=qTd[:, h, :], rhs=S_bf[:, h, :], start=False, stop=True))
                P.op("pe", fl, reads=["smk", "vd", "qTd", "S_bf"], writes=["B4", "B5"])

            def o_e5(t):
                kvps = h4(PB[1][:])
                P.op("pe", [lambda e, h=h: e.matmul(kvps[:, h, :], lhsT=k_rot[:, h, :], rhs=vd[:, h, :], start=True, stop=True) for h in range(4)],
                     reads=["k_rota", "k_rotb", "vd"], writes=["B2", "B3"])
                P.op("dve", [lambda e, h=h: e.scalar_tensor_tensor(out=S[:, h, :], in0=S[:, h, :], scalar=GAMC[h], in1=kvps[:, h, :],
                                                                   op0=ALU.mult, op1=ALU.add) for h in range(4)],
                     reads=["B2", "B3", "S"], writes=["S"])
                P.op("act", lambda e: e.copy(out=S_bf, in_=S), reads=["S"], writes=["S_bf"])

            def o_e6a(t):
                yps = h4(PB[2][:])
                P.op("dve", [lambda e, h=h: e.bn_stats(out=stt[:, h, :], in_=yps[:, h, :]) for h in range(4)], reads=["B4", "B5"], writes=["stt"])
                P.op("dve", [lambda e, h=h: e.bn_aggr(out=mv[:, h, :], in_=stt[:, h, :]) for h in range(4)], reads=["stt"], writes=["mv"])
                P.op("act", lambda e: e.activation(out=sq, in_=mv[:, :, 1], func=AF.Sqrt, bias=epsT[:, 0:1]), reads=["mv", "epsT"], writes=["sq"])
                P.op("dve", lambda e: e.reciprocal(out=rstd, in_=sq), reads=["sq"], writes=["rstd"])
                P.op("dve", [lambda e, h=h: e.scalar_tensor_tensor(out=yn[:, h, :], in0=yps[:, h, :], scalar=mv[:, h, 0:1], in1=gnw[:, h, :],
                                                                   op0=ALU.subtract, op1=ALU.mult) for h in range(4)],
                     reads=["B4", "B5", "mv", "gnw"], writes=["yn"])
                yr = h4(yret)
                P.op("dve", [lambda e, h=h: e.scalar_tensor_tensor(out=yr[:, h, :], in0=yn[:, h, :], scalar=rstd[:, h:h + 1], in1=sg[:, h, :],
                                                                   op0=ALU.mult, op1=ALU.mult) for h in range(4)],
                     reads=["yn", "rstd", "sg"], writes=["yret"])

            def o_e6b(t):
                tp = bank_bf(6).rearrange("p (k c) -> p k c", k=8)
                P.op("pe", [lambda e, k=k: e.transpose(out=tp[:, k, :], in_=yret[:, k * 128:(k + 1) * 128], identity=identb[:]) for k in range(8)],
                     reads=["yret", "identb"], writes=["B6"])
                P.op("act", lambda e, t=t: e.copy(out=Z[:, :, t * 128:(t + 1) * 128], in_=tp), reads=["B6"], writes=[f"yT{t}", f"xTp{t}"])

            o_e1(0)
            for t in range(NT):
                o_g(t)
                o_e2(t)
                o_e3(t)
                if t == NT - 1:
                    W.release("wg0")
                    W.release("wg1")
                o_e4(t)
                o_e5(t)
                o_e6a(t)
                if t + 1 < NT:
                    o_e1(t + 1)
                    if t + 1 == NT - 1:
                        for nm in ("wk", "wv0", "wv1", "wq"):
                            W.release(nm)
                o_e6b(t)
            P.barrier()
            _stop(2)
            if debug:
                P.dma("sp", lambda e: e.dma_start(out=dbg["z"], in_=Z), "st_dbg", reads=[f"yT{t}" for t in range(NT)], is_store=True)

            gsb = [acc_f32(SC0 + i * 2048, 512) for i in range(2)]
            tmpm = [acc_bf16(SC0 + 4096 + i * 2048, 512) for i in range(2)]
            MERGE_KEYS = [f"M{j}_{g}" for j in range(8) for g in range(NG)]

            def merge_branch(yT, ykeys, nkc, gta, gtb, wlist, first, hook=None):
                ga_s, ga_k = W.get(gta)
                gb_s, gb_k = W.get(gtb)
                it = 0
                for j in range(8):
                    if j == 4:
                        W.release(gta)
                        if nkc == 8:
                            W.release(wlist[0])
                    gs_, gk_ = (ga_s, ga_k) if j < 4 else (gb_s, gb_k)
                    gv = gs_[:].rearrange("p (k n) -> p k n", k=8)
                    if nkc == 8:
                        ws_, wk_ = W.get(wlist[0] if j < 4 else wlist[1])
                        wv = ws_[:].rearrange("p (k n) -> p k n", k=8)
                        lhs = [wv[:, k, (j % 4) * 128:(j % 4) * 128 + 128] for k in range(8)]
                    else:
                        ws_, wk_ = W.get(wlist[0])
                        wv = ws_[:].rearrange("p (k n) -> p k n", k=4)
                        lhs = [wv[:, k, j * 128:(j + 1) * 128] for k in range(4)]
                    for g in range(NG):
                        b = it % 2
                        it += 1
                        cols = slice(g * 512, (g + 1) * 512)
                        P.op("pe", [lambda e, k=k, gv=gv, j=j, cols=cols, b=b: e.matmul(bank(b), lhsT=gv[:, k, (j % 4) * 128:(j % 4) * 128 + 128],
                                                                                      rhs=xT[:, k, cols], start=(k == 0), stop=(k == 7)) for k in range(8)],
                             reads=gk_ + XT_ALL[g * 4:(g + 1) * 4], writes=[f"B{b}"])
                        P.op("pe", [lambda e, k=k, lhs=lhs, cols=cols, b=b: e.matmul(bank(2 + b), lhsT=lhs[k], rhs=yT[:, k, cols],
                                                                                   start=(k == 0), stop=(k == nkc - 1)) for k in range(nkc)],
                             reads=wk_ + ykeys, writes=[f"B{2 + b}"])
                        P.op("act", lambda e, b=b: e.activation(out=gsb[b], in_=bank(b), func=AF.Sigmoid), reads=[f"B{b}"], writes=[f"gsb{b}"])
                        mk = f"M{j}_{g}"
                        if first:
                            P.op("dve", lambda e, b=b, j=j, cols=cols: e.tensor_tensor(out=M[:, j, cols], in0=bank(2 + b), in1=gsb[b], op=ALU.mult),
                                 reads=[f"B{2 + b}", f"gsb{b}"], writes=[mk])
                        else:
                            P.op("dve", lambda e, b=b: e.tensor_tensor(out=tmpm[b], in0=bank(2 + b), in1=gsb[b], op=ALU.mult),
                                 reads=[f"B{2 + b}", f"gsb{b}"], writes=[f"tmpm{b}"])
                            P.op("dve", lambda e, b=b, j=j, cols=cols: e.tensor_tensor(out=M[:, j, cols], in0=M[:, j, cols], in1=tmpm[b], op=ALU.add),
                                 reads=[f"tmpm{b}", mk], writes=[mk])
                        if hook is not None:
                            hook(it - 1)

            UW = 16 + 512
            ub = [acc_f32(40960 + i * 2176, UW) for i in range(3)]
            pooled = acc_bf16(47488, 512)
            YP = acc_bf16(48512, 4 * T).rearrange("p (k t) -> p k t", k=4)
            wp_s, wp_k = W.get("wpool")
            wpv = wp_s[:].rearrange("p (k n) -> p k n", k=8)
            WINS = (2, 4, 8, 16)

            def pool_iter(i):
                gi, g = i // NG, i % NG
                b = i % 2
                P.op("pe", [lambda e, k=k: e.matmul(bank(4 + b), lhsT=wpv[:, k, gi * 128:(gi + 1) * 128],
                                                    rhs=xT[:, k, g * 512:(g + 1) * 512], start=(k == 0), stop=(k == 7)) for k in range(8)],
                     reads=wp_k + XT_ALL[g * 4:(g + 1) * 4], writes=[f"B{4 + b}"])
                if g == 0:
                    hb = 7 - b
                    P.op("pe", [lambda e, k=k: e.matmul(bank(hb)[:, 0:16], lhsT=wpv[:, k, gi * 128:(gi + 1) * 128], rhs=xTh[:, k, :],
                                                        start=(k == 0), stop=(k == 7)) for k in range(8)],
                         reads=wp_k + ["xTh"], writes=[f"B{hb}"])
                    P.op("act", lambda e: e.copy(out=ub[0][:, 0:16], in_=bank(hb)[:, 0:16]), reads=[f"B{hb}"], writes=["ub0h"])
                P.op("act", lambda e: e.copy(out=ub[0][:, 16:UW], in_=bank(4 + b)), reads=[f"B{4 + b}"], writes=["ub0m"])
                cur = 0
                for s_ in range(gi + 1):
                    sh = 1 << s_
                    nxt = 1 if cur != 1 else 2
                    P.op("dve", lambda e, cur=cur, nxt=nxt, sh=sh: e.tensor_tensor(out=ub[nxt][:, sh:UW], in0=ub[cur][:, sh:UW],
                                                                                  in1=ub[cur][:, 0:UW - sh], op=ALU.add),
                         reads=["ub0m", "ub0h", f"ubs{cur}"], writes=[f"ubs{nxt}"])
                    cur = nxt
                w = WINS[gi]
                P.op("dve", lambda e, cur=cur: e.scalar_tensor_tensor(out=pooled, in0=ub[cur][:, 16:UW], scalar=float(1.0 / w),
                                                                      in1=ub[0][:, 16:UW], op0=ALU.mult, op1=ALU.subtract),
                     reads=[f"ubs{cur}", "ub0m"], writes=["pooled"])
                if g == 0:
                    P.op("dve", lambda e, cur=cur: e.tensor_tensor(out=ub[cur][:, 16:32], in0=ub[cur][:, 16:32], in1=invcnt[:, gi, :], op=ALU.mult),
                         reads=[f"ubs{cur}", "invcnt", "pooled"], writes=[f"ubs{cur}"])
                    P.op("dve", lambda e, cur=cur: e.tensor_tensor(out=pooled[:, 0:16], in0=ub[cur][:, 16:32], in1=ub[0][:, 16:32], op=ALU.subtract),
                         reads=[f"ubs{cur}", "ub0m"], writes=["pooled"])
                if g < NG - 1:
                    P.op("act", lambda e: e.copy(out=ub[0][:, 0:16], in_=ub[0][:, UW - 16:UW]), reads=["ub0m"], writes=["ub0h"])

            def pool_iter_b(i):
                gi, g = i // NG, i % NG
                b = i % 2
                P.op("pe", lambda e: e.matmul(bank(6 + b), lhsT=wgrp_b[:, gi, :], rhs=pooled, start=True, stop=True),
                     reads=["wgrp_b", "pooled"], writes=[f"B{6 + b}"])
                P.op("act", lambda e: e.activation(out=YP[:, gi, g * 512:(g + 1) * 512], in_=bank(6 + b), func=AF.Identity,
                                                   scale=pscale[:, gi:gi + 1]),
                     reads=[f"B{6 + b}", "pscale"], writes=[f"yp{gi}_{g}"])

            def pool_hook(it):
                if it % 2 == 1:
                    pool_iter(it // 2)
                elif it >= 2:
                    pool_iter_b(it // 2 - 1)

            YT_KEYS = [f"yT{t}" for t in range(NT)]
            merge_branch(Z, YT_KEYS, 8, "gt1a", "gt1b", ["wbr0", "wbr1"], True, hook=pool_hook)
            pool_iter_b(15)
            for nm in ("gt1a", "gt1b", "wbr0", "wbr1"):
                W.release(nm)
            P.barrier()
            _stop(3)

            W.release("wpool")
            YP_KEYS = [f"yp{gi}_{g}" for gi in range(4) for g in range(NG)]
            merge_branch(YP, YP_KEYS, 4, "gt0a", "gt0b", ["wbp"], False)
            for nm in ("gt0a", "gt0b", "wbp"):
                W.release(nm)
            P.barrier()
            _stop(4)

            memT = acc_bf16(40960, 8 * 256).rearrange("p (k m) -> p k m", k=8)
            KmT = acc_bf16(45056, 4 * 256).rearrange("p (h m) -> p h m", h=4)
            Vm = acc_bf16(47104, 2 * 512).rearrange("p (c n) -> p c n", c=2)
            xqT = acc_bf16(49152, 512)
            expT = acc_bf16(50176, 1024).rearrange("p (c n) -> p c n", c=2)
            lnS = acc_f32(52224, 512)
            rS = acc_f32(54272, 512)
            xsX = [acc_f32(57344, 1024)]
            xbX = [acc_bf16(61440, 1024)]
            load_transposed(mem, memT, 2, "memT", xsX, xbX)
            wmk_s, wmk_k = W.get("wmk")
            wmv_s, wmv_k = W.get("wmv")
            wmkv = wmk_s[:].rearrange("p (k n) -> p k n", k=8)
            wmvv = wmv_s[:].rearrange("p (k n) -> p k n", k=8)
            for h in range(4):
                b = h % 2
                P.op("pe", [lambda e, k=k, h=h, b=b: e.matmul(bank(4 + b)[:, 0:256], lhsT=wmkv[:, k, h * 128:(h + 1) * 128], rhs=memT[:, k, :],
                                                            start=(k == 0), stop=(k == 7)) for k in range(8)],
                     reads=wmk_k + ["memT0", "memT1"], writes=[f"B{4 + b}"])
                P.op("act", lambda e, h=h, b=b: e.copy(out=KmT[:, h, :], in_=bank(4 + b)[:, 0:256]), reads=[f"B{4 + b}"], writes=[f"KmT{h}"])
            for c in range(2):
                b = c % 2
                P.op("pe", [lambda e, k=k, c=c, b=b: e.matmul(bank(4 + b), lhsT=memT[:, k, c * 128:(c + 1) * 128], rhs=wmvv[:, k, :],
                                                            start=(k == 0), stop=(k == 7)) for k in range(8)],
                     reads=wmv_k + ["memT0", "memT1"], writes=[f"B{4 + b}"])
                P.op("act", lambda e, c=c, b=b: e.copy(out=Vm[:, c, :], in_=bank(4 + b)), reads=[f"B{4 + b}"], writes=[f"Vm{c}"])
            W.release("wmk")
            W.release("wmv")
            wxq_s, wxq_k = W.get("wxq")
            wxqv = wxq_s[:].rearrange("p (k n) -> p k n", k=8)
            YX = Z
            XSC = float(128.0 ** -0.5)
            expT2 = [expT, acc_bf16(63488, 1024).rearrange("p (c n) -> p c n", c=2)]

            def xa_a(i):
                h, g = i // NG, i % NG
                p = i % 2
                cols = slice(g * 512, (g + 1) * 512)
                P.op("pe", [lambda e, k=k: e.matmul(bank(4), lhsT=wxqv[:, k, h * 128:(h + 1) * 128], rhs=xT[:, k, cols],
                                                    start=(k == 0), stop=(k == 7)) for k in range(8)],
                     reads=wxq_k + XT_ALL[g * 4:(g + 1) * 4], writes=["B4"])
                P.op("act", lambda e: e.activation(out=xqT, in_=bank(4), func=AF.Copy, scale=XSC), reads=["B4"], writes=["xqT"])
                for c in range(2):
                    P.op("pe", lambda e, c=c: e.matmul(bank(5 + c), lhsT=KmT[:, h, c * 128:(c + 1) * 128], rhs=xqT, start=True, stop=True),
                         reads=[f"KmT{h}", "xqT"], writes=[f"B{5 + c}"])
                    P.op("act", lambda e, c=c: e.activation(out=expT2[p][:, c, :], in_=bank(5 + c), func=AF.Exp), reads=[f"B{5 + c}"],
                         writes=[f"expT{p}{c}"])

            def xa_b(i):
                h, g = i // NG, i % NG
                p = i % 2
                cols = slice(g * 512, (g + 1) * 512)
                ek = [f"expT{p}0", f"expT{p}1"]
                P.op("pe", [lambda e, c=c: e.matmul(bank(7), lhsT=Vm[:, c, h * 128:(h + 1) * 128], rhs=expT2[p][:, c, :], start=(c == 0), stop=(c == 1))
                            for c in range(2)], reads=["Vm0", "Vm1"] + ek, writes=["B7"])
                P.op("pe", [lambda e, c=c: e.matmul(bank(0), lhsT=onesb[:], rhs=expT2[p][:, c, :], start=(c == 0), stop=(c == 1)) for c in range(2)],
                     reads=["onesb"] + ek, writes=["B0"])
                P.op("act", lambda e: e.activation(out=lnS, in_=bank(0), func=AF.Ln), reads=["B0"], writes=["lnS"])
                P.op("act", lambda e: e.activation(out=rS, in_=lnS, func=AF.Exp, scale=-1.0), reads=["lnS"], writes=["rS"])
                P.op("dve", lambda e: e.tensor_tensor(out=YX[:, h, cols], in0=bank(7), in1=rS, op=ALU.mult),
                     reads=["B7", "rS"], writes=[f"yx{h}_{g}"])

            xa_a(0)
            for i in range(16):
                if i + 1 < 16:
                    xa_a(i + 1)
                xa_b(i)
            W.release("wxq")
            YX_KEYS = [f"yx{h}_{g}" for h in range(4) for g in range(NG)]
            merge_branch(YX, YX_KEYS, 4, "gt2a", "gt2b", ["wbx"], False)
            for nm in ("gt2a", "gt2b", "wbx"):
                W.release(nm)
            P.barrier()
            _stop(5)
            if debug:
                P.dma("sp", lambda e: e.dma_start(out=dbg["mT"], in_=M[:]), "st_dbg", reads=MERGE_KEYS, is_store=True)

            ld(lnw[:], ln1_w.partition_broadcast(128), "lnw")
            ld(lnb[:], ln1_b.partition_broadcast(128), "lnb")
            wo0_s, wo0_k = W.get("wo0")
            wo1_s, wo1_k = W.get("wo1")
            wov = [wo0_s[:].rearrange("p (k n) -> p k n", k=8), wo1_s[:].rearrange("p (k n) -> p k n", k=8)]
            x1T = A[:]
            st6 = small[:, 40:52].rearrange("p (c s) -> p c s", c=2)
            mv1 = small[:, 52:54]
            r1 = small[:, 54:55]
            nmr = small[:, 55:56]
            s1 = small[:, 56:57]
            L = small[:, 64:100]
            gneg = small[:, 100:101]
            gsum = small[:, 101:102]
            pgrp = small[:, 102:103]
            oh = small[:, 104:108]
            ge = small[:, 108:112]
            sel = small[:, 112:120]
            sel2 = small[:, 120:128]
            m1 = small[:, 128:129]
            m2 = small[:, 129:130]
            dd = small[:, 130:131]
            e2 = small[:, 131:132]
            w1p = small[:, 132:133]
            w2p = small[:, 133:134]
            mk1 = small[:, 136:144]
            mk2 = small[:, 144:152]
            wg8 = small[:, 152:160]

            def layer_norm(src, dst, tagr, tagw):
                P.op("dve", [lambda e, c=c: e.bn_stats(out=st6[:, c, :], in_=src[:, c * 512:(c + 1) * 512]) for c in range(2)], reads=tagr, writes=["st6"])
                P.op("dve", lambda e: e.bn_aggr(out=mv1, in_=st6.rearrange("p c s -> p (c s)")), reads=["st6"], writes=["mv1"])
                P.op("act", lambda e: e.activation(out=s1, in_=mv1[:, 1:2], func=AF.Sqrt, bias=epsT[:, 0:1]), reads=["mv1", "epsT"], writes=["s1"])
                P.op("dve", lambda e: e.reciprocal(out=r1, in_=s1), reads=["s1"], writes=["r1"])
                P.op("dve", lambda e: e.scalar_tensor_tensor(out=nmr, in0=mv1[:, 0:1], scalar=-1.0, in1=r1, op0=ALU.mult, op1=ALU.mult),
                     reads=["mv1", "r1"], writes=["nmr"])
                P.op("act", lambda e: e.activation(out=dst, in_=src, func=AF.Identity, scale=r1, bias=nmr), reads=tagr + ["r1", "nmr"], writes=tagw)
                P.op("dve", lambda e: e.tensor_tensor(out=dst, in0=dst, in1=lnw[:], op=ALU.mult), reads=tagw + ["lnw"], writes=tagw)
                P.op("dve", lambda e: e.tensor_tensor(out=dst, in0=dst, in1=lnb[:], op=ALU.add), reads=tagw + ["lnb"], writes=tagw)

            Lg = small[:, 160:224].rearrange("p (t g) -> p t g", t=NT)

            def attn_mm(t):
                bi = t % 2
                fl = []
                for hf in range(2):
                    for k in range(8):
                        fl.append(lambda e, hf=hf, k=k: e.matmul(PB[bi][:, hf * 512:(hf + 1) * 512], lhsT=M[:, k, t * 128:(t + 1) * 128],
                                                                rhs=wov[hf][:, k, :], start=(k == 0), stop=(k == 7)))
                P.op("pe", fl, reads=wo0_k + wo1_k + [f"M{j}_{t // 4}" for j in range(8)], writes=[f"B{2 * bi}", f"B{2 * bi + 1}"])

            def front_ops(t):
                bi = t % 2
                buf = xo[bi]
                bk = f"xo{bi}"
                o = 40 + bi * 20
                st6p = small[:, o:o + 12].rearrange("p (c s) -> p c s", c=2)
                mv1p = small[:, o + 12:o + 14]
                r1p = small[:, o + 14:o + 15]
                nmrp = small[:, o + 15:o + 16]
                s1p = small[:, o + 16:o + 17]
                k = f"ln{bi}"
                return [
                    lambda: P.dma("sp", lambda e: e.dma_start(out=buf, in_=x_own[t * 128:(t + 1) * 128, :]), f"ld_xo{bi}", writes=[bk]),
                    lambda: P.op("dve", lambda e: e.scalar_tensor_tensor(out=buf, in0=buf, scalar=ALPHA, in1=PB[bi][:], op0=ALU.mult, op1=ALU.add),
                                 reads=[bk, f"B{2 * bi}", f"B{2 * bi + 1}"], writes=[bk]),
                    lambda: P.op("dve", [lambda e, c=c: e.bn_stats(out=st6p[:, c, :], in_=buf[:, c * 512:(c + 1) * 512]) for c in range(2)],
                                 reads=[bk], writes=[k + "st"]),
                    lambda: P.op("dve", lambda e: e.bn_aggr(out=mv1p, in_=st6p.rearrange("p c s -> p (c s)")), reads=[k + "st"], writes=[k + "mv"]),
                    lambda: P.op("act", lambda e: e.activation(out=s1p, in_=mv1p[:, 1:2], func=AF.Sqrt, bias=epsT[:, 0:1]), reads=[k + "mv", "epsT"], writes=[k + "s1"]),
                    lambda: P.op("dve", lambda e: e.reciprocal(out=r1p, in_=s1p), reads=[k + "s1"], writes=[k + "r1"]),
                    lambda: P.op("dve", lambda e: e.scalar_tensor_tensor(out=nmrp, in0=mv1p[:, 0:1], scalar=-1.0, in1=r1p, op0=ALU.mult, op1=ALU.mult),
                                 reads=[k + "mv", k + "r1"], writes=[k + "nmr"]),
                    lambda: P.op("act", lambda e: e.activation(out=buf, in_=buf, func=AF.Identity, scale=r1p, bias=nmrp), reads=[bk, k + "r1", k + "nmr"], writes=[bk]),
                    lambda: P.op("dve", lambda e: e.tensor_tensor(out=buf, in0=buf, in1=lnw[:], op=ALU.mult), reads=[bk, "lnw"], writes=[bk]),
                    lambda: P.op("dve", lambda e: e.tensor_tensor(out=buf, in0=buf, in1=lnb[:], op=ALU.add), reads=[bk, "lnb"], writes=[bk]),
                ]

            def back_ops(t):
                bi = t % 2
                buf = xo[bi]
                bk = f"xo{bi}"
                bh, bl = 4 + 2 * bi, 5 + 2 * bi
                tph = bank_bf(bh).rearrange("p (k c) -> p k c", k=8)
                tpl = bank_bf(bl).rearrange("p (k c) -> p k c", k=8)
                lg = bank(bh)[:, 0:36]

                def router():
                    fl = []
                    for k in range(8):
                        fl.append(lambda e, k=k: e.matmul(lg, lhsT=x1T[:, k, t * 128:(t + 1) * 128], rhs=wr_hi[:, k, :], start=(k == 0), stop=False))
                        fl.append(lambda e, k=k: e.matmul(lg, lhsT=x1Tlo[:, k, :], rhs=wr_hi[:, k, :], start=False, stop=False))
                        fl.append(lambda e, k=k: e.matmul(lg, lhsT=x1T[:, k, t * 128:(t + 1) * 128], rhs=wr_lo[:, k, :], start=False, stop=(k == 7)))
                    P.op("pe", fl, reads=[f"x1T{t}", "x1Tlo", "wr_hi", "wr_lo"], writes=[f"B{bh}"])
                return [
                    lambda: P.op("act", lambda e: e.copy(out=hib, in_=buf), reads=[bk], writes=["hib"]),
                    lambda: P.op("act", lambda e: e.copy(out=ACC[:, t, :], in_=hib), reads=["hib"], writes=[f"acc{t}"]),
                    lambda: P.op("dve", lambda e: e.tensor_tensor(out=lob, in0=buf, in1=ACC[:, t, :], op=ALU.subtract), reads=[bk, f"acc{t}"], writes=["lob"]),
                    lambda: P.op("act", lambda e: e.activation(out=ACC[:, t, :], in_=buf, func=AF.Copy, scale=ALPHA), reads=[bk], writes=[f"acc{t}"]),
                    lambda: P.op("pe", [lambda e, k=k: e.transpose(out=tph[:, k, :], in_=hib[:, k * 128:(k + 1) * 128], identity=identb[:]) for k in range(8)],
                                 reads=["hib", "identb"], writes=[f"B{bh}"]),
                    lambda: P.op("pe", [lambda e, k=k: e.transpose(out=tpl[:, k, :], in_=lob[:, k * 128:(k + 1) * 128], identity=identb[:]) for k in range(8)],
                                 reads=["lob", "identb"], writes=[f"B{bl}"]),
                    lambda: P.op("act", lambda e: e.copy(out=x1T[:, :, t * 128:(t + 1) * 128], in_=tph), reads=[f"B{bh}"], writes=[f"x1T{t}"]),
                    lambda: P.op("dve", lambda e: e.tensor_copy(out=x1Tlo[:], in_=tpl), reads=[f"B{bl}"], writes=["x1Tlo"]),
                    router,
                    lambda: P.op("dve", lambda e: e.tensor_tensor(out=Lg[:, t, :], in0=lg[:, 0:4], in1=br32[:, 0:4], op=ALU.add), reads=[f"B{bh}", "br32"], writes=[f"Lg{t}"]),
                    lambda: P.op("dve", lambda e: e.tensor_tensor(out=gates[:, t, :], in0=lg[:, 4:36], in1=br32[:, 4:36], op=ALU.add), reads=[f"B{bh}", "br32"], writes=[f"gate{t}"]),
                ]

            attn_mm(0)
            attn_mm(1)
            for f in front_ops(0):
                f()
            FO = [0, 1, 2, 3, 4, 5, 6, 7, 8, 9]
            ORDER = [("f", 0), ("f", 1), ("b", 0), ("f", 2), ("b", 1), ("f", 3), ("f", 4), ("b", 2), ("f", 5), ("f", 6), ("b", 3),
                     ("f", 7), ("b", 4), ("b", 5), ("b", 6), ("f", 8), ("b", 7), ("b", 8), ("f", 9), ("b", 9), ("b", 10)]
            for t in range(NT):
                if t + 2 < NT:
                    attn_mm(t + 2)
                fl_ = front_ops(t + 1) if t + 1 < NT else None
                bl_ = back_ops(t)
                for kind, idx in ORDER:
                    if kind == "f":
                        if fl_ is not None:
                            fl_[idx]()
                    else:
                        bl_[idx]()
            P.barrier()
            GK = [f"gate{t}" for t in range(NT)]
            LK = [f"Lg{t}" for t in range(NT)]
            un = lambda o, n: UN[:, o:o + n]
            gmax = un(0, 16)
            gsum = un(16, 16)
            pgrp = un(32, 16)
            m1 = un(48, 16)
            m2 = un(64, 16)
            e2 = un(80, 16)
            w1 = un(96, 16)
            w2 = un(112, 16)
            Lgs = un(128, 64).rearrange("p (t g) -> p t g", t=NT)
            oh = un(192, 64).rearrange("p (t g) -> p t g", t=NT)
            ge = un(256, 64).rearrange("p (t g) -> p t g", t=NT)
            sel = un(320, 128).rearrange("p (t e) -> p t e", t=NT)
            sel2 = un(448, 128).rearrange("p (t e) -> p t e", t=NT)
            mk1 = un(576, 128).rearrange("p (t e) -> p t e", t=NT)
            mk2 = un(704, 128).rearrange("p (t e) -> p t e", t=NT)
            wg8 = un(832, 128).rearrange("p (t e) -> p t e", t=NT)
            E4 = gates[:].rearrange("p t (g e) -> p t g e", g=4)
            E4T = gates[:].rearrange("p t (g e) -> p t e g", g=4)
            bc3 = lambda a, n: a.unsqueeze(2).broadcast_to([128, NT, n])
            P.op("dve", lambda e: e.tensor_reduce(out=gmax, in_=Lg, axis=AX.X, op=ALU.max), reads=LK, writes=["gmax"])
            P.op("dve", lambda e: e.tensor_tensor(out=Lgs, in0=Lg, in1=bc3(gmax, 4), op=ALU.subtract), reads=LK + ["gmax"], writes=["Lgs"])
            P.op("dve", lambda e: e.tensor_scalar(out=oh, in0=Lgs, scalar1=0.0, scalar2=None, op0=ALU.is_equal), reads=["Lgs"], writes=["oh"])
            P.op("act", lambda e: e.activation(out=ge, in_=Lgs, func=AF.Exp), reads=["Lgs"], writes=["ge"])
            P.op("dve", lambda e: e.tensor_reduce(out=gsum, in_=ge, axis=AX.X, op=ALU.add), reads=["ge"], writes=["gsum"])
            P.op("dve", lambda e: e.reciprocal(out=pgrp, in_=gsum), reads=["gsum"], writes=["pgrp"])
            P.op("dve", lambda e: e.tensor_tensor(out=E4, in0=E4, in1=oh.unsqueeze(3).broadcast_to([128, NT, 4, 8]), op=ALU.mult), reads=GK + ["oh"], writes=GK)
            P.op("dve", lambda e: e.tensor_reduce(out=sel, in_=E4T, axis=AX.X, op=ALU.add), reads=GK, writes=["sel"])
            P.op("dve", lambda e: e.tensor_reduce(out=m1, in_=sel, axis=AX.X, op=ALU.max), reads=["sel"], writes=["m1"])
            P.op("dve", lambda e: e.tensor_tensor(out=mk1, in0=sel, in1=bc3(m1, 8), op=ALU.is_equal), reads=["sel", "m1"], writes=["mk1"])
            P.op("dve", lambda e: e.scalar_tensor_tensor(out=sel2, in0=mk1, scalar=-1.0e30, in1=sel, op0=ALU.mult, op1=ALU.add), reads=["mk1", "sel"], writes=["sel2"])
            P.op("dve", lambda e: e.tensor_reduce(out=m2, in_=sel2, axis=AX.X, op=ALU.max), reads=["sel2"], writes=["m2"])
            P.op("dve", lambda e: e.tensor_tensor(out=mk2, in0=sel2, in1=bc3(m2, 8), op=ALU.is_equal), reads=["sel2", "m2"], writes=["mk2"])
            P.op("dve", lambda e: e.tensor_tensor(out=e2, in0=m2, in1=m1, op=ALU.subtract), reads=["m1", "m2"], writes=["e2"])
            P.op("act", lambda e: e.activation(out=e2, in_=e2, func=AF.Exp), reads=["e2"], writes=["e2"])
            P.op("dve", lambda e: e.tensor_scalar(out=w2, in0=e2, scalar1=1.0, scalar2=None, op0=ALU.add), reads=["e2"], writes=["w2"])
            P.op("dve", lambda e: e.reciprocal(out=w1, in_=w2), reads=["w2"], writes=["w1"])
            P.op("dve", lambda e: e.tensor_tensor(out=w1, in0=w1, in1=pgrp, op=ALU.mult), reads=["w1", "pgrp"], writes=["w1"])
            P.op("dve", lambda e: e.tensor_tensor(out=w2, in0=e2, in1=w1, op=ALU.mult), reads=["e2", "w1", "w2"], writes=["w2"])
            P.op("dve", lambda e: e.tensor_tensor(out=mk1, in0=mk1, in1=bc3(w1, 8), op=ALU.mult), reads=["mk1", "w1"], writes=["mk1"])
            P.op("dve", lambda e: e.tensor_tensor(out=mk2, in0=mk2, in1=bc3(w2, 8), op=ALU.mult), reads=["mk2", "w2"], writes=["mk2"])
            P.op("dve", lambda e: e.tensor_tensor(out=wg8, in0=mk1, in1=mk2, op=ALU.add), reads=["mk1", "mk2"], writes=["wg8"])
            P.op("dve", lambda e: e.tensor_tensor(out=E4, in0=wg8.unsqueeze(2).broadcast_to([128, NT, 4, 8]),
                                                  in1=oh.unsqueeze(3).broadcast_to([128, NT, 4, 8]), op=ALU.mult), reads=["wg8", "oh"], writes=GK)
            W.release("wo0")
            W.release("wo1")
            P.barrier()
            _stop(6)
            if debug:
                P.dma("sp", lambda e: e.dma_start(out=dbg["acc"], in_=ACC[:]), "st_dbg", reads=[f"acc{t}" for t in range(NT)], is_store=True)
                P.dma("sp", lambda e: e.dma_start(out=dbg["gates"], in_=gates[:]), "st_dbg", reads=[f"gate{t}" for t in range(NT)], is_store=True)
                P.barrier()

            sa = [m_f32(i * 4096, 1024).rearrange("p (c n) -> p c n", c=2) for i in range(2)]
            hT = [m_bf16(16384 + i * 2048, 1024).rearrange("p (c n) -> p c n", c=2) for i in range(2)]
            X1T_ALL = [f"x1T{t}" for t in range(NT)]
            ld(lnw[:], ln2_w.partition_broadcast(128), "lnw")
            ld(lnb[:], ln2_b.partition_broadcast(128), "lnb")
            n_exp = NEXP if not MOE_LIMIT else MOE_LIMIT
            slots_of = {}

            def moe_ug(ei, g, sb_):
                gu_s, gu_k = W.get(f"egu{ei}")
                guv = gu_s[:].rearrange("p (k n) -> p k n", k=8)
                cols = slice(g * 512, (g + 1) * 512)
                for fc in range(2):
                    P.op("pe", [lambda e, k=k, fc=fc, cols=cols, guv=guv: e.matmul(bank(fc), lhsT=guv[:, k, fc * 128:(fc + 1) * 128], rhs=x1T[:, k, cols],
                                                                                   start=(k == 0), stop=(k == 7)) for k in range(8)],
                         reads=gu_k + X1T_ALL[g * 4:(g + 1) * 4], writes=[f"B{fc}"])
                    P.op("pe", [lambda e, k=k, fc=fc, cols=cols, guv=guv: e.matmul(bank(2 + fc), lhsT=guv[:, k, 256 + fc * 128:256 + (fc + 1) * 128], rhs=x1T[:, k, cols],
                                                                                   start=(k == 0), stop=(k == 7)) for k in range(8)],
                         reads=gu_k + X1T_ALL[g * 4:(g + 1) * 4], writes=[f"B{2 + fc}"])
                    P.op("act", lambda e, fc=fc, sb_=sb_: e.activation(out=sa[sb_][:, fc, :], in_=bank(fc), func=AF.Silu), reads=[f"B{fc}"], writes=[f"sa{sb_}{fc}"])
                    P.op("dve", lambda e, fc=fc, sb_=sb_: e.tensor_tensor(out=hT[sb_][:, fc, :], in0=bank(2 + fc), in1=sa[sb_][:, fc, :], op=ALU.mult),
                         reads=[f"B{2 + fc}", f"sa{sb_}{fc}"], writes=[f"hT{sb_}{fc}"])
                if g == NG - 1:
                    W.release(f"egu{ei}")

            def moe_d(ei, g, sb_):
                d_s, d_k = W.get(f"ed{ei}")
                dv = d_s[:, 0:2048].rearrange("p (k n) -> p k n", k=2)
                for tt in range(4):
                    t = g * 4 + tt
                    pb = 2 + (tt % 2)
                    fl = []
                    for hf in range(2):
                        for fc in range(2):
                            fl.append(lambda e, hf=hf, fc=fc, tt=tt, pb=pb, sb_=sb_, dv=dv: e.matmul(PB[pb][:, hf * 512:(hf + 1) * 512], lhsT=hT[sb_][:, fc, tt * 128:(tt + 1) * 128],
                                                                                                    rhs=dv[:, fc, hf * 512:(hf + 1) * 512], start=(fc == 0), stop=(fc == 1)))
                    P.op("pe", fl, reads=d_k + [f"hT{sb_}0", f"hT{sb_}1"], writes=[f"B{2 * pb}", f"B{2 * pb + 1}"])
                    P.op("dve", lambda e, t=t, pb=pb, ei=ei: e.scalar_tensor_tensor(out=ACC[:, t, :], in0=PB[pb][:], scalar=gates[:, t, ei:ei + 1], in1=ACC[:, t, :],
                                                                                   op0=ALU.mult, op1=ALU.add),
                         reads=[f"B{2 * pb}", f"B{2 * pb + 1}", f"gate{t}", f"acc{t}"], writes=[f"acc{t}"])
                    if ei == n_exp - 1:
                        bi = t % 2
                        o = xo[bi]
                        layer_norm(ACC[:, t, :], o, [f"acc{t}"], [f"xo{bi}"])
                        P.dma("sp", lambda e, t=t, o=o: e.dma_start(out=y_out[t * 128:(t + 1) * 128, :], in_=o), f"st_y{bi}", reads=[f"xo{bi}"], is_store=True)
                if g == NG - 1:
                    W.release(f"ed{ei}")

            def ug_pieces(ei, g, sb_):
                gu_s, gu_k = W.get(f"egu{ei}")
                guv = gu_s[:].rearrange("p (k n) -> p k n", k=8)
                cols = slice(g * 512, (g + 1) * 512)
                rk = gu_k + X1T_ALL[g * 4:(g + 1) * 4]
                pieces = []
                for fc in range(2):
                    def pa(fc=fc):
                        P.op("pe", [lambda e, k=k, fc=fc: e.matmul(bank(fc), lhsT=guv[:, k, fc * 128:(fc + 1) * 128], rhs=x1T[:, k, cols],
                                                                     start=(k == 0), stop=(k == 7)) for k in range(8)], reads=rk, writes=[f"B{fc}"])
                        P.op("act", lambda e, fc=fc: e.activation(out=sa[sb_][:, fc, :], in_=bank(fc), func=AF.Silu), reads=[f"B{fc}"], writes=[f"sa{sb_}{fc}"])

                    def pb_(fc=fc):
                        P.op("pe", [lambda e, k=k, fc=fc: e.matmul(bank(2 + fc), lhsT=guv[:, k, 256 + fc * 128:256 + (fc + 1) * 128], rhs=x1T[:, k, cols],
                                                                     start=(k == 0), stop=(k == 7)) for k in range(8)], reads=rk, writes=[f"B{2 + fc}"])
                        P.op("dve", lambda e, fc=fc: e.tensor_tensor(out=hT[sb_][:, fc, :], in0=bank(2 + fc), in1=sa[sb_][:, fc, :], op=ALU.mult),
                             reads=[f"B{2 + fc}", f"sa{sb_}{fc}"], writes=[f"hT{sb_}{fc}"])
                    pieces += [pa, pb_]
                return pieces

            def d_tile(ei, g, sb_, tt):
                d_s, d_k = W.get(f"ed{ei}")
                dv = d_s[:, 0:2048].rearrange("p (k n) -> p k n", k=2)
                t = g * 4 + tt
                pb = 2 + (tt % 2)
                fl = []
                for hf in range(2):
                    for fc in range(2):
                        fl.append(lambda e, hf=hf, fc=fc: e.matmul(PB[pb][:, hf * 512:(hf + 1) * 512], lhsT=hT[sb_][:, fc, tt * 128:(tt + 1) * 128],
                                                                  rhs=dv[:, fc, hf * 512:(hf + 1) * 512], start=(fc == 0), stop=(fc == 1)))
                P.op("pe", fl, reads=d_k + [f"hT{sb_}0", f"hT{sb_}1"], writes=[f"B{2 * pb}", f"B{2 * pb + 1}"])
                P.op("dve", lambda e: e.scalar_tensor_tensor(out=ACC[:, t, :], in0=PB[pb][:], scalar=gates[:, t, ei:ei + 1], in1=ACC[:, t, :],
                                                             op0=ALU.mult, op1=ALU.add),
                     reads=[f"B{2 * pb}", f"B{2 * pb + 1}", f"gate{t}", f"acc{t}"], writes=[f"acc{t}"])
                if ei == n_exp - 1:
                    bi = t % 2
                    o = xo[bi]
                    layer_norm(ACC[:, t, :], o, [f"acc{t}"], [f"xo{bi}"])
                    P.dma("sp", lambda e: e.dma_start(out=y_out[t * 128:(t + 1) * 128, :], in_=o), f"st_y{bi}", reads=[f"xo{bi}"], is_store=True)

            steps = [(ei, g) for ei in range(n_exp) for g in range(NG)]
            for i in range(len(steps) + 1):
                pieces = ug_pieces(steps[i][0], steps[i][1], i % 2) if i < len(steps) else [None] * 4
                for q in range(4):
                    if pieces[q] is not None:
                        pieces[q]()
                    if i > 0:
                        pe_, pg_ = steps[i - 1]
                        d_tile(pe_, pg_, (i - 1) % 2, q)
                if i < len(steps) and steps[i][1] == NG - 1:
                    W.release(f"egu{steps[i][0]}")
                if i > 0 and steps[i - 1][1] == NG - 1:
                    W.release(f"ed{steps[i - 1][0]}")
        except _StopBuild:
            P.dma("sp", lambda e: e.dma_start(out=y_out[0:128, 0:128], in_=identf[:]), "st_y0", reads=["identf"], is_store=True)
        P.finish()
        P.emit()
    return nc


MOE_LIMIT = 0
STOP = 0


class _StopBuild(Exception):
    pass


def _stop(n):
    if STOP == n:
        raise _StopBuild()

_NC_CACHE = {}


def _consts(hf):
    c = {}
    c["c_ident"] = np.eye(128, dtype=np.float32)
    inv_freq = (10000.0 ** (-np.arange(64, dtype=np.float32) / np.float32(64))).astype(np.float32)
    c["c_invf"] = np.tile((inv_freq.astype(np.float64) / (2 * np.pi)).astype(np.float32)[None, :], (128, 1))
    hh = np.arange(4, dtype=np.float64)
    lg = np.log1p(-np.exp2(-5.0 - hh))
    s = np.arange(128)[:, None]
    cc = np.arange(128)[None, :]
    maskT = np.zeros((128, 4, 128), np.float64)
    for h in range(4):
        maskT[:, h, :] = np.where(s <= cc, np.exp(lg[h] * (cc - 127.0)), 0.0) * np.ones((128, 1))
    c["c_maskT"] = maskT.astype(np.float32)
    qd = np.exp(lg[None, :] * (np.arange(128)[:, None] + 1.0))
    c["c_qdec"] = qd.astype(np.float32)
    kd = np.exp(lg[None, :] * (127.0 - np.arange(128)[:, None])) * (128.0 ** -0.5)
    c["c_kdec"] = kd.astype(np.float32)
    ic = np.zeros((4, 16), np.float64)
    for gi, w in enumerate((2, 4, 8, 16)):
        for t in range(16):
            ic[gi, t] = 1.0 / (min(t + 1, w) if hf == 0 else w)
    c["c_invcnt"] = np.tile(ic[None, :, :], (128, 1, 1)).astype(np.float32)
    return c


def kernel(x, mem, positions, w_in, w_pool_grp, pool_scale, ret_gn_w, w_mem_kv, w_br_pool, w_br_ret,
           w_br_xa, w_out, ln1_w, ln1_b, w_grp_router, b_grp_router, w_exp_router, b_exp_router,
           w_exp_gate, w_exp_up, w_exp_down, ln2_w, ln2_b, _debug=False):
    f = lambda a: np.ascontiguousarray(np.asarray(a, dtype=np.float32))
    x = f(x)
    mem = f(mem)
    positions = np.asarray(positions, dtype=np.int32)
    shared = {
        "w_in": f(w_in)[0], "w_pool_grp": f(w_pool_grp)[0],
        "pool_scale": np.ascontiguousarray(f(pool_scale)[0].reshape(4, 128).T),
        "ret_gn_w": f(ret_gn_w)[0].reshape(1024), "w_mem_kv": f(w_mem_kv)[0], "w_br_pool": f(w_br_pool)[0],
        "w_br_ret": f(w_br_ret)[0], "w_br_xa": f(w_br_xa)[0], "w_out": f(w_out)[0],
        "ln1_w": f(ln1_w)[0], "ln1_b": f(ln1_b)[0], "ln2_w": f(ln2_w)[0], "ln2_b": f(ln2_b)[0],
        "w_router": np.ascontiguousarray(np.concatenate([f(w_grp_router)[0], f(w_exp_router)[0]], axis=1)),
        "b_router": np.ascontiguousarray(np.concatenate([f(b_grp_router)[0], f(b_exp_router)[0]], axis=0)[None, :]),
        "w_exp_gate": f(w_exp_gate)[0].reshape(32, 1024, 256), "w_exp_up": f(w_exp_up)[0].reshape(32, 1024, 256),
        "w_exp_down": f(w_exp_down)[0].reshape(32, 256, 1024),
    }
    key = bool(_debug)
    if key not in _NC_CACHE:
        _NC_CACHE[key] = build_program(debug=_debug)
    nc = _NC_CACHE[key]
    in_maps = []
    for c in range(8):
        b, hf = c // 2, c % 2
        m = dict(shared)
        m.update(_consts(hf))
        m["x_own"] = np.ascontiguousarray(x[b, hf * T:(hf + 1) * T])
        m["x_prev"] = np.ascontiguousarray(x[b, 0:T]) if hf == 1 else np.zeros((T, D), np.float32)
        po = positions[b, hf * T:(hf + 1) * T].reshape(NT, 128).T
        pp = positions[b, 0:T].reshape(NT, 128).T if hf == 1 else np.zeros((128, NT), np.int32)
        m["pos_own"] = np.ascontiguousarray(po)
        m["pos_prev"] = np.ascontiguousarray(pp)
        m["mem"] = np.ascontiguousarray(mem[b])
        in_maps.append(m)
    res = run_bass_kernel_spmd(nc, in_maps, core_ids=list(range(8)))
    out = np.zeros((4, SEQ, D), np.float32)
    for c in range(8):
        b, hf = c // 2, c % 2
        out[b, hf * T:(hf + 1) * T] = res.results[c]["y_out"]
    if _debug:
        return out, res
    return out
```

```python
from contextlib import ExitStack
import numpy as np
import concourse.bass as bass
import concourse.mybir as mybir
from concourse.bass_utils import run_bass_kernel_spmd

F32 = mybir.dt.float32
BF16 = mybir.dt.bfloat16
I32 = mybir.dt.int32
ALU = mybir.AluOpType
AF = mybir.ActivationFunctionType
AX = mybir.AxisListType

D = 1024
SEQ = 4096
T = 2048
NT = 16
NG = 4
C = 128
H = 4
DV = 256
NEXP = 32
LN_EPS = 1e-5
ALPHA = 2.0 ** 0.25
NSLOT = 6
ENGS = ("pe", "act", "dve", "pool", "sp")


class Prog:
    def __init__(self, nc, es):
        self.nc = nc
        self.es = es
        self.streams = {e: [] for e in ENGS}
        self.sems = {}
        self.cnt = {}
        self.waited = {e: {} for e in ENGS}
        self.res = {}
        self.store_events = []
        for e in ("pe", "act", "dve", "pool"):
            self._sem(e)

    def _sem(self, name):
        if name not in self.sems:
            self.sems[name] = self.es.enter_context(self.nc.semaphore("s_" + str(name)))
            self.cnt[name] = 0
        return self.sems[name]

    def _deps(self, eng, reads, writes):
        deps = {}

        def add(ev):
            if ev is None:
                return
            s, v = ev
            if deps.get(s, 0) < v:
                deps[s] = v

        for r in reads:
            st = self.res.get(r)
            if st:
                add(st["w"])
        for w in writes:
            st = self.res.get(w)
            if st:
                add(st["w"])
                for ev in st["r"]:
                    add(ev)
        for s, v in deps.items():
            if eng == "pe" and s == "pe":
                continue
            if self.waited[eng].get(s, 0) < v:
                self.waited[eng][s] = v
                sem = self.sems[s]
                self.streams[eng].append(lambda e, sem=sem, v=v: e.wait_ge(sem, v))

    def _mark(self, ev, reads, writes):
        for r in reads:
            st = self.res.setdefault(r, {"w": None, "r": []})
            st["r"].append(ev)
            if len(st["r"]) > 64:
                best = {}
                for s, v in st["r"]:
                    best[s] = max(best.get(s, 0), v)
                st["r"] = list(best.items())
        for w in writes:
            self.res[w] = {"w": ev, "r": []}

    def op(self, eng, fns, reads=(), writes=()):
        if callable(fns):
            fns = [fns]
        self._deps(eng, reads, writes)
        self.cnt[eng] += 1
        ev = (eng, self.cnt[eng])
        sem = self.sems[eng]
        st = self.streams[eng]
        for f in fns[:-1]:
            st.append(f)
        last = fns[-1]
        st.append(lambda e, last=last, sem=sem: last(e).then_inc(sem, 1))
        self._mark(ev, reads, writes)
        return ev

    def dma(self, queue, fn, semkey, reads=(), writes=(), is_store=False):
        self._deps(queue, reads, writes)
        sem = self._sem(semkey)
        self.cnt[semkey] += 16
        ev = (semkey, self.cnt[semkey])
        self.streams[queue].append(lambda e, fn=fn, sem=sem: fn(e).then_inc(sem, 16))
        self._mark(ev, reads, writes)
        if is_store:
            self.store_events.append(ev)
        return ev

    def barrier(self, engs=("pe", "act", "dve", "sp")):
        for eng in engs:
            for sname, v in self.cnt.items():
                if v == 0 or str(sname).startswith("w"):
                    continue
                if eng == "pe" and sname == "pe":
                    continue
                if self.waited[eng].get(sname, 0) < v:
                    self.waited[eng][sname] = v
                    sem = self.sems[sname]
                    self.streams[eng].append(lambda e, sem=sem, v=v: e.wait_ge(sem, v))

    def finish(self):
        for s, v in self.cnt.items():
            if s in ("pe", "act", "dve", "pool") or v == 0:
                continue
            if self.waited["sp"].get(s, 0) < v:
                self.waited["sp"][s] = v
                sem = self.sems[s]
                self.streams["sp"].append(lambda e, sem=sem, v=v: e.wait_ge(sem, v))

    def emit(self):
        nc = self.nc
        with nc.Block() as block:
            @block.tensor
            def _(e):
                for f in self.streams["pe"]:
                    f(e)

            @block.scalar
            def _(e):
                for f in self.streams["act"]:
                    f(e)

            @block.vector
            def _(e):
                for f in self.streams["dve"]:
                    f(e)

            @block.gpsimd
            def _(e):
                for f in self.streams["pool"]:
                    f(e)

            @block.sync
            def _(e):
                for f in self.streams["sp"]:
                    f(e)


class WStream:
    def __init__(self, P, slots, items, n_init=None):
        self.P = P
        self.slots = slots
        self.items = items
        self.idx = {it[0]: i for i, it in enumerate(items)}
        self.emitted = 0
        self.rel = set()
        self.wm = 0
        for _ in range(min(n_init if n_init is not None else len(slots), len(items))):
            self._emit_next()

    def kick(self, after_keys=()):
        self.P._deps("pool", tuple(after_keys), ())
        while self.emitted < self.wm + len(self.slots) and self.emitted < len(self.items):
            self._emit_next()

    def _emit_next(self):
        i = self.emitted
        if i >= len(self.items):
            return
        P = self.P
        s = i % len(self.slots)
        slot = self.slots[s]
        allkeys = [f"ring{s}", f"ring{s}a", f"ring{s}b"]
        P._deps("pool", (), allkeys)
        for k in allkeys:
            P.res[k] = {"w": None, "r": []}
        for sub, fn in self.items[i][1]:
            semkey = f"w{s}{sub}"
            sem = P._sem(semkey)
            P.cnt[semkey] += 16
            ev = (semkey, P.cnt[semkey])
            P.streams["pool"].append(lambda e, fn=fn, slot=slot, sem=sem: fn(e, slot).then_inc(sem, 16))
            P.res[f"ring{s}{sub}"] = {"w": ev, "r": []}
        self.emitted += 1

    def get(self, name):
        i = self.idx[name]
        assert i < self.emitted, f"weight item {name} not yet emitted"
        s = i % len(self.slots)
        keys = [f"ring{s}{sub}" for sub, _ in self.items[i][1]]
        return self.slots[s], keys

    def release(self, name):
        i = self.idx[name]
        self.rel.add(i)
        while self.wm in self.rel:
            self.wm += 1
        while self.emitted < self.wm + len(self.slots) and self.emitted < len(self.items):
            self._emit_next()


def build_program(debug=False):
    nc = bass.Bass("TRN2", target_bir_lowering=False)

    def din(name, shape, dt=F32):
        return nc.dram_tensor(name, list(shape), dt, kind="ExternalInput").ap()

    x_own = din("x_own", [T, D])
    x_prev = din("x_prev", [T, D])
    pos_own = din("pos_own", [128, NT], I32)
    pos_prev = din("pos_prev", [128, NT], I32)
    mem = din("mem", [256, D])
    w_in = din("w_in", [D, 7168])
    w_pool_grp = din("w_pool_grp", [4, 128, 128])
    pool_scale = din("pool_scale", [128, 4])
    ret_gn_w = din("ret_gn_w", [1024])
    w_mem_kv = din("w_mem_kv", [D, 1024])
    w_br_pool = din("w_br_pool", [512, D])
    w_br_ret = din("w_br_ret", [1024, D])
    w_br_xa = din("w_br_xa", [512, D])
    w_out = din("w_out", [D, D])
    ln1_w = din("ln1_w", [D])
    ln1_b = din("ln1_b", [D])
    ln2_w = din("ln2_w", [D])
    ln2_b = din("ln2_b", [D])
    w_router = din("w_router", [D, 36])
    b_router = din("b_router", [1, 36])
    w_exp_gate = din("w_exp_gate", [NEXP, D, 256])
    w_exp_up = din("w_exp_up", [NEXP, D, 256])
    w_exp_down = din("w_exp_down", [NEXP, 256, D])
    c_ident = din("c_ident", [128, 128])
    c_invf = din("c_invf", [128, 64])
    c_maskT = din("c_maskT", [128, 4, 128])
    c_qdec = din("c_qdec", [128, 4])
    c_kdec = din("c_kdec", [128, 4])
    c_invcnt = din("c_invcnt", [128, 4, 16])
    y_out = nc.dram_tensor("y_out", [T, D], F32, kind="ExternalOutput").ap()
    dbg = {}
    if debug:
        dbg["mT"] = nc.dram_tensor("dbg_mT", [128, 8, T], BF16, kind="ExternalOutput").ap()
        dbg["acc"] = nc.dram_tensor("dbg_acc", [128, NT, D], F32, kind="ExternalOutput").ap()
        dbg["gates"] = nc.dram_tensor("dbg_gates", [128, NT, 32], F32, kind="ExternalOutput").ap()
        dbg["z"] = nc.dram_tensor("dbg_z", [128, 8, T], BF16, kind="ExternalOutput").ap()

    with ExitStack() as es:
        P = Prog(nc, es)

        def sb(name, shape, dt):
            return es.enter_context(nc.sbuf_tensor(name, list(shape), dt))

        A = sb("A", [128, 8, T], BF16)
        M = sb("M", [128, 8, T], BF16)
        ACC = sb("ACC", [128, NT, D], F32)
        ring = [sb(f"ring{i}", [128, 4096], BF16) for i in range(NSLOT)]
        UN = sb("UN", [128, 3072], F32)
        lnw = sb("lnw", [128, D], F32)
        lnb = sb("lnb", [128, D], F32)
        identf = sb("identf", [128, 128], F32)
        identb = sb("identb", [128, 128], BF16)
        invf = sb("invf", [128, 64], F32)
        kdec = sb("kdec", [128, 4], F32)
        invcnt = sb("invcnt", [128, 4, 16], F32)
        pscale = sb("pscale", [128, 4], F32)
        wr32 = sb("wr32", [128, 8, 36], F32)
        br32 = sb("br32", [128, 36], F32)
        wr_hi = sb("wr_hi", [128, 8, 36], BF16)
        wr_lo = sb("wr_lo", [128, 8, 36], BF16)
        x1Tlo = sb("x1Tlo", [128, 8, 128], BF16)
        onesb = sb("onesb", [128, 128], BF16)
        wgrp_b = sb("wgrp_b", [128, 4, 128], BF16)
        gates = sb("gates", [128, NT, 32], F32)
        epsT = sb("epsT", [128, 1], F32)
        small = sb("small", [128, 256], F32)
        xTh = sb("xTh", [128, 8, 16], BF16)
        posi = sb("posi", [128, 2, NT], I32)
        posf = sb("posf", [128, 2, NT], F32)

        S = UN[:, 0:1024].rearrange("p (h c) -> p h c", h=4)
        S_bf = UN[:, 1024:1536].bitcast(BF16).rearrange("p (h c) -> p h c", h=4)
        maskT = UN[:, 1536:2048].rearrange("p (h c) -> p h c", h=4)
        qdec = UN[:, 2048:2052]
        qrd = UN[:, 2056:2312].bitcast(BF16).rearrange("p (h c) -> p h c", h=4)
        xo = [UN[:, 0:1024], UN[:, 1024:2048]]
        hib = UN[:, 2048:2560].bitcast(BF16)
        lob = UN[:, 2560:3072].bitcast(BF16)

        accb = ACC[:].rearrange("p t d -> p (t d)")
        accbf = accb.bitcast(BF16)

        def acc_f32(off_bytes, n):
            assert off_bytes % 4 == 0 and off_bytes + 4 * n <= 65536
            return accb[:, off_bytes // 4: off_bytes // 4 + n]

        def acc_bf16(off_bytes, n):
            assert off_bytes % 2 == 0 and off_bytes + 2 * n <= 65536
            return accbf[:, off_bytes // 2: off_bytes // 2 + n]

        Z = acc_bf16(0, 8 * T).rearrange("p (k t) -> p k t", k=8)
        SC0 = 32768
        Mb = M[:].rearrange("p k t -> p (k t)")
        Mf = Mb.bitcast(F32)

        def m_f32(off_bytes, n):
            assert off_bytes % 4 == 0 and off_bytes + 4 * n <= 32768
            return Mf[:, off_bytes // 4: off_bytes // 4 + n]

        def m_bf16(off_bytes, n):
            assert off_bytes % 2 == 0 and off_bytes + 2 * n <= 32768
            return Mb[:, off_bytes // 2: off_bytes // 2 + n]

        PB = [es.enter_context(nc.psum_tensor(f"pb{i}", [128, 1024], F32)) for i in range(4)]

        def bank(i):
            return PB[i // 2][:, (i % 2) * 512:(i % 2) * 512 + 512]

        def bank_bf(i):
            return bank(i).bitcast(BF16)

        def col_chunk(w, col0):
            def f(e, slot):
                return e.dma_start(out=slot[:].rearrange("p (k n) -> p k n", k=8),
                                   in_=w[:, col0:col0 + 512].rearrange("(k p) n -> p k n", p=128))
            return f

        def mat_rows4(w):
            def f(e, slot):
                return e.dma_start(out=slot[:].rearrange("p (k n) -> p k n", k=4),
                                   in_=w.rearrange("(k p) n -> p k n", p=128))
            return f

        def exp_gu(w, ei, c0):
            def f(e, slot):
                return e.dma_start(out=slot[:].rearrange("p (k n) -> p k n", k=8)[:, :, c0:c0 + 256],
                                   in_=w[ei].rearrange("(k p) n -> p k n", p=128))
            return f

        def exp_d(ei):
            def f(e, slot):
                return e.dma_start(out=slot[:, 0:2048].rearrange("p (k n) -> p k n", k=2),
                                   in_=w_exp_down[ei].rearrange("(k p) n -> p k n", p=128))
            return f

        COL_POOL, COL_Q, COL_K, COL_V, COL_G, COL_XQ, COL_GATE = 0, 512, 1024, 1536, 2560, 3584, 4096
        items = [
            ("wk", [("", col_chunk(w_in, COL_K))]),
            ("wv0", [("", col_chunk(w_in, COL_V))]),
            ("wv1", [("", col_chunk(w_in, COL_V + 512))]),
            ("wq", [("", col_chunk(w_in, COL_Q))]),
            ("wg0", [("", col_chunk(w_in, COL_G))]),
            ("wg1", [("", col_chunk(w_in, COL_G + 512))]),
            ("gt1a", [("", col_chunk(w_in, COL_GATE + 1024))]),
            ("gt1b", [("", col_chunk(w_in, COL_GATE + 1536))]),
            ("wbr0", [("", col_chunk(w_br_ret, 0))]),
            ("wbr1", [("", col_chunk(w_br_ret, 512))]),
            ("wpool", [("", col_chunk(w_in, COL_POOL))]),
            ("gt0a", [("", col_chunk(w_in, COL_GATE))]),
            ("gt0b", [("", col_chunk(w_in, COL_GATE + 512))]),
            ("wbp", [("", mat_rows4(w_br_pool))]),
            ("wmk", [("", col_chunk(w_mem_kv, 0))]),
            ("wmv", [("", col_chunk(w_mem_kv, 512))]),
            ("wxq", [("", col_chunk(w_in, COL_XQ))]),
            ("gt2a", [("", col_chunk(w_in, COL_GATE + 2048))]),
            ("gt2b", [("", col_chunk(w_in, COL_GATE + 2560))]),
            ("wbx", [("", mat_rows4(w_br_xa))]),
            ("wo0", [("", col_chunk(w_out, 0))]),
            ("wo1", [("", col_chunk(w_out, 512))]),
        ]
        for ei in range(NEXP):
            items.append((f"egu{ei}", [("a", exp_gu(w_exp_gate, ei, 0)), ("b", exp_gu(w_exp_up, ei, 256))]))
            items.append((f"ed{ei}", [("", exp_d(ei))]))

        def ld(dst, src, key):
            P.dma("sp", lambda e: e.dma_start(out=dst, in_=src), "ld_" + key, writes=[key])

        wgrp_f = acc_f32(53248, 512).rearrange("p (g d) -> p g d", g=4)
        ld(identf[:], c_ident, "identf")
        pre_thunks = [
            lambda: ld(posi[:, 0, :], pos_own, "posi0"),
            lambda: ld(posi[:, 1, :], pos_prev, "posi1"),
            lambda: ld(invf[:], c_invf, "invf"),
            lambda: ld(kdec[:], c_kdec, "kdec"),
            lambda: ld(maskT, c_maskT, "maskT"),
            lambda: ld(qdec, c_qdec, "qdec"),
            lambda: ld(invcnt[:], c_invcnt, "invcnt"),
            lambda: ld(pscale[:], pool_scale, "pscale"),
            lambda: ld(wr32[:], w_router.rearrange("(k p) n -> p k n", p=128), "wr32"),
            lambda: ld(br32[:], b_router[0].partition_broadcast(128), "br32"),
            lambda: ld(wgrp_f, w_pool_grp.rearrange("g c d -> c g d"), "wgrp_f"),
        ]

        W = WStream(P, ring, items, n_init=3)
        try:

            P.op("dve", lambda e: e.tensor_copy(out=identb[:], in_=identf[:]), reads=["identf"], writes=["identb"])
            wr_hif = acc_f32(55296, 288).rearrange("p (k n) -> p k n", k=8)
            post_thunks = [
                lambda: P.op("dve", lambda e: e.memset(S, 0.0), writes=["S"]),
                lambda: P.op("dve", lambda e: e.memset(S_bf, 0.0), writes=["S_bf"]),
                lambda: P.op("dve", lambda e: e.memset(onesb[:], 1.0), writes=["onesb"]),
                lambda: P.op("dve", lambda e: e.memset(epsT[:], LN_EPS), writes=["epsT"]),
                lambda: P.op("dve", lambda e: e.tensor_copy(out=wr_hi[:], in_=wr32[:]), reads=["wr32"], writes=["wr_hi"]),
                lambda: P.op("dve", lambda e: e.tensor_copy(out=wr_hif, in_=wr_hi[:]), reads=["wr_hi"], writes=["wr_hif"]),
                lambda: P.op("dve", lambda e: e.tensor_tensor(out=wr_lo[:], in0=wr32[:], in1=wr_hif, op=ALU.subtract), reads=["wr32", "wr_hif"], writes=["wr_lo"]),
                lambda: P.op("dve", lambda e: e.tensor_copy(out=wgrp_b[:], in_=wgrp_f), reads=["wgrp_f"], writes=["wgrp_b"]),
            ]

            posf_thunk = lambda: P.op("dve", lambda e: e.tensor_copy(out=posf[:], in_=posi[:]), reads=["posi0", "posi1"], writes=["posf"])
            tab_own = m_f32(0, NT * 192).rearrange("p (t c) -> p t c", t=NT)
            tab_prev = acc_f32(SC0, NT * 192).rearrange("p (t c) -> p t c", t=NT)
            tmpA = acc_f32(SC0 + 12288, NT * 128).rearrange("p (t c) -> p t c", t=NT)
            tmpI = m_f32(12288, NT * 128).bitcast(I32).rearrange("p (t c) -> p t c", t=NT)
            tab_thunks = []
            for which, tab, key in ((1, tab_prev, "tab_prev"), (0, tab_own, "tab_own")):
                pz = posf[:, which, :]
                tab_thunks.append(lambda pz=pz: P.op("dve", lambda e: e.tensor_tensor(
                    out=tmpA[:, :, 0:64], in0=pz.unsqueeze(2).broadcast_to([128, NT, 64]),
                    in1=invf[:].unsqueeze(1).broadcast_to([128, NT, 64]), op=ALU.mult),
                    reads=["posf", "invf"], writes=["tmpA"]))
                tab_thunks.append(lambda: P.op("dve", lambda e: e.tensor_scalar(out=tmpA[:, :, 64:128], in0=tmpA[:, :, 0:64], scalar1=0.25, scalar2=None, op0=ALU.add),
                                               reads=["tmpA"], writes=["tmpA"]))
                tab_thunks.append(lambda: P.op("dve", lambda e: e.tensor_copy(out=tmpI, in_=tmpA), reads=["tmpA"], writes=["tmpI"]))
                tab_thunks.append(lambda: P.op("dve", lambda e: e.tensor_tensor(out=tmpA, in0=tmpA, in1=tmpI, op=ALU.subtract), reads=["tmpA", "tmpI"], writes=["tmpA"]))
                tab_thunks.append(lambda: P.op("dve", lambda e: e.scalar_tensor_tensor(out=tmpA, in0=tmpA, scalar=0.5, in1=tmpA, op0=ALU.is_gt, op1=ALU.subtract),
                                               reads=["tmpA"], writes=["tmpA"]))
                tab_thunks.append(lambda tab=tab, key=key: P.op("act", lambda e: e.activation(out=tab[:, :, 0:128], in_=tmpA, func=AF.Sin, scale=float(-2.0 * np.pi)),
                                                               reads=["tmpA"], writes=[key]))
                tab_thunks.append(lambda tab=tab, key=key: P.op("act", lambda e: e.copy(out=tab[:, :, 128:192], in_=tab[:, :, 0:64]), reads=[key], writes=[key]))

            tab_thunks[:] = pre_thunks[0:3] + [posf_thunk] + tab_thunks + pre_thunks[3:] + post_thunks

            def drip():
                if tab_thunks:
                    tab_thunks.pop(0)()

            def load_transposed(src, dstT, ntiles, tag, xs_l, xb_l, halo=False, drip_on=False):
                for t in range(ntiles):
                    bs = t % len(xs_l)
                    b = t % len(xb_l)
                    P.dma("sp", lambda e, t=t, bs=bs: e.dma_start(out=xs_l[bs], in_=src[t * 128:(t + 1) * 128, :]), f"ld_x{bs}", writes=[f"xs{bs}"])
                    P.op("act", lambda e, b=b, bs=bs: e.copy(out=xb_l[b], in_=xs_l[bs]), reads=[f"xs{bs}"], writes=[f"xb{b}"])
                    pb_ = 6 + (t % 2)
                    pt = bank_bf(pb_).rearrange("p (k t) -> p k t", k=8)
                    P.op("pe", [lambda e, k=k, b=b, pt=pt: e.transpose(out=pt[:, k, :], in_=xb_l[b][:, k * 128:(k + 1) * 128], identity=identb[:])
                                for k in range(8)], reads=[f"xb{b}", "identb"], writes=[f"B{pb_}"])
                    P.op("dve", lambda e, t=t, pt=pt: e.tensor_copy(out=dstT[:, :, t * 128:(t + 1) * 128], in_=pt),
                         reads=[f"B{pb_}"], writes=[f"{tag}{t}"])
                    if halo and t == ntiles - 1:
                        P.op("dve", lambda e, pt=pt: e.tensor_copy(out=xTh[:], in_=pt[:, :, 112:128]), reads=[f"B{pb_}"], writes=["xTh"])
                    if drip_on:
                        drip()

            xT = A[:]
            xTp = Z
            xsA = [acc_f32(57344, 1024), acc_f32(61440, 1024), m_f32(20480, 1024), m_f32(28672, 1024)]
            xbA = [m_bf16(24576, 1024), m_bf16(26624, 1024)]
            load_transposed(x_prev, xTp, NT, "xTp", xsA, xbA, halo=True, drip_on=True)
            load_transposed(x_own, xT, NT, "xT", xsA, xbA, drip_on=True)
            W.kick(after_keys=["xs0", "xs1", "xs2", "xs3"])
            while tab_thunks:
                drip()
            XT_ALL = [f"xT{t}" for t in range(NT)]
            P.barrier()
            _stop(1)

            GAM = [1.0 - 2.0 ** (-5.0 - h) for h in range(H)]
            GAMC = [float(g ** C) for g in GAM]
            h4 = lambda ap: ap.rearrange("p (h c) -> p h c", h=4)
            t12 = h4(m_f32(12288, 512))
            t34 = h4(m_f32(14336, 512))
            q_rot = h4(m_bf16(16384, 512))
            k_rot = h4(m_bf16(17408, 512))
            vd = h4(m_bf16(18432, 1024))
            qT = h4(m_bf16(20480, 512))
            qTd = h4(m_bf16(21504, 512))
            kT = h4(m_bf16(22528, 512))
            smk = h4(m_bf16(23552, 512))
            sg = h4(m_f32(24576, 1024))
            gnw = h4(acc_f32(SC0 + 12288, 1024))
            yn = h4(acc_f32(SC0 + 16384, 1024))
            yret = acc_bf16(SC0 + 20480, 1024)
            stt = small[:, 0:24].rearrange("p (h s) -> p h s", h=4)
            mv = small[:, 24:32].rearrange("p (h s) -> p h s", h=4)
            rstd = small[:, 32:36]
            sq = small[:, 36:40]

            P.dma("sp", lambda e: e.dma_start(out=gnw.rearrange("p h c -> p (h c)"), in_=ret_gn_w.partition_broadcast(128)), "ld_gnw", writes=["gnw"])

            def rot(ps_bank, tab, t, dst, dst_key, tab_key):
                src = h4(bank(ps_bank))
                cs1 = tab[:, t, 64:192].unsqueeze(1).broadcast_to([128, 4, 128])
                cs2 = tab[:, t, 0:128].unsqueeze(1).broadcast_to([128, 4, 128])
                P.op("dve", lambda e: e.tensor_tensor(out=t12, in0=src, in1=cs1, op=ALU.mult), reads=[f"B{ps_bank}", tab_key], writes=["t12"])
                P.op("dve", lambda e: e.tensor_tensor(out=t34, in0=src, in1=cs2, op=ALU.mult), reads=[f"B{ps_bank}", tab_key], writes=["t34"])
                P.op("dve", lambda e: e.tensor_tensor(out=dst[:, :, 0:64], in0=t12[:, :, 0:64], in1=t12[:, :, 64:128], op=ALU.subtract),
                     reads=["t12"], writes=[dst_key + "a"])
                P.op("dve", lambda e: e.tensor_tensor(out=dst[:, :, 64:128], in0=t34[:, :, 0:64], in1=t34[:, :, 64:128], op=ALU.add),
                     reads=["t34"], writes=[dst_key + "b"])

            def proj_tok(ps_bank, srcT, t, wname, src_keys):
                slot, keys = W.get(wname)
                wv = slot[:].rearrange("p (k n) -> p k n", k=8)
                P.op("pe", [lambda e, k=k: e.matmul(bank(ps_bank), lhsT=srcT[:, k, t * 128:(t + 1) * 128], rhs=wv[:, k, :],
                                                    start=(k == 0), stop=(k == 7)) for k in range(8)],
                     reads=keys + src_keys, writes=[f"B{ps_bank}"])

            def ret_chunk(srcT, src_tag, t, tab, tab_key, full):
                skeys = [f"{src_tag}{t}"]
                proj_tok(1, srcT, t, "wk", skeys)
                proj_tok(2, srcT, t, "wv0", skeys)
                proj_tok(3, srcT, t, "wv1", skeys)
                if full:
                    proj_tok(0, srcT, t, "wq", skeys)
                    proj_tok(4, srcT, t, "wg0", skeys)
                    proj_tok(5, srcT, t, "wg1", skeys)
                rot(1, tab, t, k_rot, "k_rot", tab_key)
                vps = h4(PB[1][:])
                P.op("act", [lambda e, h=h: e.activation(out=vd[:, h, :], in_=vps[:, h, :], func=AF.Identity, scale=kdec[:, h:h + 1]) for h in range(4)],
                     reads=["B2", "B3", "kdec"], writes=["vd"])
                if full:
                    rot(0, tab, t, q_rot, "q_rot", tab_key)
                    P.op("act", lambda e: e.activation(out=sg.rearrange("p h c -> p (h c)"), in_=PB[2][:], func=AF.Silu),
                         reads=["B4", "B5"], writes=["sg"])
                    if t == 0: _stop(13)
                    tp = bank_bf(6).rearrange("p (k c) -> p k c", k=8)
                    P.op("pe", [lambda e, h=h: e.transpose(out=tp[:, h, :], in_=q_rot[:, h, :], identity=identb[:]) for h in range(4)] +
                         [lambda e, h=h: e.transpose(out=tp[:, 4 + h, :], in_=k_rot[:, h, :], identity=identb[:]) for h in range(4)],
                         reads=["q_rota", "q_rotb", "k_rota", "k_rotb", "identb"], writes=["B6"])
                    if t == 0: _stop(141)
                    P.op("act", lambda e: e.copy(out=qT, in_=tp[:, 0:4, :]), reads=["B6"], writes=["qT"])
                    if t == 0: _stop(142)
                    P.op("act", [lambda e, h=h: e.activation(out=qrd[:, h, :], in_=q_rot[:, h, :], func=AF.Identity, scale=qdec[:, h:h + 1]) for h in range(4)],
                         reads=["q_rota", "q_rotb", "qdec"], writes=["qrd"])
                    tpd = bank_bf(7)[:, 0:512].rearrange("p (k c) -> p k c", k=4)
                    P.op("pe", [lambda e, h=h: e.transpose(out=tpd[:, h, :], in_=qrd[:, h, :], identity=identb[:]) for h in range(4)],
                         reads=["qrd", "identb"], writes=["B7"])
                    P.op("act", lambda e: e.copy(out=qTd, in_=tpd), reads=["B7"], writes=["qTd"])
                    if t == 0: _stop(143)
                    P.op("act", lambda e: e.copy(out=kT, in_=tp[:, 4:8, :]), reads=["B6"], writes=["kT"])
                    if t == 0: _stop(14)
                    sc = h4(bank(7))
                    P.op("pe", [lambda e, h=h: e.matmul(sc[:, h, :], lhsT=kT[:, h, :], rhs=qT[:, h, :], start=True, stop=True) for h in range(4)],
                         reads=["kT", "qT"], writes=["B7"])
                    P.op("dve", lambda e: e.tensor_tensor(out=smk, in0=sc, in1=maskT, op=ALU.mult), reads=["B7", "maskT"], writes=["smk"])
                    yps = h4(PB[0][:])
                    fl = []
                    for h in range(4):
                        fl.append(lambda e, h=h: e.matmul(yps[:, h, :], lhsT=smk[:, h, :], rhs=vd[:, h, :], start=True, stop=False))
                        fl.append(lambda e, h=h: e.matmul(yps[:, h, :], lhsT=qTd[:, h, :], rhs=S_bf[:, h, :], start=False, stop=True))
                    P.op("pe", fl, reads=["smk", "vd", "qTd", "S_bf"], writes=["B0", "B1"])
                    if t == 0: _stop(15)
                kvps = h4(PB[1][:])
                P.op("pe", [lambda e, h=h: e.matmul(kvps[:, h, :], lhsT=k_rot[:, h, :], rhs=vd[:, h, :], start=True, stop=True) for h in range(4)],
                     reads=["k_rota", "k_rotb", "vd"], writes=["B2", "B3"])
                P.op("dve", [lambda e, h=h: e.scalar_tensor_tensor(out=S[:, h, :], in0=S[:, h, :], scalar=GAMC[h], in1=kvps[:, h, :],
                                                                   op0=ALU.mult, op1=ALU.add) for h in range(4)],
                     reads=["B2", "B3", "S"], writes=["S"])
                P.op("act", lambda e: e.copy(out=S_bf, in_=S), reads=["S"], writes=["S_bf"])
                if full and t == 0: _stop(16)
                if full:
                    yps = h4(PB[0][:])
                    P.op("dve", [lambda e, h=h: e.bn_stats(out=stt[:, h, :], in_=yps[:, h, :]) for h in range(4)], reads=["B0", "B1"], writes=["stt"])
                    P.op("dve", [lambda e, h=h: e.bn_aggr(out=mv[:, h, :], in_=stt[:, h, :]) for h in range(4)], reads=["stt"], writes=["mv"])
                    P.op("act", lambda e: e.activation(out=sq, in_=mv[:, :, 1], func=AF.Sqrt, bias=epsT[:, 0:1]), reads=["mv", "epsT"], writes=["sq"])
                    P.op("dve", lambda e: e.reciprocal(out=rstd, in_=sq), reads=["sq"], writes=["rstd"])
                    P.op("dve", [lambda e, h=h: e.scalar_tensor_tensor(out=yn[:, h, :], in0=yps[:, h, :], scalar=mv[:, h, 0:1], in1=gnw[:, h, :],
                                                                       op0=ALU.subtract, op1=ALU.mult) for h in range(4)],
                         reads=["B0", "B1", "mv", "gnw"], writes=["yn"])
                    yr = h4(yret)
                    P.op("dve", [lambda e, h=h: e.scalar_tensor_tensor(out=yr[:, h, :], in0=yn[:, h, :], scalar=rstd[:, h:h + 1], in1=sg[:, h, :],
                                                                       op0=ALU.mult, op1=ALU.mult) for h in range(4)],
                         reads=["yn", "rstd", "sg"], writes=["yret"])
                    if t == 0: _stop(17)
                    tp = bank_bf(6).rearrange("p (k c) -> p k c", k=8)
                    P.op("pe", [lambda e, k=k: e.transpose(out=tp[:, k, :], in_=yret[:, k * 128:(k + 1) * 128], identity=identb[:]) for k in range(8)],
                         reads=["yret", "identb"], writes=["B6"])
                    P.op("act", lambda e: e.copy(out=Z[:, :, t * 128:(t + 1) * 128], in_=tp), reads=["B6"], writes=[f"yT{t}", f"xTp{t}"])
                    if t == 0: _stop(18)

            _stop(10)
            for t in range(NT):
                ret_chunk(xTp, "xTp", t, tab_prev, "tab_prev", False)
                if t == 0:
                    _stop(11)
            _stop(12)
            def o_e1k(t):
                proj_tok(1, xT, t, "wk", [f"xT{t}"])

            def o_e1(t):
                sk = [f"xT{t}"]
                proj_tok(2, xT, t, "wv0", sk)
                proj_tok(3, xT, t, "wv1", sk)
                proj_tok(0, xT, t, "wq", sk)

            def o_e2(t):
                if t == 0:
                    rot(1, tab_own, t, k_rot, "k_rot", "tab_own")
                vps = h4(PB[1][:])
                P.op("act", [lambda e, h=h: e.activation(out=vd[:, h, :], in_=vps[:, h, :], func=AF.Identity, scale=kdec[:, h:h + 1]) for h in range(4)],
                     reads=["B2", "B3", "kdec"], writes=["vd"])
                rot(0, tab_own, t, q_rot, "q_rot", "tab_own")
                P.op("act", [lambda e, h=h: e.activation(out=qrd[:, h, :], in_=q_rot[:, h, :], func=AF.Identity, scale=qdec[:, h:h + 1]) for h in range(4)],
                     reads=["q_rota", "q_rotb", "qdec"], writes=["qrd"])
                tp = bank_bf(6).rearrange("p (k c) -> p k c", k=8)
                tpd = bank_bf(7)[:, 0:512].rearrange("p (k c) -> p k c", k=4)
                P.op("pe", [lambda e, h=h: e.transpose(out=tp[:, h, :], in_=q_rot[:, h, :], identity=identb[:]) for h in range(4)] +
                     [lambda e, h=h: e.transpose(out=tp[:, 4 + h, :], in_=k_rot[:, h, :], identity=identb[:]) for h in range(4)],
                     reads=["q_rota", "q_rotb", "k_rota", "k_rotb", "identb"], writes=["B6"])
                P.op("pe", [lambda e, h=h: e.transpose(out=tpd[:, h, :], in_=qrd[:, h, :], identity=identb[:]) for h in range(4)],
                     reads=["qrd", "identb"], writes=["B7"])
                P.op("act", lambda e: e.copy(out=qT, in_=tp[:, 0:4, :]), reads=["B6"], writes=["qT"])
                P.op("dve", lambda e: e.tensor_copy(out=kT, in_=tp[:, 4:8, :]), reads=["B6", "qT"], writes=["kT"])
                P.op("act", lambda e: e.copy(out=qTd, in_=tpd), reads=["B7"], writes=["qTd"])
                sc = h4(bank(7))
                P.op("pe", [lambda e, h=h: e.matmul(sc[:, h, :], lhsT=kT[:, h, :], rhs=qT[:, h, :], start=True, stop=True) for h in range(4)],
                     reads=["kT", "qT"], writes=["B7"])
                P.op("dve", lambda e: e.tensor_tensor(out=smk, in0=sc, in1=maskT, op=ALU.mult), reads=["B7", "maskT"], writes=["smk"])

            def o_g(t):
                sk = [f"xT{t}"]
                proj_tok(4, xT, t, "wg0", sk)
                proj_tok(5, xT, t, "wg1", sk)

            def o_e3(t):
                P.op("act", lambda e: e.activation(out=sg.rearrange("p h c -> p (h c)"), in_=PB[2][:], func=AF.Silu),
                     reads=["B4", "B5"], writes=["sg"])

            def o_e4(t):
                yps = h4(PB[2][:])
                fl = []
                for h in range(4):
                    fl.append(lambda e, h=h: e.matmul(yps[:, h, :], lhsT=smk[:, h, :], rhs=vd[:, h, :], start=True, stop=False))
                    fl.append(lambda e, h=h: e.matmul(yps[:, h, :], lhsT=qTd[:, h, :], rhs=S_bf[:, h, :], start=False, stop=True))
                P.op("pe", fl, reads=["smk", "vd", "qTd", "S_bf"], writes=["B4", "B5"])

            def o_e5(t):
                kvps = h4(PB[1][:])
                P.op("pe", [lambda e, h=h: e.matmul(kvps[:, h, :], lhsT=k_rot[:, h, :], rhs=vd[:, h, :], start=True, stop=True) for h in range(4)],
                     reads=["k_rota", "k_rotb", "vd"], writes=["B2", "B3"])
                P.op("dve", [lambda e, h=h: e.scalar_tensor_tensor(out=S[:, h, :], in0=S[:, h, :], scalar=GAMC[h], in1=kvps[:, h, :],
                                                                   op0=ALU.mult, op1=ALU.add) for h in range(4)],
                     reads=["B2", "B3", "S"], writes=["S"])
                P.op("act", lambda e: e.copy(out=S_bf, in_=S), reads=["S"], writes=["S_bf"])

            def o_e6a(t):
                yps = h4(PB[2][:])
                P.op("dve", [lambda e, h=h: e.bn_stats(out=stt[:, h, :], in_=yps[:, h, :]) for h in range(4)], reads=["B4", "B5"], writes=["stt"])
                P.op("dve", [lambda e, h=h: e.bn_aggr(out=mv[:, h, :], in_=stt[:, h, :]) for h in range(4)], reads=["stt"], writes=["mv"])
                P.op("act", lambda e: e.activation(out=sq, in_=mv[:, :, 1], func=AF.Sqrt, bias=epsT[:, 0:1]), reads=["mv", "epsT"], writes=["sq"])
                if t + 1 < NT:
                    rot(1, tab_own, t + 1, k_rot, "k_rot", "tab_own")
                P.op("dve", lambda e: e.reciprocal(out=rstd, in_=sq), reads=["sq"], writes=["rstd"])
                P.op("dve", [lambda e, h=h: e.scalar_tensor_tensor(out=yn[:, h, :], in0=yps[:, h, :], scalar=mv[:, h, 0:1], in1=gnw[:, h, :],
                                                                   op0=ALU.subtract, op1=ALU.mult) for h in range(4)],
                     reads=["B4", "B5", "mv", "gnw"], writes=["yn"])
                yr = h4(yret)
                P.op("dve", [lambda e, h=h: e.scalar_tensor_tensor(out=yr[:, h, :], in0=yn[:, h, :], scalar=rstd[:, h:h + 1], in1=sg[:, h, :],
                                                                   op0=ALU.mult, op1=ALU.mult) for h in range(4)],
                     reads=["yn", "rstd", "sg"], writes=["yret"])

            def o_e6b(t):
                tp = bank_bf(6).rearrange("p (k c) -> p k c", k=8)
                P.op("pe", [lambda e, k=k: e.transpose(out=tp[:, k, :], in_=yret[:, k * 128:(k + 1) * 128], identity=identb[:]) for k in range(8)],
                     reads=["yret", "identb"], writes=["B6"])
                P.op("act", lambda e, t=t: e.copy(out=Z[:, :, t * 128:(t + 1) * 128], in_=tp), reads=["B6"], writes=[f"yT{t}", f"xTp{t}"])

            o_e1k(0)
            o_e1(0)
            for t in range(NT):
                o_g(t)
                o_e2(t)
                o_e3(t)
                if t == NT - 1:
                    W.release("wg0")
                    W.release("wg1")
                o_e4(t)
                o_e5(t)
                if t + 1 < NT:
                    o_e1k(t + 1)
                o_e6a(t)
                if t + 1 < NT:
                    o_e1(t + 1)
                    if t + 1 == NT - 1:
                        for nm in ("wk", "wv0", "wv1", "wq"):
                            W.release(nm)
                o_e6b(t)
            P.barrier()
            _stop(2)
            if debug:
                P.dma("sp", lambda e: e.dma_start(out=dbg["z"], in_=Z), "st_dbg", reads=[f"yT{t}" for t in range(NT)], is_store=True)

            gsb = [acc_f32(SC0 + i * 2048, 512) for i in range(2)]
            tmpm = [acc_bf16(SC0 + 4096 + i * 2048, 512) for i in range(2)]
            MERGE_KEYS = [f"M{j}_{g}" for j in range(8) for g in range(NG)]

            def merge_branch(yT, ykeys, nkc, gta, gtb, wlist, first, hook=None):
                ga_s, ga_k = W.get(gta)
                gb_s, gb_k = W.get(gtb)
                it = 0
                for j in range(8):
                    if j == 4:
                        W.release(gta)
                        if nkc == 8:
                            W.release(wlist[0])
                    gs_, gk_ = (ga_s, ga_k) if j < 4 else (gb_s, gb_k)
                    gv = gs_[:].rearrange("p (k n) -> p k n", k=8)
                    if nkc == 8:
                        ws_, wk_ = W.get(wlist[0] if j < 4 else wlist[1])
                        wv = ws_[:].rearrange("p (k n) -> p k n", k=8)
                        lhs = [wv[:, k, (j % 4) * 128:(j % 4) * 128 + 128] for k in range(8)]
                    else:
                        ws_, wk_ = W.get(wlist[0])
                        wv = ws_[:].rearrange("p (k n) -> p k n", k=4)
                        lhs = [wv[:, k, j * 128:(j + 1) * 128] for k in range(4)]
                    for g in range(NG):
                        b = it % 2
                        it += 1
                        cols = slice(g * 512, (g + 1) * 512)
                        P.op("pe", [lambda e, k=k, gv=gv, j=j, cols=cols, b=b: e.matmul(bank(b), lhsT=gv[:, k, (j % 4) * 128:(j % 4) * 128 + 128],
                                                                                      rhs=xT[:, k, cols], start=(k == 0), stop=(k == 7)) for k in range(8)],
                             reads=gk_ + XT_ALL[g * 4:(g + 1) * 4], writes=[f"B{b}"])
                        P.op("pe", [lambda e, k=k, lhs=lhs, cols=cols, b=b: e.matmul(bank(2 + b), lhsT=lhs[k], rhs=yT[:, k, cols],
                                                                                   start=(k == 0), stop=(k == nkc - 1)) for k in range(nkc)],
                             reads=wk_ + ykeys, writes=[f"B{2 + b}"])
                        P.op("act", lambda e, b=b: e.activation(out=gsb[b], in_=bank(b), func=AF.Sigmoid), reads=[f"B{b}"], writes=[f"gsb{b}"])
                        mk = f"M{j}_{g}"
                        if first:
                            P.op("dve", lambda e, b=b, j=j, cols=cols: e.tensor_tensor(out=M[:, j, cols], in0=bank(2 + b), in1=gsb[b], op=ALU.mult),
                                 reads=[f"B{2 + b}", f"gsb{b}"], writes=[mk])
                        else:
                            P.op("dve", lambda e, b=b: e.tensor_tensor(out=tmpm[b], in0=bank(2 + b), in1=gsb[b], op=ALU.mult),
                                 reads=[f"B{2 + b}", f"gsb{b}"], writes=[f"tmpm{b}"])
                            P.op("dve", lambda e, b=b, j=j, cols=cols: e.tensor_tensor(out=M[:, j, cols], in0=M[:, j, cols], in1=tmpm[b], op=ALU.add),
                                 reads=[f"tmpm{b}", mk], writes=[mk])
                        if hook is not None:
                            hook(it - 1)

            UW = 16 + 512
            ub = [acc_f32(40960 + i * 2176, UW) for i in range(3)]
            pooled = acc_bf16(47488, 512)
            YP = acc_bf16(48512, 4 * T).rearrange("p (k t) -> p k t", k=4)
            wp_s, wp_k = W.get("wpool")
            wpv = wp_s[:].rearrange("p (k n) -> p k n", k=8)
            WINS = (2, 4, 8, 16)

            def pool_iter(i):
                gi, g = i // NG, i % NG
                b = i % 2
                P.op("pe", [lambda e, k=k: e.matmul(bank(4 + b), lhsT=wpv[:, k, gi * 128:(gi + 1) * 128],
                                                    rhs=xT[:, k, g * 512:(g + 1) * 512], start=(k == 0), stop=(k == 7)) for k in range(8)],
                     reads=wp_k + XT_ALL[g * 4:(g + 1) * 4], writes=[f"B{4 + b}"])
                if g == 0:
                    hb = 7 - b
                    P.op("pe", [lambda e, k=k: e.matmul(bank(hb)[:, 0:16], lhsT=wpv[:, k, gi * 128:(gi + 1) * 128], rhs=xTh[:, k, :],
                                                        start=(k == 0), stop=(k == 7)) for k in range(8)],
                         reads=wp_k + ["xTh"], writes=[f"B{hb}"])
                    P.op("act", lambda e: e.copy(out=ub[0][:, 0:16], in_=bank(hb)[:, 0:16]), reads=[f"B{hb}"], writes=["ub0h"])
                P.op("act", lambda e: e.copy(out=ub[0][:, 16:UW], in_=bank(4 + b)), reads=[f"B{4 + b}"], writes=["ub0m"])
                cur = 0
                for s_ in range(gi + 1):
                    sh = 1 << s_
                    nxt = 1 if cur != 1 else 2
                    P.op("dve", lambda e, cur=cur, nxt=nxt, sh=sh: e.tensor_tensor(out=ub[nxt][:, sh:UW], in0=ub[cur][:, sh:UW],
                                                                                  in1=ub[cur][:, 0:UW - sh], op=ALU.add),
                         reads=["ub0m", "ub0h", f"ubs{cur}"], writes=[f"ubs{nxt}"])
                    cur = nxt
                w = WINS[gi]
                P.op("dve", lambda e, cur=cur: e.scalar_tensor_tensor(out=pooled, in0=ub[cur][:, 16:UW], scalar=float(1.0 / w),
                                                                      in1=ub[0][:, 16:UW], op0=ALU.mult, op1=ALU.subtract),
                     reads=[f"ubs{cur}", "ub0m"], writes=["pooled"])
                if g == 0:
                    P.op("dve", lambda e, cur=cur: e.tensor_tensor(out=ub[cur][:, 16:32], in0=ub[cur][:, 16:32], in1=invcnt[:, gi, :], op=ALU.mult),
                         reads=[f"ubs{cur}", "invcnt", "pooled"], writes=[f"ubs{cur}"])
                    P.op("dve", lambda e, cur=cur: e.tensor_tensor(out=pooled[:, 0:16], in0=ub[cur][:, 16:32], in1=ub[0][:, 16:32], op=ALU.subtract),
                         reads=[f"ubs{cur}", "ub0m"], writes=["pooled"])
                if g < NG - 1:
                    P.op("act", lambda e: e.copy(out=ub[0][:, 0:16], in_=ub[0][:, UW - 16:UW]), reads=["ub0m"], writes=["ub0h"])

            def pool_iter_b(i):
                gi, g = i // NG, i % NG
                b = i % 2
                P.op("pe", lambda e: e.matmul(bank(6 + b), lhsT=wgrp_b[:, gi, :], rhs=pooled, start=True, stop=True),
                     reads=["wgrp_b", "pooled"], writes=[f"B{6 + b}"])
                P.op("act", lambda e: e.activation(out=YP[:, gi, g * 512:(g + 1) * 512], in_=bank(6 + b), func=AF.Identity,
                                                   scale=pscale[:, gi:gi + 1]),
                     reads=[f"B{6 + b}", "pscale"], writes=[f"yp{gi}_{g}"])

            def pool_hook(it):
                if it % 2 == 1:
                    pool_iter(it // 2)
                elif it >= 2:
                    pool_iter_b(it // 2 - 1)

            YT_KEYS = [f"yT{t}" for t in range(NT)]
            merge_branch(Z, YT_KEYS, 8, "gt1a", "gt1b", ["wbr0", "wbr1"], True, hook=pool_hook)
            pool_iter_b(15)
            for nm in ("gt1a", "gt1b", "wbr0", "wbr1"):
                W.release(nm)
            P.barrier()
            _stop(3)

            W.release("wpool")
            YP_KEYS = [f"yp{gi}_{g}" for gi in range(4) for g in range(NG)]
            merge_branch(YP, YP_KEYS, 4, "gt0a", "gt0b", ["wbp"], False)
            for nm in ("gt0a", "gt0b", "wbp"):
                W.release(nm)
            P.barrier()
            _stop(4)

            memT = acc_bf16(40960, 8 * 256).rearrange("p (k m) -> p k m", k=8)
            KmT = acc_bf16(45056, 4 * 256).rearrange("p (h m) -> p h m", h=4)
            Vm = acc_bf16(47104, 2 * 512).rearrange("p (c n) -> p c n", c=2)
            xqT = acc_bf16(49152, 512)
            expT = acc_bf16(50176, 1024).rearrange("p (c n) -> p c n", c=2)
            lnS = acc_f32(52224, 512)
            rS = acc_f32(54272, 512)
            xsX = [acc_f32(57344, 1024)]
            xbX = [acc_bf16(61440, 1024)]
            load_transposed(mem, memT, 2, "memT", xsX, xbX)
            wmk_s, wmk_k = W.get("wmk")
            wmv_s, wmv_k = W.get("wmv")
            wmkv = wmk_s[:].rearrange("p (k n) -> p k n", k=8)
            wmvv = wmv_s[:].rearrange("p (k n) -> p k n", k=8)
            for h in range(4):
                b = h % 2
                P.op("pe", [lambda e, k=k, h=h, b=b: e.matmul(bank(4 + b)[:, 0:256], lhsT=wmkv[:, k, h * 128:(h + 1) * 128], rhs=memT[:, k, :],
                                                            start=(k == 0), stop=(k == 7)) for k in range(8)],
                     reads=wmk_k + ["memT0", "memT1"], writes=[f"B{4 + b}"])
                P.op("act", lambda e, h=h, b=b: e.copy(out=KmT[:, h, :], in_=bank(4 + b)[:, 0:256]), reads=[f"B{4 + b}"], writes=[f"KmT{h}"])
            for c in range(2):
                b = c % 2
                P.op("pe", [lambda e, k=k, c=c, b=b: e.matmul(bank(4 + b), lhsT=memT[:, k, c * 128:(c + 1) * 128], rhs=wmvv[:, k, :],
                                                            start=(k == 0), stop=(k == 7)) for k in range(8)],
                     reads=wmv_k + ["memT0", "memT1"], writes=[f"B{4 + b}"])
                P.op("act", lambda e, c=c, b=b: e.copy(out=Vm[:, c, :], in_=bank(4 + b)), reads=[f"B{4 + b}"], writes=[f"Vm{c}"])
            W.release("wmk")
            W.release("wmv")
            wxq_s, wxq_k = W.get("wxq")
            wxqv = wxq_s[:].rearrange("p (k n) -> p k n", k=8)
            YX = Z
            XSC = float(128.0 ** -0.5)
            expT2 = [expT, acc_bf16(63488, 1024).rearrange("p (c n) -> p c n", c=2)]

            def xa_a(i):
                h, g = i // NG, i % NG
                p = i % 2
                cols = slice(g * 512, (g + 1) * 512)
                P.op("pe", [lambda e, k=k: e.matmul(bank(4), lhsT=wxqv[:, k, h * 128:(h + 1) * 128], rhs=xT[:, k, cols],
                                                    start=(k == 0), stop=(k == 7)) for k in range(8)],
                     reads=wxq_k + XT_ALL[g * 4:(g + 1) * 4], writes=["B4"])
                P.op("act", lambda e: e.activation(out=xqT, in_=bank(4), func=AF.Copy, scale=XSC), reads=["B4"], writes=["xqT"])
                for c in range(2):
                    P.op("pe", lambda e, c=c: e.matmul(bank(5 + c), lhsT=KmT[:, h, c * 128:(c + 1) * 128], rhs=xqT, start=True, stop=True),
                         reads=[f"KmT{h}", "xqT"], writes=[f"B{5 + c}"])
                    P.op("act", lambda e, c=c: e.activation(out=expT2[p][:, c, :], in_=bank(5 + c), func=AF.Exp), reads=[f"B{5 + c}"],
                         writes=[f"expT{p}{c}"])

            def xa_b(i):
                h, g = i // NG, i % NG
                p = i % 2
                cols = slice(g * 512, (g + 1) * 512)
                ek = [f"expT{p}0", f"expT{p}1"]
                P.op("pe", [lambda e, c=c: e.matmul(bank(7), lhsT=Vm[:, c, h * 128:(h + 1) * 128], rhs=expT2[p][:, c, :], start=(c == 0), stop=(c == 1))
                            for c in range(2)], reads=["Vm0", "Vm1"] + ek, writes=["B7"])
                P.op("pe", [lambda e, c=c: e.matmul(bank(0), lhsT=onesb[:], rhs=expT2[p][:, c, :], start=(c == 0), stop=(c == 1)) for c in range(2)],
                     reads=["onesb"] + ek, writes=["B0"])
                P.op("act", lambda e: e.activation(out=lnS, in_=bank(0), func=AF.Ln), reads=["B0"], writes=["lnS"])
                P.op("act", lambda e: e.activation(out=rS, in_=lnS, func=AF.Exp, scale=-1.0), reads=["lnS"], writes=["rS"])
                P.op("dve", lambda e: e.tensor_tensor(out=YX[:, h, cols], in0=bank(7), in1=rS, op=ALU.mult),
                     reads=["B7", "rS"], writes=[f"yx{h}_{g}"])

            xa_a(0)
            for i in range(16):
                if i + 1 < 16:
                    xa_a(i + 1)
                xa_b(i)
            W.release("wxq")
            YX_KEYS = [f"yx{h}_{g}" for h in range(4) for g in range(NG)]
            merge_branch(YX, YX_KEYS, 4, "gt2a", "gt2b", ["wbx"], False)
            for nm in ("gt2a", "gt2b", "wbx"):
                W.release(nm)
            P.barrier()
            _stop(5)
            if debug:
                P.dma("sp", lambda e: e.dma_start(out=dbg["mT"], in_=M[:]), "st_dbg", reads=MERGE_KEYS, is_store=True)

            ld(lnw[:], ln1_w.partition_broadcast(128), "lnw")
            ld(lnb[:], ln1_b.partition_broadcast(128), "lnb")
            wo0_s, wo0_k = W.get("wo0")
            wo1_s, wo1_k = W.get("wo1")
            wov = [wo0_s[:].rearrange("p (k n) -> p k n", k=8), wo1_s[:].rearrange("p (k n) -> p k n", k=8)]
            x1T = A[:]
            st6 = small[:, 40:52].rearrange("p (c s) -> p c s", c=2)
            mv1 = small[:, 52:54]
            r1 = small[:, 54:55]
            nmr = small[:, 55:56]
            s1 = small[:, 56:57]
            L = small[:, 64:100]
            gneg = small[:, 100:101]
            gsum = small[:, 101:102]
            pgrp = small[:, 102:103]
            oh = small[:, 104:108]
            ge = small[:, 108:112]
            sel = small[:, 112:120]
            sel2 = small[:, 120:128]
            m1 = small[:, 128:129]
            m2 = small[:, 129:130]
            dd = small[:, 130:131]
            e2 = small[:, 131:132]
            w1p = small[:, 132:133]
            w2p = small[:, 133:134]
            mk1 = small[:, 136:144]
            mk2 = small[:, 144:152]
            wg8 = small[:, 152:160]

            def layer_norm(src, dst, tagr, tagw):
                P.op("dve", [lambda e, c=c: e.bn_stats(out=st6[:, c, :], in_=src[:, c * 512:(c + 1) * 512]) for c in range(2)], reads=tagr, writes=["st6"])
                P.op("dve", lambda e: e.bn_aggr(out=mv1, in_=st6.rearrange("p c s -> p (c s)")), reads=["st6"], writes=["mv1"])
                P.op("act", lambda e: e.activation(out=s1, in_=mv1[:, 1:2], func=AF.Sqrt, bias=epsT[:, 0:1]), reads=["mv1", "epsT"], writes=["s1"])
                P.op("dve", lambda e: e.reciprocal(out=r1, in_=s1), reads=["s1"], writes=["r1"])
                P.op("dve", lambda e: e.scalar_tensor_tensor(out=nmr, in0=mv1[:, 0:1], scalar=-1.0, in1=r1, op0=ALU.mult, op1=ALU.mult),
                     reads=["mv1", "r1"], writes=["nmr"])
                P.op("act", lambda e: e.activation(out=dst, in_=src, func=AF.Identity, scale=r1, bias=nmr), reads=tagr + ["r1", "nmr"], writes=tagw)
                P.op("dve", lambda e: e.tensor_tensor(out=dst, in0=dst, in1=lnw[:], op=ALU.mult), reads=tagw + ["lnw"], writes=tagw)
                P.op("dve", lambda e: e.tensor_tensor(out=dst, in0=dst, in1=lnb[:], op=ALU.add), reads=tagw + ["lnb"], writes=tagw)

            Lg = small[:, 160:224].rearrange("p (t g) -> p t g", t=NT)

            def attn_mm(t):
                bi = t % 2
                fl = []
                for hf in range(2):
                    for k in range(8):
                        fl.append(lambda e, hf=hf, k=k: e.matmul(PB[bi][:, hf * 512:(hf + 1) * 512], lhsT=M[:, k, t * 128:(t + 1) * 128],
                                                                rhs=wov[hf][:, k, :], start=(k == 0), stop=(k == 7)))
                P.op("pe", fl, reads=wo0_k + wo1_k + [f"M{j}_{t // 4}" for j in range(8)], writes=[f"B{2 * bi}", f"B{2 * bi + 1}"])

            def front_ops(t):
                bi = t % 2
                buf = xo[bi]
                bk = f"xo{bi}"
                o = 40 + bi * 20
                st6p = small[:, o:o + 12].rearrange("p (c s) -> p c s", c=2)
                mv1p = small[:, o + 12:o + 14]
                r1p = small[:, o + 14:o + 15]
                nmrp = small[:, o + 15:o + 16]
                s1p = small[:, o + 16:o + 17]
                k = f"ln{bi}"
                return [
                    lambda: P.dma("sp", lambda e: e.dma_start(out=buf, in_=x_own[t * 128:(t + 1) * 128, :]), f"ld_xo{bi}", writes=[bk]),
                    lambda: P.op("dve", lambda e: e.scalar_tensor_tensor(out=buf, in0=buf, scalar=ALPHA, in1=PB[bi][:], op0=ALU.mult, op1=ALU.add),
                                 reads=[bk, f"B{2 * bi}", f"B{2 * bi + 1}"], writes=[bk]),
                    lambda: P.op("dve", [lambda e, c=c: e.bn_stats(out=st6p[:, c, :], in_=buf[:, c * 512:(c + 1) * 512]) for c in range(2)],
                                 reads=[bk], writes=[k + "st"]),
                    lambda: P.op("dve", lambda e: e.bn_aggr(out=mv1p, in_=st6p.rearrange("p c s -> p (c s)")), reads=[k + "st"], writes=[k + "mv"]),
                    lambda: P.op("act", lambda e: e.activation(out=s1p, in_=mv1p[:, 1:2], func=AF.Sqrt, bias=epsT[:, 0:1]), reads=[k + "mv", "epsT"], writes=[k + "s1"]),
                    lambda: P.op("dve", lambda e: e.reciprocal(out=r1p, in_=s1p), reads=[k + "s1"], writes=[k + "r1"]),
                    lambda: P.op("dve", lambda e: e.scalar_tensor_tensor(out=nmrp, in0=mv1p[:, 0:1], scalar=-1.0, in1=r1p, op0=ALU.mult, op1=ALU.mult),
                                 reads=[k + "mv", k + "r1"], writes=[k + "nmr"]),
                    lambda: P.op("act", lambda e: e.activation(out=buf, in_=buf, func=AF.Identity, scale=r1p, bias=nmrp), reads=[bk, k + "r1", k + "nmr"], writes=[bk]),
                    lambda: P.op("dve", lambda e: e.tensor_tensor(out=buf, in0=buf, in1=lnw[:], op=ALU.mult), reads=[bk, "lnw"], writes=[bk]),
                    lambda: P.op("dve", lambda e: e.tensor_tensor(out=buf, in0=buf, in1=lnb[:], op=ALU.add), reads=[bk, "lnb"], writes=[bk]),
                ]

            def back_ops(t):
                bi = t % 2
                buf = xo[bi]
                bk = f"xo{bi}"
                bh, bl = 4 + 2 * bi, 5 + 2 * bi
                tph = bank_bf(bh).rearrange("p (k c) -> p k c", k=8)
                tpl = bank_bf(bl).rearrange("p (k c) -> p k c", k=8)
                lg = bank(bh)[:, 0:36]

                def router():
                    fl = []
                    for k in range(8):
                        fl.append(lambda e, k=k: e.matmul(lg, lhsT=x1T[:, k, t * 128:(t + 1) * 128], rhs=wr_hi[:, k, :], start=(k == 0), stop=False))
                        fl.append(lambda e, k=k: e.matmul(lg, lhsT=x1Tlo[:, k, :], rhs=wr_hi[:, k, :], start=False, stop=False))
                        fl.append(lambda e, k=k: e.matmul(lg, lhsT=x1T[:, k, t * 128:(t + 1) * 128], rhs=wr_lo[:, k, :], start=False, stop=(k == 7)))
                    P.op("pe", fl, reads=[f"x1T{t}", "x1Tlo", "wr_hi", "wr_lo"], writes=[f"B{bh}"])
                return [
                    lambda: P.op("act", lambda e: e.copy(out=hib, in_=buf), reads=[bk], writes=["hib"]),
                    lambda: P.op("act", lambda e: e.copy(out=ACC[:, t, :], in_=hib), reads=["hib"], writes=[f"acc{t}"]),
                    lambda: P.op("dve", lambda e: e.tensor_tensor(out=lob, in0=buf, in1=ACC[:, t, :], op=ALU.subtract), reads=[bk, f"acc{t}"], writes=["lob"]),
                    lambda: P.op("act", lambda e: e.activation(out=ACC[:, t, :], in_=buf, func=AF.Copy, scale=ALPHA), reads=[bk], writes=[f"acc{t}"]),
                    lambda: P.op("pe", [lambda e, k=k: e.transpose(out=tph[:, k, :], in_=hib[:, k * 128:(k + 1) * 128], identity=identb[:]) for k in range(8)],
                                 reads=["hib", "identb"], writes=[f"B{bh}"]),
                    lambda: P.op("pe", [lambda e, k=k: e.transpose(out=tpl[:, k, :], in_=lob[:, k * 128:(k + 1) * 128], identity=identb[:]) for k in range(8)],
                                 reads=["lob", "identb"], writes=[f"B{bl}"]),
                    lambda: P.op("act", lambda e: e.copy(out=x1T[:, :, t * 128:(t + 1) * 128], in_=tph), reads=[f"B{bh}"], writes=[f"x1T{t}"]),
                    lambda: P.op("dve", lambda e: e.tensor_copy(out=x1Tlo[:], in_=tpl), reads=[f"B{bl}"], writes=["x1Tlo"]),
                    router,
                    lambda: P.op("dve", lambda e: e.tensor_tensor(out=Lg[:, t, :], in0=lg[:, 0:4], in1=br32[:, 0:4], op=ALU.add), reads=[f"B{bh}", "br32"], writes=[f"Lg{t}"]),
                    lambda: P.op("dve", lambda e: e.tensor_tensor(out=gates[:, t, :], in0=lg[:, 4:36], in1=br32[:, 4:36], op=ALU.add), reads=[f"B{bh}", "br32"], writes=[f"gate{t}"]),
                ]

            attn_mm(0)
            attn_mm(1)
            for f in front_ops(0):
                f()
            FO = [0, 1, 2, 3, 4, 5, 6, 7, 8, 9]
            ORDER = [("f", 0), ("f", 1), ("b", 0), ("f", 2), ("b", 1), ("f", 3), ("f", 4), ("b", 2), ("f", 5), ("f", 6), ("b", 3),
                     ("f", 7), ("b", 4), ("b", 5), ("b", 6), ("f", 8), ("b", 7), ("b", 8), ("f", 9), ("b", 9), ("b", 10)]
            for t in range(NT):
                if t + 2 < NT:
                    attn_mm(t + 2)
                fl_ = front_ops(t + 1) if t + 1 < NT else None
                bl_ = back_ops(t)
                for kind, idx in ORDER:
                    if kind == "f":
                        if fl_ is not None:
                            fl_[idx]()
                    else:
                        bl_[idx]()
            P.barrier()
            GK = [f"gate{t}" for t in range(NT)]
            LK = [f"Lg{t}" for t in range(NT)]
            un = lambda o, n: UN[:, o:o + n]
            gmax = un(0, 16)
            gsum = un(16, 16)
            pgrp = un(32, 16)
            m1 = un(48, 16)
            m2 = un(64, 16)
            e2 = un(80, 16)
            w1 = un(96, 16)
            w2 = un(112, 16)
            Lgs = un(128, 64).rearrange("p (t g) -> p t g", t=NT)
            oh = un(192, 64).rearrange("p (t g) -> p t g", t=NT)
            ge = un(256, 64).rearrange("p (t g) -> p t g", t=NT)
            sel = un(320, 128).rearrange("p (t e) -> p t e", t=NT)
            sel2 = un(448, 128).rearrange("p (t e) -> p t e", t=NT)
            mk1 = un(576, 128).rearrange("p (t e) -> p t e", t=NT)
            mk2 = un(704, 128).rearrange("p (t e) -> p t e", t=NT)
            wg8 = un(832, 128).rearrange("p (t e) -> p t e", t=NT)
            E4 = gates[:].rearrange("p t (g e) -> p t g e", g=4)
            E4T = gates[:].rearrange("p t (g e) -> p t e g", g=4)
            bc3 = lambda a, n: a.unsqueeze(2).broadcast_to([128, NT, n])
            P.op("dve", lambda e: e.tensor_reduce(out=gmax, in_=Lg, axis=AX.X, op=ALU.max), reads=LK, writes=["gmax"])
            P.op("dve", lambda e: e.tensor_tensor(out=Lgs, in0=Lg, in1=bc3(gmax, 4), op=ALU.subtract), reads=LK + ["gmax"], writes=["Lgs"])
            P.op("dve", lambda e: e.tensor_scalar(out=oh, in0=Lgs, scalar1=0.0, scalar2=None, op0=ALU.is_equal), reads=["Lgs"], writes=["oh"])
            P.op("act", lambda e: e.activation(out=ge, in_=Lgs, func=AF.Exp), reads=["Lgs"], writes=["ge"])
            P.op("dve", lambda e: e.tensor_reduce(out=gsum, in_=ge, axis=AX.X, op=ALU.add), reads=["ge"], writes=["gsum"])
            P.op("dve", lambda e: e.reciprocal(out=pgrp, in_=gsum), reads=["gsum"], writes=["pgrp"])
            P.op("dve", lambda e: e.tensor_tensor(out=E4, in0=E4, in1=oh.unsqueeze(3).broadcast_to([128, NT, 4, 8]), op=ALU.mult), reads=GK + ["oh"], writes=GK)
            P.op("dve", lambda e: e.tensor_reduce(out=sel, in_=E4T, axis=AX.X, op=ALU.add), reads=GK, writes=["sel"])
            P.op("dve", lambda e: e.tensor_reduce(out=m1, in_=sel, axis=AX.X, op=ALU.max), reads=["sel"], writes=["m1"])
            P.op("dve", lambda e: e.tensor_tensor(out=mk1, in0=sel, in1=bc3(m1, 8), op=ALU.is_equal), reads=["sel", "m1"], writes=["mk1"])
            P.op("dve", lambda e: e.scalar_tensor_tensor(out=sel2, in0=mk1, scalar=-1.0e30, in1=sel, op0=ALU.mult, op1=ALU.add), reads=["mk1", "sel"], writes=["sel2"])
            P.op("dve", lambda e: e.tensor_reduce(out=m2, in_=sel2, axis=AX.X, op=ALU.max), reads=["sel2"], writes=["m2"])
            P.op("dve", lambda e: e.tensor_tensor(out=mk2, in0=sel2, in1=bc3(m2, 8), op=ALU.is_equal), reads=["sel2", "m2"], writes=["mk2"])
            P.op("dve", lambda e: e.tensor_tensor(out=e2, in0=m2, in1=m1, op=ALU.subtract), reads=["m1", "m2"], writes=["e2"])
            P.op("act", lambda e: e.activation(out=e2, in_=e2, func=AF.Exp), reads=["e2"], writes=["e2"])
            P.op("dve", lambda e: e.tensor_scalar(out=w2, in0=e2, scalar1=1.0, scalar2=None, op0=ALU.add), reads=["e2"], writes=["w2"])
            P.op("dve", lambda e: e.reciprocal(out=w1, in_=w2), reads=["w2"], writes=["w1"])
            P.op("dve", lambda e: e.tensor_tensor(out=w1, in0=w1, in1=pgrp, op=ALU.mult), reads=["w1", "pgrp"], writes=["w1"])
            P.op("dve", lambda e: e.tensor_tensor(out=w2, in0=e2, in1=w1, op=ALU.mult), reads=["e2", "w1", "w2"], writes=["w2"])
            P.op("dve", lambda e: e.tensor_tensor(out=mk1, in0=mk1, in1=bc3(w1, 8), op=ALU.mult), reads=["mk1", "w1"], writes=["mk1"])
            P.op("dve", lambda e: e.tensor_tensor(out=mk2, in0=mk2, in1=bc3(w2, 8), op=ALU.mult), reads=["mk2", "w2"], writes=["mk2"])
            P.op("dve", lambda e: e.tensor_tensor(out=wg8, in0=mk1, in1=mk2, op=ALU.add), reads=["mk1", "mk2"], writes=["wg8"])
            P.op("dve", lambda e: e.tensor_tensor(out=E4, in0=wg8.unsqueeze(2).broadcast_to([128, NT, 4, 8]),
                                                  in1=oh.unsqueeze(3).broadcast_to([128, NT, 4, 8]), op=ALU.mult), reads=["wg8", "oh"], writes=GK)
            W.release("wo0")
            W.release("wo1")
            P.barrier()
            _stop(6)
            if debug:
                P.dma("sp", lambda e: e.dma_start(out=dbg["acc"], in_=ACC[:]), "st_dbg", reads=[f"acc{t}" for t in range(NT)], is_store=True)
                P.dma("sp", lambda e: e.dma_start(out=dbg["gates"], in_=gates[:]), "st_dbg", reads=[f"gate{t}" for t in range(NT)], is_store=True)
                P.barrier()

            sa = [m_f32(i * 4096, 1024).rearrange("p (c n) -> p c n", c=2) for i in range(2)]
            hT = [m_bf16(16384 + i * 2048, 1024).rearrange("p (c n) -> p c n", c=2) for i in range(2)]
            X1T_ALL = [f"x1T{t}" for t in range(NT)]
            ld(lnw[:], ln2_w.partition_broadcast(128), "lnw")
            ld(lnb[:], ln2_b.partition_broadcast(128), "lnb")
            n_exp = NEXP if not MOE_LIMIT else MOE_LIMIT
            slots_of = {}

            def moe_ug(ei, g, sb_):
                gu_s, gu_k = W.get(f"egu{ei}")
                guv = gu_s[:].rearrange("p (k n) -> p k n", k=8)
                cols = slice(g * 512, (g + 1) * 512)
                for fc in range(2):
                    P.op("pe", [lambda e, k=k, fc=fc, cols=cols, guv=guv: e.matmul(bank(fc), lhsT=guv[:, k, fc * 128:(fc + 1) * 128], rhs=x1T[:, k, cols],
                                                                                   start=(k == 0), stop=(k == 7)) for k in range(8)],
                         reads=gu_k + X1T_ALL[g * 4:(g + 1) * 4], writes=[f"B{fc}"])
                    P.op("pe", [lambda e, k=k, fc=fc, cols=cols, guv=guv: e.matmul(bank(2 + fc), lhsT=guv[:, k, 256 + fc * 128:256 + (fc + 1) * 128], rhs=x1T[:, k, cols],
                                                                                   start=(k == 0), stop=(k == 7)) for k in range(8)],
                         reads=gu_k + X1T_ALL[g * 4:(g + 1) * 4], writes=[f"B{2 + fc}"])
                    P.op("act", lambda e, fc=fc, sb_=sb_: e.activation(out=sa[sb_][:, fc, :], in_=bank(fc), func=AF.Silu), reads=[f"B{fc}"], writes=[f"sa{sb_}{fc}"])
                    P.op("dve", lambda e, fc=fc, sb_=sb_: e.tensor_tensor(out=hT[sb_][:, fc, :], in0=bank(2 + fc), in1=sa[sb_][:, fc, :], op=ALU.mult),
                         reads=[f"B{2 + fc}", f"sa{sb_}{fc}"], writes=[f"hT{sb_}{fc}"])
                if g == NG - 1:
                    W.release(f"egu{ei}")

            def moe_d(ei, g, sb_):
                d_s, d_k = W.get(f"ed{ei}")
                dv = d_s[:, 0:2048].rearrange("p (k n) -> p k n", k=2)
                for tt in range(4):
                    t = g * 4 + tt
                    pb = 2 + (tt % 2)
                    fl = []
                    for hf in range(2):
                        for fc in range(2):
                            fl.append(lambda e, hf=hf, fc=fc, tt=tt, pb=pb, sb_=sb_, dv=dv: e.matmul(PB[pb][:, hf * 512:(hf + 1) * 512], lhsT=hT[sb_][:, fc, tt * 128:(tt + 1) * 128],
                                                                                                    rhs=dv[:, fc, hf * 512:(hf + 1) * 512], start=(fc == 0), stop=(fc == 1)))
                    P.op("pe", fl, reads=d_k + [f"hT{sb_}0", f"hT{sb_}1"], writes=[f"B{2 * pb}", f"B{2 * pb + 1}"])
                    P.op("dve", lambda e, t=t, pb=pb, ei=ei: e.scalar_tensor_tensor(out=ACC[:, t, :], in0=PB[pb][:], scalar=gates[:, t, ei:ei + 1], in1=ACC[:, t, :],
                                                                                   op0=ALU.mult, op1=ALU.add),
                         reads=[f"B{2 * pb}", f"B{2 * pb + 1}", f"gate{t}", f"acc{t}"], writes=[f"acc{t}"])
                    if ei == n_exp - 1:
                        bi = t % 2
                        o = xo[bi]
                        layer_norm(ACC[:, t, :], o, [f"acc{t}"], [f"xo{bi}"])
                        P.dma("sp", lambda e, t=t, o=o: e.dma_start(out=y_out[t * 128:(t + 1) * 128, :], in_=o), f"st_y{bi}", reads=[f"xo{bi}"], is_store=True)
                if g == NG - 1:
                    W.release(f"ed{ei}")

            def ug_pieces(ei, g, sb_):
                gu_s, gu_k = W.get(f"egu{ei}")
                guv = gu_s[:].rearrange("p (k n) -> p k n", k=8)
                cols = slice(g * 512, (g + 1) * 512)
                rk = gu_k + X1T_ALL[g * 4:(g + 1) * 4]
                pieces = []
                for fc in range(2):
                    def pa(fc=fc):
                        P.op("pe", [lambda e, k=k, fc=fc: e.matmul(bank(fc), lhsT=guv[:, k, fc * 128:(fc + 1) * 128], rhs=x1T[:, k, cols],
                                                                     start=(k == 0), stop=(k == 7)) for k in range(8)], reads=rk, writes=[f"B{fc}"])
                        P.op("act", lambda e, fc=fc: e.activation(out=sa[sb_][:, fc, :], in_=bank(fc), func=AF.Silu), reads=[f"B{fc}"], writes=[f"sa{sb_}{fc}"])

                    def pb_(fc=fc):
                        P.op("pe", [lambda e, k=k, fc=fc: e.matmul(bank(2 + fc), lhsT=guv[:, k, 256 + fc * 128:256 + (fc + 1) * 128], rhs=x1T[:, k, cols],
                                                                     start=(k == 0), stop=(k == 7)) for k in range(8)], reads=rk, writes=[f"B{2 + fc}"])
                        P.op("dve", lambda e, fc=fc: e.tensor_tensor(out=hT[sb_][:, fc, :], in0=bank(2 + fc), in1=sa[sb_][:, fc, :], op=ALU.mult),
                             reads=[f"B{2 + fc}", f"sa{sb_}{fc}"], writes=[f"hT{sb_}{fc}"])
                    pieces += [pa, pb_]
                return pieces

            def d_tile(ei, g, sb_, tt):
                d_s, d_k = W.get(f"ed{ei}")
                dv = d_s[:, 0:2048].rearrange("p (k n) -> p k n", k=2)
                t = g * 4 + tt
                pb = 2 + (tt % 2)
                fl = []
                for hf in range(2):
                    for fc in range(2):
                        fl.append(lambda e, hf=hf, fc=fc: e.matmul(PB[pb][:, hf * 512:(hf + 1) * 512], lhsT=hT[sb_][:, fc, tt * 128:(tt + 1) * 128],
                                                                  rhs=dv[:, fc, hf * 512:(hf + 1) * 512], start=(fc == 0), stop=(fc == 1)))
                P.op("pe", fl, reads=d_k + [f"hT{sb_}0", f"hT{sb_}1"], writes=[f"B{2 * pb}", f"B{2 * pb + 1}"])
                P.op("dve", lambda e: e.scalar_tensor_tensor(out=ACC[:, t, :], in0=PB[pb][:], scalar=gates[:, t, ei:ei + 1], in1=ACC[:, t, :],
                                                             op0=ALU.mult, op1=ALU.add),
                     reads=[f"B{2 * pb}", f"B{2 * pb + 1}", f"gate{t}", f"acc{t}"], writes=[f"acc{t}"])
                if ei == n_exp - 1:
                    bi = t % 2
                    o = xo[bi]
                    layer_norm(ACC[:, t, :], o, [f"acc{t}"], [f"xo{bi}"])
                    P.dma("sp", lambda e: e.dma_start(out=y_out[t * 128:(t + 1) * 128, :], in_=o), f"st_y{bi}", reads=[f"xo{bi}"], is_store=True)

            steps = [(ei, g) for ei in range(n_exp) for g in range(NG)]
            for i in range(len(steps) + 1):
                pieces = ug_pieces(steps[i][0], steps[i][1], i % 2) if i < len(steps) else [None] * 4
                for q in range(4):
                    if pieces[q] is not None:
                        pieces[q]()
                    if i > 0:
                        pe_, pg_ = steps[i - 1]
                        d_tile(pe_, pg_, (i - 1) % 2, q)
                if i < len(steps) and steps[i][1] == NG - 1:
                    W.release(f"egu{steps[i][0]}")
                if i > 0 and steps[i - 1][1] == NG - 1:
                    W.release(f"ed{steps[i - 1][0]}")
        except _StopBuild:
            P.dma("sp", lambda e: e.dma_start(out=y_out[0:128, 0:128], in_=identf[:]), "st_y0", reads=["identf"], is_store=True)
        P.finish()
        P.emit()
    return nc


MOE_LIMIT = 0
STOP = 0


class _StopBuild(Exception):
    pass


def _stop(n):
    if STOP == n:
        raise _StopBuild()

_NC_CACHE = {}


def _consts(hf):
    c = {}
    c["c_ident"] = np.eye(128, dtype=np.float32)
    inv_freq = (10000.0 ** (-np.arange(64, dtype=np.float32) / np.float32(64))).astype(np.float32)
    c["c_invf"] = np.tile((inv_freq.astype(np.float64) / (2 * np.pi)).astype(np.float32)[None, :], (128, 1))
    hh = np.arange(4, dtype=np.float64)
    lg = np.log1p(-np.exp2(-5.0 - hh))
    s = np.arange(128)[:, None]
    cc = np.arange(128)[None, :]
    maskT = np.zeros((128, 4, 128), np.float64)
    for h in range(4):
        maskT[:, h, :] = np.where(s <= cc, np.exp(lg[h] * (cc - 127.0)), 0.0) * np.ones((128, 1))
    c["c_maskT"] = maskT.astype(np.float32)
    qd = np.exp(lg[None, :] * (np.arange(128)[:, None] + 1.0))
    c["c_qdec"] = qd.astype(np.float32)
    kd = np.exp(lg[None, :] * (127.0 - np.arange(128)[:, None])) * (128.0 ** -0.5)
    c["c_kdec"] = kd.astype(np.float32)
    ic = np.zeros((4, 16), np.float64)
    for gi, w in enumerate((2, 4, 8, 16)):
        for t in range(16):
            ic[gi, t] = 1.0 / (min(t + 1, w) if hf == 0 else w)
    c["c_invcnt"] = np.tile(ic[None, :, :], (128, 1, 1)).astype(np.float32)
    return c


def kernel(x, mem, positions, w_in, w_pool_grp, pool_scale, ret_gn_w, w_mem_kv, w_br_pool, w_br_ret,
           w_br_xa, w_out, ln1_w, ln1_b, w_grp_router, b_grp_router, w_exp_router, b_exp_router,
           w_exp_gate, w_exp_up, w_exp_down, ln2_w, ln2_b, _debug=False):
    f = lambda a: np.ascontiguousarray(np.asarray(a, dtype=np.float32))
    x = f(x)
    mem = f(mem)
    positions = np.asarray(positions, dtype=np.int32)
    shared = {
        "w_in": f(w_in)[0], "w_pool_grp": f(w_pool_grp)[0],
        "pool_scale": np.ascontiguousarray(f(pool_scale)[0].reshape(4, 128).T),
        "ret_gn_w": f(ret_gn_w)[0].reshape(1024), "w_mem_kv": f(w_mem_kv)[0], "w_br_pool": f(w_br_pool)[0],
        "w_br_ret": f(w_br_ret)[0], "w_br_xa": f(w_br_xa)[0], "w_out": f(w_out)[0],
        "ln1_w": f(ln1_w)[0], "ln1_b": f(ln1_b)[0], "ln2_w": f(ln2_w)[0], "ln2_b": f(ln2_b)[0],
        "w_router": np.ascontiguousarray(np.concatenate([f(w_grp_router)[0], f(w_exp_router)[0]], axis=1)),
        "b_router": np.ascontiguousarray(np.concatenate([f(b_grp_router)[0], f(b_exp_router)[0]], axis=0)[None, :]),
        "w_exp_gate": f(w_exp_gate)[0].reshape(32, 1024, 256), "w_exp_up": f(w_exp_up)[0].reshape(32, 1024, 256),
        "w_exp_down": f(w_exp_down)[0].reshape(32, 256, 1024),
    }
    key = bool(_debug)
    if key not in _NC_CACHE:
        _NC_CACHE[key] = build_program(debug=_debug)
    nc = _NC_CACHE[key]
    in_maps = []
    for c in range(8):
        b, hf = c // 2, c % 2
        m = dict(shared)
        m.update(_consts(hf))
        m["x_own"] = np.ascontiguousarray(x[b, hf * T:(hf + 1) * T])
        m["x_prev"] = np.ascontiguousarray(x[b, 0:T]) if hf == 1 else np.zeros((T, D), np.float32)
        po = positions[b, hf * T:(hf + 1) * T].reshape(NT, 128).T
        pp = positions[b, 0:T].reshape(NT, 128).T if hf == 1 else np.zeros((128, NT), np.int32)
        m["pos_own"] = np.ascontiguousarray(po)
        m["pos_prev"] = np.ascontiguousarray(pp)
        m["mem"] = np.ascontiguousarray(mem[b])
        in_maps.append(m)
    res = run_bass_kernel_spmd(nc, in_maps, core_ids=list(range(8)))
    out = np.zeros((4, SEQ, D), np.float32)
    for c in range(8):
        b, hf = c // 2, c % 2
        out[b, hf * T:(hf + 1) * T] = res.results[c]["y_out"]
    if _debug:
        return out, res
    return out
```
